# Optimizing a Trainium2 kernel written in Bass

```python
import math
import jax
import jax.numpy as jnp
from jax import lax
import numpy as np

D_MODEL = 2048
BATCH = 8
SEQ = 2048
DEPTH = 2

GRID_W = 64
CTX_LEN = 256

GDN_HEADS = 16
GDN_DK = 128
GDN_DV = 128
CONV_K = 5
CHUNK = 64
GDN_QK_W = GDN_HEADS * GDN_DK
GDN_V_W = GDN_HEADS * GDN_DV
QKV_W = 2 * GDN_QK_W + GDN_V_W

MLA_HEADS = 16
Q_LORA = 512
KV_LORA = 512
NOPE_DIM = 128
ROPE_DIM = 64
V_DIM = 128
ROPE_THETA = 10000.0
Q_BLOCK = 128
MLA_SCALE = (NOPE_DIM + ROPE_DIM) ** -0.5

FF_DENSE = 5632
N_EXPERTS = 8
TOP_K = 2
FF_EXPERT = 7168
N_DENSE = (DEPTH + 1) // 2
N_MOE = DEPTH // 2

DEEPNORM_ALPHA = (2 * DEPTH) ** 0.25
DEEPNORM_BETA = (8 * DEPTH) ** -0.25
EPS = 1e-6

PROJ_SIZES = (QKV_W, GDN_V_W, 2 * GDN_HEADS, 2 * GDN_HEADS, Q_LORA, KV_LORA, ROPE_DIM, D_MODEL, D_MODEL)
PROJ_W = sum(PROJ_SIZES)

kernel_name = 'hybrid_gdn_mla_moe_diffusion_trunk'


def layer_norm(x, g=None, b=None):
    xf = x.astype(jnp.float32)
    xc = xf - jnp.mean(xf, -1, keepdims=True)
    y = xc * lax.rsqrt(jnp.mean(xc * xc, -1, keepdims=True) + EPS)
    if g is not None:
        y = y * g.astype(jnp.float32) + b.astype(jnp.float32)
    return y.astype(x.dtype)


def rms_norm(x, g):
    xf = x.astype(jnp.float32)
    y = xf * lax.rsqrt(jnp.mean(xf * xf, -1, keepdims=True) + EPS) * g.astype(jnp.float32)
    return y.astype(x.dtype)


def l2_normalize(x):
    xf = x.astype(jnp.float32)
    return xf * lax.rsqrt(jnp.sum(xf * xf, -1, keepdims=True) + EPS)


def modulate(x, shift, scale):
    return layer_norm(x) * (1.0 + scale) + shift


def split_proj(p):
    cuts, acc = [], 0
    for s in PROJ_SIZES[:-1]:
        acc += s
        cuts.append(acc)
    return jnp.split(p, cuts, axis=-1)


def centred_dwconv(x, w):
    return lax.conv_general_dilated(
        x, w[:, None, :].astype(x.dtype), window_strides=(1,),
        padding=[(CONV_K // 2, CONV_K // 2)], dimension_numbers=('NWC', 'WIO', 'NWC'),
        feature_group_count=x.shape[-1])


def gdn_prep(qkv, a, b, conv_w, a_log, dt_bias):
    bsz, n = qkv.shape[:2]
    qkv = jax.nn.silu(centred_dwconv(qkv, conv_w))
    q, k, v = jnp.split(qkv, [GDN_QK_W, 2 * GDN_QK_W], axis=-1)
    q = l2_normalize(q.reshape(bsz, n, GDN_HEADS, GDN_DK)) * (GDN_DK ** -0.5)
    k = l2_normalize(k.reshape(bsz, n, GDN_HEADS, GDN_DK))
    v = v.reshape(bsz, n, GDN_HEADS, GDN_DV)
    a = a.astype(jnp.float32).reshape(bsz, n, 2, GDN_HEADS)
    b = b.astype(jnp.float32).reshape(bsz, n, 2, GDN_HEADS)
    g = -jnp.exp(a_log.astype(jnp.float32)) * jax.nn.softplus(a + dt_bias.astype(jnp.float32))
    beta = jax.nn.sigmoid(b)
    return q, k, v, g, beta


def unit_lower_inverse(l):
    eye = jnp.eye(CHUNK, dtype=l.dtype)
    p = -l
    inv = eye + p
    for _ in range(CHUNK.bit_length() - 2):
        p = p @ p
        inv = inv @ (eye + p)
    return inv


def delta_rule_chunked(q, k, v, g, beta, s0):
    bsz, n, h = q.shape[:3]
    nc = n // CHUNK
    f32 = jnp.float32

    def to_chunks(t):
        t = t.astype(f32).reshape((bsz, nc, CHUNK, h) + t.shape[3:])
        return t.transpose((1, 0, 3, 2) + tuple(range(4, t.ndim)))

    qc, kc, vc, gc, bc = (to_chunks(t) for t in (q, k, v, g, beta))
    gcum = jnp.cumsum(gc, axis=-1)
    glast = gcum[..., -1]
    incl = jnp.tril(jnp.ones((CHUNK, CHUNK), bool))
    strict = jnp.tril(jnp.ones((CHUNK, CHUNK), bool), -1)
    decay = jnp.exp(jnp.where(incl, gcum[..., :, None] - gcum[..., None, :], -jnp.inf))
    kbeta = kc * bc[..., None]
    lmat = jnp.where(strict, jnp.einsum('nbhik,nbhjk->nbhij', kbeta, kc) * decay, 0.0)
    t_inv = unit_lower_inverse(lmat)
    u = jnp.einsum('nbhij,nbhjv->nbhiv', t_inv, vc * bc[..., None])
    w = jnp.einsum('nbhij,nbhjk->nbhik', t_inv, kbeta * jnp.exp(gcum)[..., None])
    a_intra = jnp.einsum('nbhik,nbhjk->nbhij', qc, kc) * decay
    q_dec = qc * jnp.exp(gcum)[..., None]
    k_dec = kc * jnp.exp(glast[..., None] - gcum)[..., None]

    def step(state, xs):
        q_i, k_i, u_i, w_i, a_i, gl_i = xs
        v_new = u_i - jnp.einsum('bhck,bhkv->bhcv', w_i, state)
        o_i = jnp.einsum('bhck,bhkv->bhcv', q_i, state) + jnp.einsum('bhij,bhjv->bhiv', a_i, v_new)
        state = state * jnp.exp(gl_i)[..., None, None] + jnp.einsum('bhck,bhcv->bhkv', k_i, v_new)
        return state, o_i

    s_final, o = lax.scan(step, s0.astype(f32), (q_dec, k_dec, u, w, a_intra, glast))
    o = o.transpose(1, 0, 3, 2, 4).reshape(bsz, n, h, -1)
    return o, s_final


def gdn_direction(inputs, d, s0, reverse):
    q, k, v, g, beta = inputs
    g, beta = g[:, :, d], beta[:, :, d]
    if reverse:
        q, k, v, g, beta = (jnp.flip(t, axis=1) for t in (q, k, v, g, beta))
    o, s = delta_rule_chunked(q, k, v, g, beta, s0)
    if reverse:
        o = jnp.flip(o, axis=1)
    return o, s


def gdn_output(o, z, norm_g):
    bsz, n = z.shape[:2]
    y = rms_norm(o, norm_g) * jax.nn.silu(z.astype(jnp.float32).reshape(bsz, n, GDN_HEADS, GDN_DV))
    return y.reshape(bsz, n, GDN_V_W).astype(z.dtype)


def axial_rope(x, cos, sin):
    shp = x.shape
    xr = x.reshape(shp[:-1] + (2, 2, ROPE_DIM // 4))
    x1, x2 = xr[..., 0, :], xr[..., 1, :]
    out = jnp.stack([x1 * cos - x2 * sin, x2 * cos + x1 * sin], axis=-2)
    return out.reshape(shp)


def mla_prep(dq, dkv, kr, q_norm, kv_norm, w_uq, w_ukv, cos, sin):
    bsz, n = dq.shape[:2]
    q = (rms_norm(dq, q_norm) @ w_uq).reshape(bsz, n, MLA_HEADS, NOPE_DIM + ROPE_DIM)
    kv = (rms_norm(dkv, kv_norm) @ w_ukv).reshape(bsz, n, MLA_HEADS, NOPE_DIM + V_DIM)
    q_nope, q_rope = q[..., :NOPE_DIM], q[..., NOPE_DIM:]
    k_nope, v = kv[..., :NOPE_DIM], kv[..., NOPE_DIM:]
    k_rope = kr
    if cos is not None:
        q_rope = axial_rope(q_rope, cos[:, None], sin[:, None])
        k_rope = axial_rope(k_rope, cos, sin)
    return q_nope, q_rope, k_nope, k_rope, v


def mla_attend(q_nope, q_rope, k_nope, k_rope, v):
    s = (jnp.einsum('bqhd,bkhd->bhqk', q_nope, k_nope, preferred_element_type=jnp.float32)
         + jnp.einsum('bqhr,bkr->bhqk', q_rope, k_rope, preferred_element_type=jnp.float32)) * MLA_SCALE
    p = jax.nn.softmax(s, axis=-1).astype(v.dtype)
    return jnp.einsum('bhqk,bkhd->bqhd', p, v)


def mla_latent(q_nope, q_rope, k_nope, k_rope, v):
    bsz, n = q_nope.shape[:2]

    def blocks(t):
        return jnp.moveaxis(t.reshape((bsz, n // Q_BLOCK, Q_BLOCK) + t.shape[2:]), 1, 0)

    o = lax.map(lambda qb: mla_attend(qb[0], qb[1], k_nope, k_rope, v), (blocks(q_nope), blocks(q_rope)))
    return jnp.moveaxis(o, 0, 1).reshape(bsz, n, MLA_HEADS * V_DIM)


def merge_out(y_gdn, y_mla, gate_a, gate_b, w_br_a, w_br_b, w_out):
    y = jax.nn.sigmoid(gate_a) * (y_gdn @ w_br_a) + jax.nn.sigmoid(gate_b) * (y_mla @ w_br_b)
    return y @ w_out


def swiglu(h, w1, w3, w2):
    return (jax.nn.silu(h @ w1) * (h @ w3)) @ w2


def moe_swiglu(h, router, w1, w3, w2):
    shp = h.shape
    t = h.reshape(-1, shp[-1])
    logits = jnp.dot(t, router, preferred_element_type=jnp.float32)
    top_v, top_i = lax.top_k(logits, TOP_K)
    top_w = jax.nn.softmax(top_v, axis=-1)
    combine = jnp.sum(jax.nn.one_hot(top_i, N_EXPERTS, dtype=jnp.float32) * top_w[..., None], axis=1)
    y = jnp.zeros_like(t)
    for e in range(N_EXPERTS):
        y = y + combine[:, e:e + 1].astype(t.dtype) * swiglu(t, w1[e], w3[e], w2[e])
    return y.reshape(shp)


def setup_inputs(seed: int = 0) -> dict:
    key = jax.random.key(seed)
    ks = iter(jax.random.split(key, 40))
    f32 = jnp.float32
    D = D_MODEL

    def nrm(shape, fan_in, gain=1.0):
        return jax.random.normal(next(ks), shape, f32) * (gain * fan_in ** -0.5)

    def gain(shape):
        return 1.0 + 0.02 * jax.random.normal(next(ks), shape, f32)

    def small(shape):
        return 0.02 * jax.random.normal(next(ks), shape, f32)

    x = jax.random.normal(next(ks), (BATCH, SEQ, D), f32)
    c = jax.random.normal(next(ks), (BATCH, D), f32)
    ctx = jax.random.normal(next(ks), (BATCH, CTX_LEN, D), f32)
    c_ctx = jax.random.normal(next(ks), (D,), f32)
    w_mod = nrm((DEPTH, D, 6 * D), D, 0.5)
    b_mod = small((DEPTH, 6 * D))
    w_in = nrm((DEPTH, D, PROJ_W), D)
    conv_w = nrm((DEPTH, CONV_K, QKV_W), CONV_K)
    a_log = jnp.log(jax.random.uniform(next(ks), (DEPTH, 2, GDN_HEADS), f32, 1.0, 16.0))
    dt = jnp.exp(jax.random.uniform(next(ks), (DEPTH, 2, GDN_HEADS), f32, math.log(1e-3), math.log(1e-1)))
    dt_bias = dt + jnp.log(-jnp.expm1(-dt))
    gdn_norm = gain((DEPTH, GDN_DV))
    q_norm = gain((DEPTH, Q_LORA))
    kv_norm = gain((DEPTH, KV_LORA))
    w_uq = nrm((DEPTH, Q_LORA, MLA_HEADS * (NOPE_DIM + ROPE_DIM)), Q_LORA)
    w_ukv = nrm((DEPTH, KV_LORA, MLA_HEADS * (NOPE_DIM + V_DIM)), KV_LORA)
    w_br_a = nrm((DEPTH, GDN_V_W, D), GDN_V_W)
    w_br_b = nrm((DEPTH, MLA_HEADS * V_DIM, D), MLA_HEADS * V_DIM)
    w_out = nrm((DEPTH, D, D), D, DEEPNORM_BETA)
    ln1_g = gain((DEPTH, D))
    ln1_b = small((DEPTH, D))
    ln2_g = gain((DEPTH, D))
    ln2_b = small((DEPTH, D))
    ffn_w1 = nrm((N_DENSE, D, FF_DENSE), D)
    ffn_w3 = nrm((N_DENSE, D, FF_DENSE), D)
    ffn_w2 = nrm((N_DENSE, FF_DENSE, D), FF_DENSE, DEEPNORM_BETA)
    moe_router = nrm((N_MOE, D, N_EXPERTS), D)
    moe_w1 = nrm((N_MOE, N_EXPERTS, D, FF_EXPERT), D)
    moe_w3 = nrm((N_MOE, N_EXPERTS, D, FF_EXPERT), D)
    moe_w2 = nrm((N_MOE, N_EXPERTS, FF_EXPERT, D), FF_EXPERT, DEEPNORM_BETA)
    return {'x': x, 'c': c, 'ctx': ctx, 'c_ctx': c_ctx, 'w_mod': w_mod, 'b_mod': b_mod, 'w_in': w_in,
            'conv_w': conv_w, 'a_log': a_log, 'dt_bias': dt_bias, 'gdn_norm': gdn_norm, 'q_norm': q_norm,
            'kv_norm': kv_norm, 'w_uq': w_uq, 'w_ukv': w_ukv, 'w_br_a': w_br_a, 'w_br_b': w_br_b,
            'w_out': w_out, 'ln1_g': ln1_g, 'ln1_b': ln1_b, 'ln2_g': ln2_g, 'ln2_b': ln2_b,
            'ffn_w1': ffn_w1, 'ffn_w3': ffn_w3, 'ffn_w2': ffn_w2, 'moe_router': moe_router,
            'moe_w1': moe_w1, 'moe_w3': moe_w3, 'moe_w2': moe_w2}


def reference(x, c, ctx, c_ctx, w_mod, b_mod, w_in, conv_w, a_log, dt_bias, gdn_norm, q_norm, kv_norm,
              w_uq, w_ukv, w_br_a, w_br_b, w_out, ln1_g, ln1_b, ln2_g, ln2_b, ffn_w1, ffn_w3, ffn_w2,
              moe_router, moe_w1, moe_w3, moe_w2):
    bsz, n_lat, _ = x.shape
    rows = n_lat // GRID_W
    f32 = jnp.float32
    row = jnp.repeat(jnp.arange(rows), GRID_W).astype(f32)
    col = jnp.tile(jnp.arange(GRID_W), rows).astype(f32)
    n_freq = ROPE_DIM // 4
    inv_freq = ROPE_THETA ** (-jnp.arange(n_freq, dtype=f32) / n_freq)
    ang = jnp.stack([row[:, None] * inv_freq, col[:, None] * inv_freq], axis=1)
    cos, sin = jnp.cos(ang).astype(x.dtype), jnp.sin(ang).astype(x.dtype)
    s0 = jnp.zeros((bsz, GDN_HEADS, GDN_DK, GDN_DV), f32)

    for i in range(DEPTH):
        last = i == DEPTH - 1
        sh1, sc1, gt1, sh2, sc2, gt2 = jnp.split((jax.nn.silu(c) @ w_mod[i] + b_mod[i])[:, None, :], 6, axis=-1)
        csh1, csc1, cgt1, csh2, csc2, cgt2 = jnp.split(jax.nn.silu(c_ctx) @ w_mod[i] + b_mod[i], 6, axis=-1)

        pl = split_proj(modulate(x, sh1, sc1) @ w_in[i])
        pc = split_proj(modulate(ctx, csh1, csc1) @ w_in[i])

        gin_c = gdn_prep(pc[0], pc[2], pc[3], conv_w[i], a_log[i], dt_bias[i])
        gin_l = gdn_prep(pl[0], pl[2], pl[3], conv_w[i], a_log[i], dt_bias[i])
        oc_f, st_f = gdn_direction(gin_c, 0, s0, False)
        oc_b, st_b = gdn_direction(gin_c, 1, s0, True)
        ol_f, _ = gdn_direction(gin_l, 0, st_f, False)
        ol_b, _ = gdn_direction(gin_l, 1, st_b, True)
        ya_lat = gdn_output(ol_f + ol_b, pl[1], gdn_norm[i])

        ml = mla_prep(pl[4], pl[5], pl[6], q_norm[i], kv_norm[i], w_uq[i], w_ukv[i], cos, sin)
        mc = mla_prep(pc[4], pc[5], pc[6], q_norm[i], kv_norm[i], w_uq[i], w_ukv[i], None, None)
        k_nope = jnp.concatenate([ml[2], mc[2]], axis=1)
        k_rope = jnp.concatenate([ml[3], mc[3]], axis=1)
        v_all = jnp.concatenate([ml[4], mc[4]], axis=1)
        yb_lat = mla_latent(ml[0], ml[1], k_nope, k_rope, v_all)

        m_lat = merge_out(ya_lat, yb_lat, pl[7], pl[8], w_br_a[i], w_br_b[i], w_out[i])
        x_new = layer_norm(DEEPNORM_ALPHA * x + gt1 * m_lat, ln1_g[i], ln1_b[i])
        if not last:
            ya_ctx = gdn_output(oc_f + oc_b, pc[1], gdn_norm[i])
            yb_ctx = mla_attend(mc[0], mc[1], mc[2], mc[3], mc[4]).reshape(bsz, -1, MLA_HEADS * V_DIM)
            m_ctx = merge_out(ya_ctx, yb_ctx, pc[7], pc[8], w_br_a[i], w_br_b[i], w_out[i])
            ctx = layer_norm(DEEPNORM_ALPHA * ctx + cgt1 * m_ctx, ln1_g[i], ln1_b[i])
        x = x_new

        j = i // 2
        if i % 2 == 0:
            ffn = lambda h: swiglu(h, ffn_w1[j], ffn_w3[j], ffn_w2[j])
        else:
            ffn = lambda h: moe_swiglu(h, moe_router[j], moe_w1[j], moe_w3[j], moe_w2[j])
        x = layer_norm(DEEPNORM_ALPHA * x + gt2 * ffn(modulate(x, sh2, sc2)), ln2_g[i], ln2_b[i])
        if not last:
            ctx = layer_norm(DEEPNORM_ALPHA * ctx + cgt2 * ffn(modulate(ctx, csh2, csc2)), ln2_g[i], ln2_b[i])
    return x
```

```python
import numpy as np
import concourse.bass as bass
import concourse.mybir as mybir
from concourse.bass_utils import run_bass_kernel_spmd
from contextlib import ExitStack

F32 = mybir.dt.float32
BF16 = mybir.dt.bfloat16
AF = mybir.ActivationFunctionType
ALU = mybir.AluOpType
AX = mybir.AxisListType

ENGS = ("pe", "act", "dve", "pool", "sp")
EPOCH = 12000
NDS = 12
DEPOCH = 1500

D = 2048
NT = 2304
NTILE = 18
CTXT = 2
H = 16
PROJ_W = 13440
FF_DENSE = 5632
FF_EXP = 7168
NEXP = 8
ALPHA = 4 ** 0.25
EPS = 1e-6
MLA_SCALE = 192 ** -0.5
NEG = -1.0e6


class Res:
    __slots__ = ("w", "rd", "name", "excl")

    def __init__(self, name="", excl=False):
        self.w = None
        self.rd = {}
        self.name = name
        self.excl = excl


class Prog:
    def __init__(self, nc, same_engine_sync=("act", "dve", "pool")):
        self.nc = nc
        self.ops = {e: [] for e in ENGS}
        self.cnt = {e: 0 for e in ENGS}
        self.dcnt = {e: 0 for e in ENGS}
        self.know = {e: {} for e in ENGS}
        self.last = {}
        self.semkeys = {}
        self.same = set(same_engine_sync)

    def _mkwaits(self, eng, deps):
        waits = {}
        kn = self.know[eng]
        for (sk, v, e) in deps:
            if e == eng and sk[0] == "c" and eng not in self.same:
                continue
            if kn.get(sk, 0) >= v:
                continue
            kn[sk] = v
            if waits.get(sk, 0) < v:
                waits[sk] = v
        return list(waits.items())

    def _deps(self, eng, reads, writes):
        deps = []
        for r in reads:
            if r.w is not None:
                deps.append(r.w)
            if r.excl:
                for sk, (v, e) in r.rd.items():
                    if e != eng:
                        deps.append((sk, v, e))
        for r in writes:
            if r.w is not None:
                deps.append(r.w)
            for sk, (v, e) in r.rd.items():
                deps.append((sk, v, e))
        return self._mkwaits(eng, deps)

    def _record(self, ident, reads, writes):
        sk, v, e = ident
        self.last[sk] = (v, e)
        for r in writes:
            r.w = ident
            r.rd = {}
        for r in reads:
            cur = r.rd.get(sk)
            if cur is None or cur[0] < v:
                r.rd[sk] = (v, e)

    def op(self, eng, fn, reads=(), writes=()):
        waits = self._deps(eng, reads, writes)
        k = self.cnt[eng]
        self.cnt[eng] = k + 1
        sk = ("c", eng, k // EPOCH)
        v = k % EPOCH + 1
        self.ops[eng].append((waits, fn, (sk, v)))
        self._record((sk, v, eng), reads, writes)

    def dma(self, q, out, in_, reads=(), writes=(), **kw):
        waits = self._deps(q, reads, writes)
        j = self.dcnt[q]
        self.dcnt[q] = j + 1
        slot = j % NDS
        use = j // NDS
        sk = ("d", q, slot, use // DEPOCH)
        v = 16 * (use % DEPOCH + 1)
        if use % DEPOCH > 0:
            pv = v - 16
            kn = self.know[q]
            if kn.get(sk, 0) < pv:
                kn[sk] = pv
                waits.append((sk, pv))

        def fn(eng, out=out, in_=in_, kw=kw):
            return eng.dma_start(out=out, in_=in_, **kw)
        self.ops[q].append((waits, fn, (sk, v)))
        self._record((sk, v, q), reads, writes)

    def barrier(self):
        deps = [(sk, v, e) for sk, (v, e) in self.last.items()]
        for eng in ENGS:
            kn = self.know[eng]
            waits = {}
            for (sk, v, e) in deps:
                if kn.get(sk, 0) >= v:
                    continue
                kn[sk] = v
                waits[sk] = v
            if waits:
                self.ops[eng].append((list(waits.items()), None, None))

    def emit(self):
        nc = self.nc
        self.barrier()
        allkeys = set()
        for e in ENGS:
            for waits, fn, inc in self.ops[e]:
                if inc is not None:
                    allkeys.add(inc[0])
        allkeys = sorted(allkeys)
        with ExitStack() as st:
            for i, sk in enumerate(allkeys):
                self.semkeys[sk] = st.enter_context(nc.semaphore("s%d" % i))
            block = st.enter_context(nc.Block())
            sem = self.semkeys

            def run(engname, eng):
                for waits, fn, inc in self.ops[engname]:
                    for sk, v in waits:
                        eng.wait_ge(sem[sk], v)
                    if fn is None:
                        continue
                    ins = fn(eng)
                    ins.then_inc(sem[inc[0]], 16 if inc[0][0] == "d" else 1)

            @block.tensor
            def _(e):
                run("pe", e)

            @block.scalar
            def _(e):
                run("act", e)

            @block.vector
            def _(e):
                run("dve", e)

            @block.gpsimd
            def _(e):
                run("pool", e)

            @block.sync
            def _(e):
                run("sp", e)
        return len(allkeys)


class Arena:
    def __init__(self, ap, nwords):
        self.ap = ap
        self.n = nwords
        self.off = 0
        self.marks = []

    def alloc(self, free_elems, dtype):
        words = (free_elems * (2 if dtype == BF16 else 4) + 3) // 4
        words = (words + 7) // 8 * 8
        assert self.off + words <= self.n, ("arena overflow", self.off, words, self.n)
        a = self.ap[:, self.off:self.off + words]
        self.off += words
        if dtype == BF16:
            a = a.bitcast(BF16)
        return a[:, 0:free_elems]

    def mark(self):
        self.marks.append(self.off)

    def release(self):
        self.off = self.marks.pop()


class Ring:
    def __init__(self, arena, n, free_elems, dtype):
        self.bufs = [(arena.alloc(free_elems, dtype), Res()) for _ in range(n)]
        self.i = 0

    def next(self):
        b = self.bufs[self.i % len(self.bufs)]
        self.i += 1
        return b


def host_consts():
    c = {}
    c["identf"] = np.eye(128, dtype=np.float32)
    idx = np.arange(128)
    same = (idx[:, None] // 64) == (idx[None, :] // 64)
    masks = np.zeros((12, 128, 128), np.float32)
    for d in range(2):
        aft = (idx[:, None] >= idx[None, :]) if d == 0 else (idx[:, None] <= idx[None, :])
        aft_s = (idx[:, None] > idx[None, :]) if d == 0 else (idx[:, None] < idx[None, :])
        masks[0 + d] = np.where((aft & same).T, 0.0, NEG)
        masks[2 + d] = np.where((aft_s & same), 0.0, NEG)
        masks[4 + d] = np.where((aft & same).T, 1.0, 0.0)
    masks[6] = np.where(idx[:, None] < 64, 1.0, 0.0) * np.ones((1, 128))
    masks[7] = np.where(idx[:, None] >= 64, 1.0, 0.0) * np.ones((1, 128))
    masks[8] = 1.0
    c["masks"] = masks.astype(np.float32)
    rows = 2048 // 64
    row = np.repeat(np.arange(rows), 64).astype(np.float32)
    col = np.tile(np.arange(64), rows).astype(np.float32)
    inv_freq = (10000.0 ** (-np.arange(16, dtype=np.float32) / 16)).astype(np.float32)
    cosT = np.zeros((64, 2048), np.float32)
    sinT = np.zeros((64, 2048), np.float32)
    for ax, pos in enumerate((row, col)):
        ang = (pos[None, :] * inv_freq[:, None]).astype(np.float32)
        for half in range(2):
            p0 = ax * 32 + half * 16
            cosT[p0:p0 + 16] = np.cos(ang)
            sinT[p0:p0 + 16] = np.sin(ang) * (-1.0 if half == 0 else 1.0)
    c["ropec"] = np.stack([cosT, sinT]).astype(np.float32)
    return c


def build(nc, nlayers=2, dbg=()):
    def din(name, shape):
        return nc.dram_tensor(name, list(shape), F32, kind="ExternalInput").ap()

    def dscr(name, shape, dt=F32):
        kind = "ExternalOutput" if name in dbg else "Internal"
        return nc.dram_tensor(name, list(shape), dt, kind=kind).ap()

    xs_in = din("xs", [NT, D])
    cc = din("cc", [128, 32])
    w_mod = din("w_mod", [2, D, 6 * D]); b_mod = din("b_mod", [2, 6 * D])
    w_in = din("w_in", [2, D, PROJ_W])
    convw = din("convw", [2, 128, 48 * 5])
    a_log = din("a_log", [2, 32]); dt_bias = din("dt_bias", [2, 32])
    gdn_norm = din("gdn_norm", [2, 128])
    qkn = din("qkn", [2, 128, 8])
    w_uq = din("w_uq", [2, 512, 3072]); w_ukv = din("w_ukv", [2, 512, 4096])
    w_br_a = din("w_br_a", [2, D, D]); w_br_b = din("w_br_b", [2, D, D]); w_out = din("w_out", [2, D, D])
    ln1_g = din("ln1_g", [2, D]); ln1_b = din("ln1_b", [2, D]); ln2_g = din("ln2_g", [2, D]); ln2_b = din("ln2_b", [2, D])
    ffn_w1 = din("ffn_w1", [1, D, FF_DENSE]); ffn_w3 = din("ffn_w3", [1, D, FF_DENSE]); ffn_w2 = din("ffn_w2", [1, FF_DENSE, D])
    router = din("router", [128, 16 * 8])
    moe_w1 = din("moe_w1", [1, NEXP, D, FF_EXP]); moe_w3 = din("moe_w3", [1, NEXP, D, FF_EXP]); moe_w2 = din("moe_w2", [1, NEXP, FF_EXP, D])
    identf_d = din("identf", [128, 128]); masks_d = din("masks", [12, 128, 128]); ropec_d = din("ropec", [2, 64, 2048])
    out_d = nc.dram_tensor("out", [2048, D], F32, kind="ExternalOutput").ap()

    XS = dscr("XS", [NT, D])
    MODV = dscr("MODV", [2, 2, 6 * D])
    QT = dscr("QT", [H, 128, NT], BF16); KT = dscr("KT", [H, 128, NT], BF16); KT32 = dscr("KT32", [H, 128, NT])
    KM = dscr("KM", [NT, H, 128], BF16); VM = dscr("VM", [NT, H, 128], BF16)
    ZS = dscr("ZS", [NT, D])
    GT = dscr("GT", [32, 128, NT], BF16)
    QNT = dscr("QNT", [H, 128, NT], BF16); QRT = dscr("QRT", [H, 64, NT], BF16)
    KNT = dscr("KNT", [H, 128, NT], BF16); VM2 = dscr("VM2", [H, NT, 128], BF16)
    OFB = dscr("OFB", [2, NT, H * 128])
    YAT = dscr("YAT", [H, 128, NT], BF16); YBT = dscr("YBT", [H, 128, NT], BF16)
    ATS = dscr("ATS", [NEXP, 56, 128, NT], BF16)
    FO = dscr("FO", [NT, D])

    NW = 47000
    with ExitStack() as st:
        arena_t = st.enter_context(nc.sbuf_tensor("arena", [128, NW], F32))
        ps_all = st.enter_context(nc.psum_tensor("psall", [128, 4096], F32))
        A = Arena(arena_t[:], NW)
        P = Prog(nc)
        ps = [ps_all[:, b * 512:(b + 1) * 512] for b in range(8)]
        psb = [ps_all[:, b * 512:(b + 1) * 512].bitcast(BF16) for b in range(8)]
        rps = [Res("ps%d" % b, excl=True) for b in range(8)]

        identf = A.alloc(128, F32); identb = A.alloc(128, BF16); r_const = Res("const")
        masks = [A.alloc(128, F32) for _ in range(9)]
        onecol = A.alloc(1, F32); epscol = A.alloc(1, F32)
        P.dma("sp", identf, identf_d, writes=[r_const])
        for i in range(9):
            P.dma("sp", masks[i], masks_d[i], writes=[r_const])
        P.op("dve", lambda e: e.tensor_copy(identb, identf), reads=[r_const], writes=[r_const])
        P.op("dve", lambda e: e.memset(onecol, 1.0), writes=[r_const])
        P.op("dve", lambda e: e.memset(epscol, EPS), writes=[r_const])
        NEGM_AT, NEGM_P, MCUM, MCH, ONESF = masks[0:2], masks[2:4], masks[4:6], masks[6:8], masks[8]
        g_all = A.alloc(NTILE * 32, F32).rearrange("p (t c) -> p t c", c=32); r_gall = Res("gall")
        lnb_all = A.alloc(NTILE * 32, F32).rearrange("p (t c) -> p t c", c=32)
        KRT = A.alloc(NT, BF16); r_krt = Res("krt")
        comb = A.alloc(16 * 8, F32).rearrange("p (t e) -> p t e", e=8); r_comb = Res("comb")
        P.barrier()

        def release():
            A.release()
            P.barrier()

        def mm(out, lhsT, rhs, start, stop, reads, writes):
            P.op("pe", lambda e: e.matmul(out, lhsT, rhs, start=start, stop=stop), reads=reads, writes=writes)

        def tr(out, in_, reads, writes, f32=False):
            idn = identf if f32 else identb
            P.op("pe", lambda e: e.transpose(out, in_, idn), reads=list(reads) + [r_const], writes=writes)

        def act(out, in_, func, reads, writes, **kw):
            P.op("act", lambda e: e.activation(out, in_, func, **kw), reads=reads, writes=writes)

        def tt(eng, out, in0, in1, op, reads, writes):
            P.op(eng, lambda e: e.tensor_tensor(out, in0, in1, op), reads=reads, writes=writes)

        def ts(eng, out, in0, s1, s2, op0, op1, reads, writes):
            if op1 is None:
                P.op(eng, lambda e: e.tensor_scalar(out, in0, s1, None, op0), reads=reads, writes=writes)
            else:
                P.op(eng, lambda e: e.tensor_scalar(out, in0, s1, s2, op0, op1), reads=reads, writes=writes)

        def stt(eng, out, in0, scalar, in1, op0, op1, reads, writes):
            P.op(eng, lambda e: e.scalar_tensor_tensor(out, in0, scalar, in1, op0, op1), reads=reads, writes=writes)

        def wcast(dst3, src2, rw):
            P.dma("pool", dst3, src2.rearrange("(k p) n -> p k n", p=128), writes=[rw])

        TOKG = [(0, 256), (256, 512), (768, 512), (1280, 512), (1792, 512)]

        def phase_init():
            A.mark()
            for t in range(NTILE):
                P.dma("sp", XS[t * 128:(t + 1) * 128, :], xs_in[t * 128:(t + 1) * 128, :])
            cct = A.alloc(32, F32); r_cc = Res()
            P.dma("sp", cct, cc, writes=[r_cc])
            act(cct, cct, AF.Silu, [r_cc], [r_cc])
            cv = cct.rearrange("p (k t) -> p k t", t=2)
            wr = Ring(A, 2, 16 * 512, F32)
            br = Ring(A, 2, 512, F32)
            orr = Ring(A, 2, 512, F32)
            i = 0
            for l in range(2):
                for nb in range(24):
                    wt, rw = wr.next()
                    wt3 = wt.rearrange("p (k n) -> p k n", k=16)
                    P.dma("sp", wt3, w_mod[l, :, nb * 512:(nb + 1) * 512].rearrange("(k p) n -> p k n", p=128), writes=[rw])
                    bt, rb = br.next()
                    P.dma("sp", bt[0:2, :], b_mod[l, nb * 512:(nb + 1) * 512].partition_broadcast(2), writes=[rb])
                    bank = i % 2
                    i += 1
                    for k in range(16):
                        mm(ps[bank][0:2, :], cv[:, k, :], wt3[:, k, :], k == 0, k == 15, [r_cc, rw], [rps[bank]])
                    ot, ro = orr.next()
                    tt("dve", ot[0:2, :], ps[bank][0:2, :], bt[0:2, :], ALU.add, [rps[bank], rb], [ro])
                    if nb // 4 in (1, 4):
                        ts("dve", ot[0:2, :], ot[0:2, :], 1.0, None, ALU.add, None, [ro], [ro])
                    P.dma("sp", MODV[l, :, nb * 512:(nb + 1) * 512], ot[0:2, :], reads=[ro])
            release()

        def ln_mod_T(l, which, tiles, hT, r_hT, col0, extra=None):
            shi, sci = (0, 1) if which == 0 else (3, 4)
            bA, bB = [], []
            r_b = Res()
            for r in (0, 1):
                a = A.alloc(2048, F32); b = A.alloc(2048, F32)
                P.dma("sp", a, MODV[l, r, sci * 2048:(sci + 1) * 2048].partition_broadcast(128), writes=[r_b])
                P.dma("sp", b, MODV[l, r, shi * 2048:(shi + 1) * 2048].partition_broadcast(128), writes=[r_b])
                bA.append(a); bB.append(b)
            xr = Ring(A, 2, 2048, F32)
            hbr = Ring(A, 2, 2048, BF16)
            sr = Ring(A, 2, 32, F32)
            for t in tiles:
                r = 1 if t < CTXT else 0
                xt, rx = xr.next()
                P.dma("sp", xt, XS[t * 128:(t + 1) * 128, :], writes=[rx])
                s, rs = sr.next()
                st6 = s[:, 0:24].rearrange("p (c s) -> p c s", s=6)
                xv = xt.rearrange("p (c f) -> p c f", f=512)
                for c in range(4):
                    P.op("dve", lambda e, c=c, st6=st6, xv=xv: e.bn_stats(st6[:, c, :], xv[:, c, :]), reads=[rx], writes=[rs])
                P.op("dve", lambda e, s=s, st6=st6: e.bn_aggr(s[:, 24:26], st6), reads=[rs], writes=[rs])
                ts("dve", s[:, 26:27], s[:, 25:26], EPS, None, ALU.add, None, [rs], [rs])
                act(s[:, 26:27], s[:, 26:27], AF.Sqrt, [rs], [rs])
                P.op("dve", lambda e, s=s: e.reciprocal(s[:, 26:27], s[:, 26:27]), reads=[rs], writes=[rs])
                ts("dve", xt, xt, s[:, 24:25], s[:, 26:27], ALU.subtract, ALU.mult, [rx, rs], [rx])
                tt("pool", xt, xt, bA[r], ALU.mult, [rx, r_b], [rx])
                tt("dve", xt, xt, bB[r], ALU.add, [rx, r_b], [rx])
                hb, rhb = hbr.next()
                act(hb, xt, AF.Copy, [rx], [rhb])
                if extra is not None:
                    extra(t, xt, rx)
                c = col0(t)
                for g in range(4):
                    bank = 6 + (g % 2)
                    for j in range(4):
                        k = g * 4 + j
                        tr(psb[bank][:, j * 128:(j + 1) * 128], hb[:, k * 128:(k + 1) * 128], [rhb], [rps[bank]])
                    P.op("dve" if g % 2 else "act",
                         (lambda e, g=g, bank=bank, c=c: e.tensor_copy(hT[:, g * 4:(g + 1) * 4, c:c + 128], psb[bank][:, 0:512].rearrange("p (j t) -> p j t", j=4))) if g % 2 else
                         (lambda e, g=g, bank=bank, c=c: e.activation(hT[:, g * 4:(g + 1) * 4, c:c + 128], psb[bank][:, 0:512].rearrange("p (j t) -> p j t", j=4), AF.Copy)),
                         reads=[rps[bank]], writes=[r_hT])

        def phase_proj(l):
            A.mark()
            DQK = [A.alloc(4 * NT, BF16).rearrange("p (k t) -> p k t", k=4) for _ in range(2)]; r_dqk = [Res(), Res()]
            qk = A.alloc(8, F32); r_qk = Res()
            P.dma("sp", qk, qkn[l], writes=[r_qk])
            RC = {}

            def load_rope():
                ropec = A.alloc(2 * 2048, F32); RC["r"] = Res()
                RC["COS"] = ropec[0:64, 0:2048]; RC["SINS"] = ropec[0:64, 2048:4096]
                P.dma("sp", RC["COS"], ropec_d[0], writes=[RC["r"]])
                P.dma("sp", RC["SINS"], ropec_d[1], writes=[RC["r"]])
                RC["t12"] = Ring(A, 2, 1024, F32)
            A.mark()
            hT = A.alloc(16 * NT, BF16).rearrange("p (k t) -> p k t", k=16); r_hT = Res("hT")
            A.mark()
            ln_mod_T(l, 0, range(NTILE), hT, r_hT, lambda t: t * 128)
            release()
            wfm = Ring(A, 2, 16 * 128, BF16)
            cw = A.alloc(240, F32); r_cw = Res()
            P.dma("sp", cw, convw[l], writes=[r_cw])
            cwv = cw.rearrange("p (c j) -> p c j", j=5)
            W = w_in[l]

            def lin_fm(wt3, rw, kc, m, tok0, ntok, bank, inT, r_in, m0=0, po=0):
                for k in range(kc):
                    mm(ps[bank][po:po + m, 0:ntok], wt3[:, k, m0:m0 + m], inT[:, k, tok0:tok0 + ntok], k == 0, k == kc - 1, [rw, r_in], [rps[bank]])

            A.mark()
            rawr = Ring(A, 1, 2312, F32)
            for rb, rr in rawr.bufs:
                P.op("dve", lambda e, rb=rb: e.memset(rb, 0.0), writes=[rr])
            accr = Ring(A, 1, NT, F32)
            sqr = Ring(A, 1, NT, F32)
            rnr = Ring(A, 2, 512, F32)
            fmr = Ring(A, 2, NT, BF16)
            tmr = Ring(A, 1, NT, BF16)
            bi = 0
            for ch in range(48):
                kind, h = ch // 16, ch % 16
                wt, rw = wfm.next()
                wt3 = wt.rearrange("p (k n) -> p k n", k=16)
                wcast(wt3, W[:, ch * 128:(ch + 1) * 128], rw)
                raw, rr = rawr.next()
                for gi, (t0, n) in enumerate(TOKG):
                    bank = bi % 4; bi += 1
                    lin_fm(wt3, rw, 16, 128, t0, n, bank, hT, r_hT)
                    dst = raw[:, 2:258] if gi == 0 else raw[:, 262 + (t0 - 256):262 + (t0 - 256) + n]
                    act(dst, ps[bank][:, 0:n], AF.Copy, [rps[bank]], [rr])
                acc, ra = accr.next()
                for (ro, n, oo) in ((2, 256, 0), (262, 2048, 256)):
                    ts("dve", acc[:, oo:oo + n], raw[:, ro - 2:ro - 2 + n], cwv[:, ch, 0:1], None, ALU.mult, None, [rr, r_cw], [ra])
                    for j in range(1, 5):
                        stt("dve", acc[:, oo:oo + n], raw[:, ro - 2 + j:ro - 2 + j + n], cwv[:, ch, j:j + 1], acc[:, oo:oo + n], ALU.mult, ALU.add, [rr, r_cw, ra], [ra])
                fm, rf = fmr.next()
                if kind == 2:
                    act(fm, acc, AF.Silu, [ra], [rf])
                else:
                    act(acc, acc, AF.Silu, [ra], [ra])
                    sq, rs = sqr.next()
                    act(sq, acc, AF.Square, [ra], [rs])
                    for (t0, n) in TOKG:
                        bank = bi % 4; bi += 1
                        mm(ps[bank][:, 0:n], ONESF, sq[:, t0:t0 + n], True, True, [rs, r_const], [rps[bank]])
                        rn, rrn = rnr.next()
                        act(rn[:, 0:n], ps[bank][:, 0:n], AF.Sqrt, [rps[bank], r_const], [rrn], bias=epscol)
                        P.op("dve", lambda e, rn=rn, n=n: e.reciprocal(rn[:, 0:n], rn[:, 0:n]), reads=[rrn], writes=[rrn])
                        if kind == 0:
                            stt("dve", fm[:, t0:t0 + n], acc[:, t0:t0 + n], 128 ** -0.5, rn[:, 0:n], ALU.mult, ALU.mult, [ra, rrn], [rf])
                        else:
                            tt("dve", sq[:, t0:t0 + n], acc[:, t0:t0 + n], rn[:, 0:n], ALU.mult, [ra, rrn, rs], [rs])
                            act(fm[:, t0:t0 + n], sq[:, t0:t0 + n], AF.Copy, [rs], [rf])
                    P.dma("sp", (QT if kind == 0 else KT)[h], fm, reads=[rf])
                    if kind == 1:
                        P.dma("sp", KT32[h], sq, reads=[rs])
                if kind >= 1:
                    tm, rt = tmr.next()
                    tm3 = tm.rearrange("p (t d) -> p t d", d=128)
                    for g in range(5):
                        bank = 6 + (g % 2)
                        nn = 4 if g < 4 else 2
                        for j in range(nn):
                            t = g * 4 + j
                            tr(psb[bank][:, j * 128:(j + 1) * 128], fm[:, t * 128:(t + 1) * 128], [rf], [rps[bank]])
                        src = psb[bank][:, 0:nn * 128].rearrange("p (j t) -> p j t", j=nn)
                        if g % 2:
                            P.op("dve", lambda e, tm3=tm3, g=g, nn=nn, src=src: e.tensor_copy(tm3[:, g * 4:g * 4 + nn, :], src), reads=[rps[bank]], writes=[rt])
                        else:
                            act(tm3[:, g * 4:g * 4 + nn, :], src, AF.Copy, [rps[bank]], [rt])
                    dst = (KM if kind == 1 else VM).rearrange("(t p) h d -> p t h d", p=128)[:, :, h, :]
                    P.dma("sp", dst, tm3, reads=[rt])
            release()

            A.mark()
            wtm = Ring(A, 2, 16 * 512, BF16)
            zr = Ring(A, 3, 512, F32)
            for nb in range(4):
                wt, rw = wtm.next()
                wt3 = wt.rearrange("p (k n) -> p k n", k=16)
                wcast(wt3, W[:, 6144 + nb * 512:6144 + (nb + 1) * 512], rw)
                for t in range(NTILE):
                    bank = bi % 4; bi += 1
                    for k in range(16):
                        mm(ps[bank], hT[:, k, t * 128:(t + 1) * 128], wt3[:, k, :], k == 0, k == 15, [rw, r_hT], [rps[bank]])
                    z, rz = zr.next()
                    act(z, ps[bank], AF.Silu, [rps[bank]], [rz])
                    P.dma("sp", ZS[t * 128:(t + 1) * 128, nb * 512:(nb + 1) * 512], z, reads=[rz])

            wt, rw = wtm.next()
            wab = wt[:, 0:16 * 64].rearrange("p (k n) -> p k n", k=16)
            wcast(wab, W[:, 8192:8256], rw)
            negA = A.alloc(32, F32); dtb = A.alloc(32, F32); r_ab = Res()
            P.dma("sp", negA, a_log[l].partition_broadcast(128), writes=[r_ab])
            P.dma("sp", dtb, dt_bias[l].partition_broadcast(128), writes=[r_ab])
            act(negA, negA, AF.Exp, [r_ab], [r_ab])
            ts("dve", negA, negA, -1.0, None, ALU.mult, None, [r_ab], [r_ab])
            tmpr = Ring(A, 2, 64, F32)
            for t in range(NTILE):
                bank = bi % 4; bi += 1
                for k in range(16):
                    mm(ps[bank][:, 0:64], hT[:, k, t * 128:(t + 1) * 128], wab[:, k, :], k == 0, k == 15, [rw, r_hT], [rps[bank]])
                tp, rtp = tmpr.next()
                tt("dve", tp[:, 0:32], ps[bank][:, 0:32], dtb, ALU.add, [rps[bank], r_ab], [rtp])
                act(tp[:, 0:32], tp[:, 0:32], AF.Exp, [rtp], [rtp])
                act(tp[:, 32:64], ps[bank][:, 32:64], AF.Exp, [rps[bank], rtp], [rtp], scale=-1.0)
                act(tp, tp, AF.Ln, [rtp, r_const], [rtp], bias=onecol)
                tt("dve", g_all[:, t, :], tp[:, 0:32], negA, ALU.mult, [rtp, r_ab], [r_gall])
                ts("dve", lnb_all[:, t, :], tp[:, 32:64], -1.0, None, ALU.mult, None, [rtp], [r_gall])

            xnr = Ring(A, 2, 512, BF16)
            ssr = Ring(A, 2, 8, F32)
            junk = A.alloc(512, F32); r_junk = Res()
            for which in range(2):
                wt, rw = wtm.next()
                wt3 = wt.rearrange("p (k n) -> p k n", k=16)
                wcast(wt3, W[:, 8256 + which * 512:8256 + (which + 1) * 512], rw)
                for t in range(NTILE):
                    bank = bi % 4; bi += 1
                    for k in range(16):
                        mm(ps[bank], hT[:, k, t * 128:(t + 1) * 128], wt3[:, k, :], k == 0, k == 15, [rw, r_hT], [rps[bank]])
                    s, rs = ssr.next()
                    act(junk, ps[bank], AF.Square, [rps[bank]], [r_junk, rs], accum_out=s[:, 0:1])
                    ts("dve", s[:, 1:2], s[:, 0:1], 1.0 / 512, EPS, ALU.mult, ALU.add, [rs], [rs])
                    act(s[:, 1:2], s[:, 1:2], AF.Sqrt, [rs], [rs])
                    P.op("dve", lambda e, s=s: e.reciprocal(s[:, 1:2], s[:, 1:2]), reads=[rs], writes=[rs])
                    xn, rxn = xnr.next()
                    act(xn, ps[bank], AF.Copy, [rps[bank], rs], [rxn], scale=s[:, 1:2])
                    tb = 6 + (t % 2)
                    for j in range(4):
                        tr(psb[tb][:, j * 128:(j + 1) * 128], xn[:, j * 128:(j + 1) * 128], [rxn], [rps[tb]])
                    for j in range(4):
                        ts("dve", DQK[which][:, j, t * 128:(t + 1) * 128], psb[tb][:, j * 128:(j + 1) * 128], qk[:, which * 4 + j:which * 4 + j + 1], None, ALU.mult, None, [rps[tb], r_qk], [r_dqk[which]])

            release()
            load_rope()
            PERM = [(0, 16), (16, 0), (32, 48), (48, 32)]
            wt, rw = wfm.next()
            wkr = wt.rearrange("p (k n) -> p k n", k=16)
            P.dma("pool", wkr[:, :, 0:64], W[:, 9280:9344].rearrange("(k p) n -> p k n", p=128), writes=[rw])
            for (dc, sc) in PERM:
                P.dma("pool", wkr[:, :, 64 + dc:64 + dc + 16], W[:, 9280 + sc:9280 + sc + 16].rearrange("(k p) n -> p k n", p=128), writes=[rw])

            def rope_evac(bank_a, bank_b, n, t0, dst, r_dst, scale):
                if t0 == 0:
                    act(dst[0:64, t0:t0 + n], ps[bank_a][0:64, 0:n], AF.Copy, [rps[bank_a]], [r_dst], scale=scale)
                    return
                tb, rtb = RC["t12"].next()
                COS, SINS, r_rope = RC["COS"], RC["SINS"], RC["r"]
                c0 = t0 - 256
                stt("dve", tb[0:64, 0:n], ps[bank_a][0:64, 0:n], scale, COS[:, c0:c0 + n], ALU.mult, ALU.mult, [rps[bank_a], r_rope], [rtb])
                stt("dve", tb[0:64, 512:512 + n], ps[bank_b][0:64, 0:n], scale, SINS[:, c0:c0 + n], ALU.mult, ALU.mult, [rps[bank_b], r_rope, rtb], [rtb])
                tt("dve", dst[0:64, t0:t0 + n], tb[0:64, 0:n], tb[0:64, 512:512 + n], ALU.add, [rtb], [r_dst])

            for (t0, n) in TOKG:
                ba = bi % 4; bi += 1
                bb = bi % 4; bi += 1
                lin_fm(wkr, rw, 16, 64, t0, n, ba, hT, r_hT, m0=0)
                lin_fm(wkr, rw, 16, 64, t0, n, bb, hT, r_hT, m0=64)
                rope_evac(ba, bb, n, t0, KRT, r_krt, 1.0)

            gr = Ring(A, 2, NT, BF16)
            for ch in range(32):
                wt, rw = wfm.next()
                wt3 = wt.rearrange("p (k n) -> p k n", k=16)
                wcast(wt3, W[:, 9344 + ch * 128:9344 + (ch + 1) * 128], rw)
                gt, rg = gr.next()
                for (t0, n) in TOKG:
                    bank = bi % 4; bi += 1
                    lin_fm(wt3, rw, 16, 128, t0, n, bank, hT, r_hT)
                    act(gt[:, t0:t0 + n], ps[bank][:, 0:n], AF.Sigmoid, [rps[bank]], [rg])
                P.dma("sp", GT[ch], gt, reads=[rg])

            release()
            load_rope()
            wqr = Ring(A, 2, 4 * 256, BF16)
            wkr2 = Ring(A, 2, 4 * 256, BF16)
            str_ = Ring(A, 3, NT, BF16)
            vstr = Ring(A, 2, NT, BF16)
            for h in range(H):
                wq, rwq = wqr.next()
                wq3 = wq.rearrange("p (k n) -> p k n", k=4)
                src = w_uq[l]
                P.dma("pool", wq3[:, :, 0:192], src[:, h * 192:(h + 1) * 192].rearrange("(k p) n -> p k n", p=128), writes=[rwq])
                for (dc, sc) in PERM:
                    P.dma("pool", wq3[:, :, 192 + dc:192 + dc + 16], src[:, h * 192 + 128 + sc:h * 192 + 128 + sc + 16].rearrange("(k p) n -> p k n", p=128), writes=[rwq])
                wk, rwk = wkr2.next()
                wk3 = wk.rearrange("p (k n) -> p k n", k=4)
                P.dma("pool", wk3, w_ukv[l][:, h * 256:(h + 1) * 256].rearrange("(k p) n -> p k n", p=128), writes=[rwk])
                sqn, rsqn = str_.next()
                sqr_, rsqr = str_.next()
                skn, rskn = str_.next()
                for (t0, n) in TOKG:
                    bank = bi % 4; bi += 1
                    lin_fm(wq3, rwq, 4, 128, t0, n, bank, DQK[0], r_dqk[0])
                    act(sqn[:, t0:t0 + n], ps[bank][:, 0:n], AF.Copy, [rps[bank]], [rsqn], scale=MLA_SCALE)
                    ba = bi % 4; bi += 1
                    bb = bi % 4; bi += 1
                    lin_fm(wq3, rwq, 4, 64, t0, n, ba, DQK[0], r_dqk[0], m0=128)
                    lin_fm(wq3, rwq, 4, 64, t0, n, bb, DQK[0], r_dqk[0], m0=192)
                    rope_evac(ba, bb, n, t0, sqr_, rsqr, MLA_SCALE)
                    bank = bi % 4; bi += 1
                    lin_fm(wk3, rwk, 4, 128, t0, n, bank, DQK[1], r_dqk[1])
                    P.op("dve", lambda e, skn=skn, t0=t0, n=n, bank=bank: e.tensor_copy(skn[:, t0:t0 + n], ps[bank][:, 0:n]), reads=[rps[bank]], writes=[rskn])
                P.dma("sp", QNT[h], sqn, reads=[rsqn])
                P.dma("sp", QRT[h], sqr_[0:64, :], reads=[rsqr])
                P.dma("sp", KNT[h], skn, reads=[rskn])
                vs, rvs = vstr.next()
                vs3 = vs.rearrange("p (t d) -> p t d", d=128)
                for t in range(NTILE):
                    bank = 4 + (t % 2)
                    for k in range(4):
                        mm(ps[bank][:, 0:128], DQK[1][:, k, t * 128:(t + 1) * 128], wk3[:, k, 128:256], k == 0, k == 3, [rwk, r_dqk[1]], [rps[bank]])
                    act(vs3[:, t, :], ps[bank][:, 0:128], AF.Copy, [rps[bank]], [rvs])
                P.dma("sp", VM2[h].rearrange("(t p) d -> p t d", p=128), vs3, reads=[rvs])
            release()

        def phase_mla(l):
            A.mark()
            qtiles = list(range(NTILE)) if l == 0 else list(range(CTXT, NTILE))
            kn_ = Ring(A, 2, NT, BF16); qn_ = Ring(A, 2, NT, BF16); qr_ = Ring(A, 2, NT, BF16); v_ = Ring(A, 2, NT, BF16)
            p_ = Ring(A, 2, NT, BF16); pt_ = Ring(A, 2, NT, BF16); yb_ = Ring(A, 2, NT, BF16)
            sm_ = Ring(A, 3, 8, F32); ob_ = Ring(A, 2, 128, BF16)
            ei = 0
            for h in range(H):
                knt, rkn = kn_.next(); qnt, rqn = qn_.next(); qrt, rqr = qr_.next(); v, rv = v_.next()
                P.dma("sp", knt, KNT[h], writes=[rkn])
                P.dma("sp", qnt, QNT[h], writes=[rqn])
                P.dma("sp", qrt[0:64, :], QRT[h], writes=[rqr])
                v3 = v.rearrange("p (t d) -> p t d", d=128)
                P.dma("sp", v3, VM2[h].rearrange("(t p) d -> p t d", p=128), writes=[rv])
                yb, ryb = yb_.next()
                for qt in qtiles:
                    qc = slice(qt * 128, (qt + 1) * 128)
                    nk = 256 if qt < CTXT else NT
                    groups = [(0, 256)] if qt < CTXT else [(0, 512), (512, 512), (1024, 512), (1536, 512), (2048, 256)]
                    for gi, (k0, n) in enumerate(groups):
                        mm(ps[gi][:, 0:n], qnt[:, qc], knt[:, k0:k0 + n], True, False, [rqn, rkn], [rps[gi]])
                        mm(ps[gi][:, 0:n], qrt[0:64, qc], KRT[0:64, k0:k0 + n], False, True, [rqr, r_krt], [rps[gi]])
                    S = ps_all[:, 0:nk]
                    rS = [rps[gi] for gi in range(len(groups))]
                    s, rs = sm_.next()
                    P.op("dve", lambda e, s=s, S=S: e.reduce_max(s[:, 0:1], S, AX.X), reads=rS, writes=[rs])
                    ts("dve", s[:, 1:2], s[:, 0:1], -1.0, None, ALU.mult, None, [rs], [rs])
                    p, rp = p_.next()
                    act(p[:, 0:nk], S, AF.Exp, rS + [rs], [rp, rs], bias=s[:, 1:2], scale=1.0, accum_out=s[:, 2:3])
                    P.op("dve", lambda e, s=s: e.reciprocal(s[:, 3:4], s[:, 2:3]), reads=[rs], writes=[rs])
                    pt, rpt = pt_.next()
                    pt3 = pt.rearrange("p (t q) -> p t q", q=128)
                    nkt = nk // 128
                    for g in range((nkt + 3) // 4):
                        bank = 5 + (g % 2)
                        nn = min(4, nkt - 4 * g)
                        for j in range(nn):
                            kt = 4 * g + j
                            tr(psb[bank][:, j * 128:(j + 1) * 128], p[:, kt * 128:(kt + 1) * 128], [rp], [rps[bank]])
                        src = psb[bank][:, 0:nn * 128].rearrange("p (j t) -> p j t", j=nn)
                        ei += 1
                        if ei % 2:
                            P.op("dve", lambda e, pt3=pt3, g=g, nn=nn, src=src: e.tensor_copy(pt3[:, 4 * g:4 * g + nn, :], src), reads=[rps[bank]], writes=[rpt])
                        else:
                            act(pt3[:, 4 * g:4 * g + nn, :], src, AF.Copy, [rps[bank]], [rpt])
                    for kt in range(nkt):
                        mm(ps[7][:, 0:128], pt3[:, kt, :], v3[:, kt, :], kt == 0, kt == nkt - 1, [rpt, rv], [rps[7]])
                    ob, rob = ob_.next()
                    act(ob, ps[7][:, 0:128], AF.Copy, [rps[7], rs], [rob], scale=s[:, 3:4])
                    tr(psb[7][:, 512:640], ob, [rob], [rps[7]])
                    P.op("dve", lambda e, yb=yb, qc=qc: e.tensor_copy(yb[:, qc], psb[7][:, 512:640]), reads=[rps[7]], writes=[ryb])
                if l == 1:
                    P.op("dve", lambda e, yb=yb: e.memset(yb[:, 0:256], 0.0), writes=[ryb])
                P.dma("sp", YBT[h], yb, reads=[ryb])
            release()

        def phase_gdn(l):
            A.mark()
            S32 = A.alloc(32 * 128, F32).rearrange("p (c v) -> p c v", v=128)
            Sbf = A.alloc(32 * 128, BF16).rearrange("p (c v) -> p c v", v=128)
            r_S = [Res() for _ in range(32)]
            P.op("dve", lambda e: e.memset(S32, 0.0), writes=r_S)
            P.op("dve", lambda e: e.memset(Sbf, 0.0), writes=r_S)
            order = {0: list(range(NTILE)), 1: [1, 0] + list(range(NTILE - 1, 1, -1))}
            qT_ = Ring(A, 2, 2048, BF16); kT_ = Ring(A, 2, 2048, BF16); kM_ = Ring(A, 2, 2048, BF16); vM_ = Ring(A, 2, 2048, BF16)
            kF_ = Ring(A, 2, 2048, F32)
            o_ = Ring(A, 2, 2048, F32)
            sm_ = Ring(A, 2, 16 * 10, F32)
            mb_ = Ring(A, 20, 128, BF16)
            mf_ = Ring(A, 56, 128, F32)
            for (b_, r_) in mb_.bufs + mf_.bufs:
                P.op("dve", lambda e, b_=b_: e.memset(b_, 0.0), writes=[r_])
            slots = [(ps[b][:, 0:128], rps[b]) for b in range(8)]
            sl = [0]

            def slot():
                x = slots[sl[0] % 8]
                sl[0] += 1
                return x
            ev = [0]

            def evac(dst, rdst, src, rsrc):
                ev[0] += 1
                if ev[0] % 2:
                    P.op("dve", lambda e: e.tensor_copy(dst, src), reads=[rsrc], writes=[rdst])
                else:
                    act(dst, src, AF.Copy, [rsrc], [rdst])

            for s in range(GDN_STEPS):
                for d in (0, 1):
                    t = order[d][s]
                    tc = slice(t * 128, (t + 1) * 128)
                    qTt, rqT = qT_.next(); kTt, rkT = kT_.next(); kMt, rkM = kM_.next(); vMt, rvM = vM_.next()
                    qT3 = qTt.rearrange("p (h t) -> p h t", h=16); kT3 = kTt.rearrange("p (h t) -> p h t", h=16)
                    kM3 = kMt.rearrange("p (h t) -> p h t", h=16); vM3 = vMt.rearrange("p (h t) -> p h t", h=16)
                    P.dma("sp", qT3, QT[:, :, tc].rearrange("h p t -> p h t"), writes=[rqT])
                    P.dma("sp", kT3, KT[:, :, tc].rearrange("h p t -> p h t"), writes=[rkT])
                    kFt, rkF = kF_.next()
                    kF3 = kFt.rearrange("p (h t) -> p h t", h=16)
                    P.dma("sp", kF3, KT32[:, :, tc].rearrange("h p t -> p h t"), writes=[rkF])
                    P.dma("sp", kMt, KM[tc].rearrange("p h d -> p (h d)"), writes=[rkM])
                    P.dma("sp", vMt, VM[tc].rearrange("p h d -> p (h d)"), writes=[rvM])
                    sm, rsm = sm_.next()
                    smv = sm.rearrange("p (c h) -> p c h", h=16)
                    GC, GB, NGC, EGC, EGB, KD0, EGL0, EGL1, EB, KD1 = [smv[:, i, :] for i in range(10)]
                    gsl = g_all[:, t, d * 16:(d + 1) * 16]
                    pg, rpg = slot()
                    mm(pg[:, 0:16], MCUM[d], gsl, True, True, [r_const, r_gall], [rpg])
                    mm(pg[:, 16:32], MCH[0], gsl, True, True, [r_const, r_gall], [rpg])
                    mm(pg[:, 32:48], MCH[1], gsl, True, True, [r_const, r_gall], [rpg])
                    P.op("dve", lambda e, GC=GC, pg=pg: e.tensor_copy(GC, pg[:, 0:16]), reads=[rpg], writes=[rsm])
                    tt("dve", GB, GC, lnb_all[:, t, d * 16:(d + 1) * 16], ALU.add, [rsm, r_gall], [rsm])
                    ts("dve", NGC, GC, -1.0, None, ALU.mult, None, [rsm], [rsm])
                    act(EGC, GC, AF.Exp, [rsm], [rsm])
                    act(EGB, GB, AF.Exp, [rsm], [rsm])
                    act(EB, lnb_all[:, t, d * 16:(d + 1) * 16], AF.Exp, [rsm, r_gall], [rsm])
                    P.op("dve", lambda e, KD0=KD0: e.memset(KD0, 0.0), writes=[rsm])
                    P.op("dve", lambda e, KD1=KD1: e.memset(KD1, 0.0), writes=[rsm])
                    tt("dve", KD0[0:64, :], pg[0:64, 16:32], GC[0:64, :], ALU.subtract, [rpg, rsm], [rsm])
                    tt("dve", KD1[64:128, :], pg[64:128, 32:48], GC[64:128, :], ALU.subtract, [rpg, rsm], [rsm])
                    act(KD0[0:64, :], KD0[0:64, :], AF.Exp, [rsm], [rsm])
                    act(KD1[64:128, :], KD1[64:128, :], AF.Exp, [rsm], [rsm])
                    KD = (KD0, KD1)
                    act(EGL0, pg[:, 16:32], AF.Exp, [rpg, rsm], [rsm])
                    act(EGL1, pg[:, 32:48], AF.Exp, [rpg, rsm], [rsm])
                    EGL = (EGL0, EGL1)
                    ot, ro = o_.next()
                    o3 = ot.rearrange("p (h v) -> p h v", h=16)
                    for h in range(GDN_HEADS):
                        dh = d * 16 + h
                        hc = slice(h, h + 1)
                        pK, rK = slot(); mm(pK, kF3[:, h, :], kF3[:, h, :], True, True, [rkF], [rK])
                        pQ, rQ = slot(); mm(pQ, kT3[:, h, :], qT3[:, h, :], True, True, [rkT, rqT], [rQ])
                        dg1, rdg1 = mf_.next()
                        act(dg1, identf, AF.Copy, [r_const, rsm], [rdg1], scale=NGC[:, hc])
                        pD, rD = slot()
                        mm(pD, ONESF, dg1, True, False, [r_const, rdg1], [rD])
                        mm(pD, identf, NEGM_P[d], False, True, [r_const], [rD])
                        DP, rDP = mf_.next()
                        act(DP, pD, AF.Exp, [rD, rsm], [rDP], bias=GB[:, hc], scale=1.0)
                        dg2, rdg2 = mf_.next()
                        act(dg2, identf, AF.Copy, [r_const, rsm], [rdg2], scale=GC[:, hc])
                        pD2, rD2 = slot()
                        mm(pD2, ONESF, dg2, True, False, [r_const, rdg2], [rD2])
                        mm(pD2, identf, NEGM_AT[d], False, True, [r_const], [rD2])
                        DAT, rDAT = mf_.next()
                        act(DAT, pD2, AF.Exp, [rD2, rsm], [rDAT], bias=NGC[:, hc], scale=1.0)
                        Pm = [None] * 6; Qm = [None] * 5; TTm = [None] * 6
                        Pm[0] = mf_.next()
                        stt("dve", Pm[0][0], pK, -1.0, DP, ALU.mult, ALU.mult, [rK, rDP], [Pm[0][1]])
                        AT, rAT = mb_.next()
                        tt("dve", AT, pQ, DAT, ALU.mult, [rQ, rDAT], [rAT])
                        pT, rT = slot()
                        tr(pT, Pm[0][0], [Pm[0][1]], [rT], f32=True)
                        Qm[0] = mf_.next()
                        evac(Qm[0][0], Qm[0][1], pT, rT)
                        for j in range(4):
                            p1, r1 = slot(); mm(p1, Qm[j][0], Pm[j][0], True, True, [Qm[j][1], Pm[j][1]], [r1])
                            Pm[j + 1] = mf_.next(); evac(Pm[j + 1][0], Pm[j + 1][1], p1, r1)
                            p2, r2 = slot(); mm(p2, Pm[j][0], Qm[j][0], True, True, [Qm[j][1], Pm[j][1]], [r2])
                            Qm[j + 1] = mf_.next(); evac(Qm[j + 1][0], Qm[j + 1][1], p2, r2)
                        p1, r1 = slot(); mm(p1, Qm[4][0], Pm[4][0], True, True, [Qm[4][1], Pm[4][1]], [r1])
                        Pm[5] = mf_.next(); evac(Pm[5][0], Pm[5][1], p1, r1)
                        TTm[0] = mf_.next()
                        tt("pool", TTm[0][0], Qm[0][0], identf, ALU.add, [Qm[0][1], r_const], [TTm[0][1]])
                        for m in range(5):
                            p1, r1 = slot()
                            mm(p1, identf, TTm[m][0], True, False, [r_const, TTm[m][1]], [r1])
                            mm(p1, Pm[m + 1][0], TTm[m][0], False, True, [Pm[m + 1][1], TTm[m][1]], [r1])
                            TTm[m + 1] = mf_.next(); evac(TTm[m + 1][0], TTm[m + 1][1], p1, r1)
                        TT5, rTT5 = TTm[5]
                        vb, rvb = mf_.next(); kbg, rkbg = mf_.next()
                        kdecs = [mb_.next(), mb_.next()]
                        act(vb, vM3[:, h, :], AF.Copy, [rvM, rsm], [rvb], scale=EB[:, hc])
                        act(kbg, kM3[:, h, :], AF.Copy, [rkM, rsm], [rkbg], scale=EGB[:, hc])
                        for ci in (0, 1):
                            ts("dve", kdecs[ci][0], kM3[:, h, :], KD[ci][:, hc], None, ALU.mult, None, [rkM, rsm], [kdecs[ci][1]])
                        pU, rU = slot(); mm(pU, TT5, vb, True, True, [rTT5, rvb], [rU])
                        u, ru = mf_.next(); evac(u, ru, pU, rU)
                        pW, rW = slot(); mm(pW, kbg, TT5, True, True, [rTT5, rkbg], [rW])
                        wT, rwT = mb_.next(); evac(wT, rwT, pW, rW)
                        vnew, rvn = mb_.next()
                        tmp, rtmp = mf_.next()
                        for ci in ((0, 1) if d == 0 else (1, 0)):
                            pr = slice(ci * 64, ci * 64 + 64)
                            kdec, rkdec = kdecs[ci]
                            pv, rv_ = slot()
                            mm(pv, wT, Sbf[:, dh, :], True, True, [rwT, r_S[dh]], [rv_])
                            tt("dve", vnew[pr, :], u[pr, :], pv[pr, :], ALU.subtract, [ru, rv_], [rvn])
                            po1, ro1 = slot()
                            mm(po1, qT3[:, h, :], Sbf[:, dh, :], True, True, [rqT, r_S[dh]], [ro1])
                            po2, ro2 = slot()
                            mm(po2, AT, vnew, True, True, [rAT, rvn], [ro2])
                            act(tmp[pr, :], po1[pr, :], AF.Copy, [ro1, rsm], [rtmp], scale=EGC[pr, hc])
                            tt("dve", o3[pr, h, :], tmp[pr, :], po2[pr, :], ALU.add, [rtmp, ro2], [ro])
                            pS, rS_ = slot()
                            mm(pS, kdec, vnew, True, True, [rkdec, rvn], [rS_])
                            stt("dve", S32[:, dh, :], S32[:, dh, :], EGL[ci][:, hc], pS, ALU.mult, ALU.add, [r_S[dh], rsm, rS_], [r_S[dh]])
                            act(Sbf[:, dh, :], S32[:, dh, :], AF.Copy, [r_S[dh]], [r_S[dh]])
                    P.dma("sp", OFB[d, tc, :], ot, reads=[ro])
            release()

        def phase_gdn_out(l):
            A.mark()
            tiles = list(range(NTILE)) if l == 0 else list(range(CTXT, NTILE))
            gnb = A.alloc(128, F32); r_gn = Res()
            P.dma("sp", gnb, gdn_norm[l].partition_broadcast(128), writes=[r_gn])
            of_ = Ring(A, 2, 2048, F32); ob_ = Ring(A, 2, 2048, F32); z_ = Ring(A, 2, 2048, F32)
            sq_ = Ring(A, 1, 2048, F32); ss_ = Ring(A, 2, 32, F32); yb_ = Ring(A, 2, 2048, BF16); yt_ = Ring(A, 2, 2048, BF16)
            for t in tiles:
                tc = slice(t * 128, (t + 1) * 128)
                of, rof = of_.next(); ob, rob = ob_.next(); z, rz = z_.next()
                P.dma("sp", of, OFB[0, tc, :], writes=[rof])
                P.dma("sp", ob, OFB[1, tc, :], writes=[rob])
                P.dma("sp", z, ZS[tc, :], writes=[rz])
                tt("pool", of, of, ob, ALU.add, [rof, rob], [rof])
                sq, rsq = sq_.next()
                act(sq, of, AF.Square, [rof], [rsq])
                ss, rss = ss_.next()
                P.op("dve", lambda e, ss=ss, sq=sq: e.reduce_sum(ss[:, 0:16], sq.rearrange("p (h v) -> p h v", h=16), AX.X), reads=[rsq], writes=[rss])
                ts("dve", ss[:, 0:16], ss[:, 0:16], 1.0 / 128, EPS, ALU.mult, ALU.add, [rss], [rss])
                act(ss[:, 0:16], ss[:, 0:16], AF.Sqrt, [rss], [rss])
                P.op("dve", lambda e, ss=ss: e.reciprocal(ss[:, 0:16], ss[:, 0:16]), reads=[rss], writes=[rss])
                for h in range(H):
                    hs = slice(h * 128, (h + 1) * 128)
                    stt("dve", of[:, hs], of[:, hs], ss[:, h:h + 1], gnb, ALU.mult, ALU.mult, [rof, rss, r_gn], [rof])
                yb, ryb = yb_.next()
                tt("dve", yb, of, z, ALU.mult, [rof, rz], [ryb])
                yt, ryt = yt_.next()
                yt3 = yt.rearrange("p (h t) -> p h t", h=16)
                for g in range(4):
                    bank = 6 + (g % 2)
                    for j in range(4):
                        k = g * 4 + j
                        tr(psb[bank][:, j * 128:(j + 1) * 128], yb[:, k * 128:(k + 1) * 128], [ryb], [rps[bank]])
                    act(yt3[:, g * 4:(g + 1) * 4, :], psb[bank][:, 0:512].rearrange("p (j t) -> p j t", j=4), AF.Copy, [rps[bank]], [ryt])
                P.dma("sp", YAT[:, :, tc].rearrange("h p t -> p h t"), yt3, reads=[ryt])
            release()

        def resid_ln(xt, rx, r_, rr, gtb, gb_, bb_, r_b, dst, sr):
            tt("pool", r_, r_, gtb, ALU.mult, [rr, r_b], [rr])
            stt("dve", xt, xt, ALPHA, r_, ALU.mult, ALU.add, [rx, rr], [rx])
            s, rs = sr.next()
            st6 = s[:, 0:24].rearrange("p (c s) -> p c s", s=6)
            xv = xt.rearrange("p (c f) -> p c f", f=512)
            for c in range(4):
                P.op("dve", lambda e, c=c: e.bn_stats(st6[:, c, :], xv[:, c, :]), reads=[rx], writes=[rs])
            P.op("dve", lambda e: e.bn_aggr(s[:, 24:26], st6), reads=[rs], writes=[rs])
            ts("dve", s[:, 26:27], s[:, 25:26], EPS, None, ALU.add, None, [rs], [rs])
            act(s[:, 26:27], s[:, 26:27], AF.Sqrt, [rs], [rs])
            P.op("dve", lambda e: e.reciprocal(s[:, 26:27], s[:, 26:27]), reads=[rs], writes=[rs])
            ts("dve", xt, xt, s[:, 24:25], s[:, 26:27], ALU.subtract, ALU.mult, [rx, rs], [rx])
            tt("pool", xt, xt, gb_, ALU.mult, [rx, r_b], [rx])
            tt("dve", xt, xt, bb_, ALU.add, [rx, r_b], [rx])
            P.dma("sp", dst, xt, reads=[rx])

        def load_bcast(l, gt_idx, g_d, b_d):
            r_b = Res()
            gts = []
            for r in (0, 1):
                a = A.alloc(2048, F32)
                P.dma("sp", a, MODV[l, r, gt_idx * 2048:(gt_idx + 1) * 2048].partition_broadcast(128), writes=[r_b])
                gts.append(a)
            gb_ = A.alloc(2048, F32); bb_ = A.alloc(2048, F32)
            P.dma("sp", gb_, g_d[l].partition_broadcast(128), writes=[r_b])
            P.dma("sp", bb_, b_d[l].partition_broadcast(128), writes=[r_b])
            return gts, gb_, bb_, r_b

        def phase_merge(l):
            A.mark()
            groups = TOKG if l == 0 else TOKG[1:]
            gts, gb_, bb_, r_b = load_bcast(l, 2, ln1_g, ln1_b)
            ya_ = Ring(A, 1, 16 * 512, BF16); yb_ = Ring(A, 1, 16 * 512, BF16); yT_ = Ring(A, 1, 16 * 512, BF16)
            w_ = Ring(A, 4, 16 * 128, BF16); g_ = Ring(A, 4, 512, BF16); t_ = Ring(A, 4, 512, F32)
            wo_ = Ring(A, 2, 16 * 256, BF16)
            m_ = Ring(A, 4, 2048, F32); x_ = Ring(A, 1, 2048, F32); sr = Ring(A, 2, 32, F32)
            bi = 0
            for (t0, n) in groups:
                ya, rya = ya_.next(); yb, ryb = yb_.next(); yT, ryT = yT_.next()
                ya3 = ya.rearrange("p (k t) -> p k t", k=16)[:, :, 0:n]; yb3 = yb.rearrange("p (k t) -> p k t", k=16)[:, :, 0:n]
                yT3 = yT.rearrange("p (k t) -> p k t", k=16)
                P.dma("sp", ya3, YAT[:, :, t0:t0 + n].rearrange("h p t -> p h t"), writes=[rya])
                P.dma("sp", yb3, YBT[:, :, t0:t0 + n].rearrange("h p t -> p h t"), writes=[ryb])
                for m in range(16):
                    wa, rwa = w_.next(); wb, rwb = w_.next()
                    wa3 = wa.rearrange("p (k n) -> p k n", k=16); wb3 = wb.rearrange("p (k n) -> p k n", k=16)
                    wcast(wa3, w_br_a[l][:, m * 128:(m + 1) * 128], rwa)
                    wcast(wb3, w_br_b[l][:, m * 128:(m + 1) * 128], rwb)
                    ga, rga = g_.next(); gb2, rgb = g_.next()
                    P.dma("sp", ga[:, 0:n], GT[m][:, t0:t0 + n], writes=[rga])
                    P.dma("sp", gb2[:, 0:n], GT[16 + m][:, t0:t0 + n], writes=[rgb])
                    ba = bi % 6; bi += 1
                    bb = bi % 6; bi += 1
                    for k in range(16):
                        mm(ps[ba][:, 0:n], wa3[:, k, :], ya3[:, k, :], k == 0, k == 15, [rwa, rya], [rps[ba]])
                    for k in range(16):
                        mm(ps[bb][:, 0:n], wb3[:, k, :], yb3[:, k, :], k == 0, k == 15, [rwb, ryb], [rps[bb]])
                    t1, rt1 = t_.next(); t2, rt2 = t_.next()
                    tt("dve", t1[:, 0:n], ps[ba][:, 0:n], ga[:, 0:n], ALU.mult, [rps[ba], rga], [rt1])
                    tt("dve", t2[:, 0:n], ps[bb][:, 0:n], gb2[:, 0:n], ALU.mult, [rps[bb], rgb], [rt2])
                    tt("pool", yT3[:, m, 0:n], t1[:, 0:n], t2[:, 0:n], ALU.add, [rt1, rt2], [ryT])
                ntl = n // 128
                ms = [m_.next() for _ in range(ntl)]
                for cb in range(8):
                    wo, rwo = wo_.next()
                    wo3 = wo.rearrange("p (k n) -> p k n", k=16)
                    wcast(wo3, w_out[l][:, cb * 256:(cb + 1) * 256], rwo)
                    for ti in range(ntl):
                        bank = bi % 6; bi += 1
                        for k in range(16):
                            mm(ps[bank][:, 0:256], yT3[:, k, ti * 128:(ti + 1) * 128], wo3[:, k, :], k == 0, k == 15, [rwo, ryT], [rps[bank]])
                        act(ms[ti][0][:, cb * 256:(cb + 1) * 256], ps[bank][:, 0:256], AF.Copy, [rps[bank]], [ms[ti][1]])
                for ti in range(ntl):
                    t = t0 // 128 + ti
                    xt, rx = x_.next()
                    P.dma("sp", xt, XS[t * 128:(t + 1) * 128, :], writes=[rx])
                    resid_ln(xt, rx, ms[ti][0], ms[ti][1], gts[1 if t < CTXT else 0], gb_, bb_, r_b, XS[t * 128:(t + 1) * 128, :], sr)
            release()

        def phase_ffn(l, last):
            moe = (l % 2 == 1)
            tiles = list(range(NTILE)) if not last else list(range(CTXT, NTILE))
            ntl = len(tiles); ntok = ntl * 128; tb = tiles[0]
            E = NEXP if moe else 1
            FFC = (FF_EXP if moe else FF_DENSE) // 128
            A.mark()
            hT = A.alloc(16 * ntok, BF16).rearrange("p (k t) -> p k t", k=16); r_hT = Res()
            A.mark()
            extra = None
            if moe:
                rwt = A.alloc(128, F32); r_rw = Res()
                P.dma("sp", rwt, router, writes=[r_rw])
                rw3 = rwt.rearrange("p (k e) -> p k e", e=8)
                hf_ = Ring(A, 1, 2048, F32); lg_ = Ring(A, 2, 40, F32)

                def extra(t, xt, rx):
                    ti = t - tb
                    hf, rhf = hf_.next()
                    hf3 = hf.rearrange("p (k t) -> p k t", k=16)
                    for g in range(4):
                        bank = g % 4
                        for j in range(4):
                            k = g * 4 + j
                            tr(ps[bank][:, j * 128:(j + 1) * 128], xt[:, k * 128:(k + 1) * 128], [rx], [rps[bank]], f32=True)
                        act(hf3[:, g * 4:(g + 1) * 4, :], ps[bank].rearrange("p (j t) -> p j t", j=4), AF.Copy, [rps[bank]], [rhf])
                    for k in range(16):
                        mm(ps[4][:, 0:8], hf3[:, k, :], rw3[:, k, :], k == 0, k == 15, [rhf, r_rw], [rps[4]])
                    lg, rlg = lg_.next()
                    P.op("dve", lambda e: e.tensor_copy(lg[:, 0:8], ps[4][:, 0:8]), reads=[rps[4]], writes=[rlg])
                    P.op("dve", lambda e: e.max(lg[:, 8:16], lg[:, 0:8]), reads=[rlg], writes=[rlg])
                    ts("dve", lg[:, 16:24], lg[:, 0:8], lg[:, 9:10], None, ALU.is_ge, None, [rlg], [rlg])
                    ts("dve", lg[:, 32:33], lg[:, 8:9], -1.0, None, ALU.mult, None, [rlg], [rlg])
                    act(lg[:, 24:32], lg[:, 0:8], AF.Exp, [rlg], [rlg], bias=lg[:, 32:33], scale=1.0)
                    tt("dve", lg[:, 24:32], lg[:, 24:32], lg[:, 16:24], ALU.mult, [rlg], [rlg])
                    P.op("dve", lambda e: e.reduce_sum(lg[:, 33:34], lg[:, 24:32], AX.X), reads=[rlg], writes=[rlg])
                    P.op("dve", lambda e: e.reciprocal(lg[:, 33:34], lg[:, 33:34]), reads=[rlg], writes=[rlg])
                    ts("dve", comb[:, ti, :], lg[:, 24:32], lg[:, 33:34], None, ALU.mult, None, [rlg], [r_comb])
            ln_mod_T(l, 1, tiles, hT, r_hT, lambda t: (t - tb) * 128, extra=extra)
            release()
            A.mark()
            w1_ = Ring(A, 2, 16 * 512, BF16); w3_ = Ring(A, 2, 16 * 512, BF16)
            s1_ = Ring(A, 3, 512, F32); st_ = Ring(A, 3, ntok, BF16)
            tgs = [(c0, min(512, ntok - c0)) for c0 in range(0, ntok, 512)]
            bi = 0
            for e in range(E):
                W1 = moe_w1[0, e] if moe else ffn_w1[0]
                W3 = moe_w3[0, e] if moe else ffn_w3[0]
                for fb in range((FFC + 3) // 4):
                    nfc = min(4, FFC - 4 * fb)
                    w1, rw1 = w1_.next(); w3, rw3_ = w3_.next()
                    w13 = w1.rearrange("p (k n) -> p k n", k=16)[:, :, 0:nfc * 128]; w33 = w3.rearrange("p (k n) -> p k n", k=16)[:, :, 0:nfc * 128]
                    wcast(w13, W1[:, fb * 512:fb * 512 + nfc * 128], rw1)
                    wcast(w33, W3[:, fb * 512:fb * 512 + nfc * 128], rw3_)
                    for fc in range(nfc):
                        stg, rst = st_.next()
                        for (c0, n) in tgs:
                            b1 = bi % 8; bi += 1
                            b3 = bi % 8; bi += 1
                            for k in range(16):
                                mm(ps[b1][:, 0:n], w13[:, k, fc * 128:(fc + 1) * 128], hT[:, k, c0:c0 + n], k == 0, k == 15, [rw1, r_hT], [rps[b1]])
                            for k in range(16):
                                mm(ps[b3][:, 0:n], w33[:, k, fc * 128:(fc + 1) * 128], hT[:, k, c0:c0 + n], k == 0, k == 15, [rw3_, r_hT], [rps[b3]])
                            s1, rs1 = s1_.next()
                            act(s1[:, 0:n], ps[b1][:, 0:n], AF.Silu, [rps[b1]], [rs1])
                            tt("dve", stg[:, c0:c0 + n], s1[:, 0:n], ps[b3][:, 0:n], ALU.mult, [rs1, rps[b3]], [rst])
                        P.dma("sp", ATS[e, fb * 4 + fc, :, 0:ntok], stg, reads=[rst])
            release()
            release()
            A.mark()
            HF = FFC // 2
            w2_ = Ring(A, 2, HF * 512, BF16); at_ = Ring(A, 3, HF * 128, BF16)
            acc = A.alloc(ntl * 512, F32).rearrange("p (t n) -> p t n", n=512); r_acc = [Res() for _ in range(ntl)]
            for cb in range(4):
                for e in range(E):
                    W2 = moe_w2[0, e] if moe else ffn_w2[0]
                    for half in range(2):
                        w2, rw2 = w2_.next()
                        w23 = w2.rearrange("p (f n) -> p f n", n=512)
                        wcast(w23, W2[half * HF * 128:(half + 1) * HF * 128, cb * 512:(cb + 1) * 512], rw2)
                        for ti in range(ntl):
                            at, rat = at_.next()
                            at3 = at.rearrange("p (f t) -> p f t", t=128)
                            P.dma("sp", at3, ATS[e, half * HF:(half + 1) * HF, :, ti * 128:(ti + 1) * 128].rearrange("f p t -> p f t"), writes=[rat])
                            bank = bi % 8; bi += 1
                            for f in range(HF):
                                mm(ps[bank], at3[:, f, :], w23[:, f, :], f == 0, f == HF - 1, [rat, rw2], [rps[bank]])
                            first = (e == 0 and half == 0)
                            if moe:
                                if first:
                                    ts("dve", acc[:, ti, :], ps[bank], comb[:, ti, e:e + 1], None, ALU.mult, None, [rps[bank], r_comb], [r_acc[ti]])
                                else:
                                    stt("dve", acc[:, ti, :], ps[bank], comb[:, ti, e:e + 1], acc[:, ti, :], ALU.mult, ALU.add, [rps[bank], r_comb, r_acc[ti]], [r_acc[ti]])
                            else:
                                if first:
                                    act(acc[:, ti, :], ps[bank], AF.Copy, [rps[bank]], [r_acc[ti]])
                                else:
                                    tt("dve", acc[:, ti, :], acc[:, ti, :], ps[bank], ALU.add, [rps[bank], r_acc[ti]], [r_acc[ti]])
                for ti in range(ntl):
                    t = tiles[ti]
                    P.dma("sp", FO[t * 128:(t + 1) * 128, cb * 512:(cb + 1) * 512], acc[:, ti, :], reads=[r_acc[ti]])
            release()
            A.mark()
            gts, gb_, bb_, r_b = load_bcast(l, 5, ln2_g, ln2_b)
            x_ = Ring(A, 2, 2048, F32); f_ = Ring(A, 2, 2048, F32); sr = Ring(A, 2, 32, F32)
            for t in tiles:
                xt, rx = x_.next(); ft, rf = f_.next()
                P.dma("sp", xt, XS[t * 128:(t + 1) * 128, :], writes=[rx])
                P.dma("sp", ft, FO[t * 128:(t + 1) * 128, :], writes=[rf])
                dst = out_d[(t - CTXT) * 128:(t - CTXT + 1) * 128, :] if last else XS[t * 128:(t + 1) * 128, :]
                resid_ln(xt, rx, ft, rf, gts[1 if t < CTXT else 0], gb_, bb_, r_b, dst, sr)
            release()

        GDN_STEPS = 1 if 'gdn_small' in dbg else NTILE
        GDN_HEADS = 1 if 'gdn_small' in dbg else H
        GDN_LVL = 99
        for x in dbg:
            if x.startswith('gdnlvl'):
                GDN_LVL = int(x[6:])
        phase_init()
        for l in range(nlayers):
            last = (l == 1)
            phase_proj(l)
            if "stop_proj" in dbg:
                break
            phase_mla(l)
            if "stop_mla" in dbg:
                break
            phase_gdn(l)
            phase_gdn_out(l)
            if "stop_gdn" in dbg:
                break
            phase_merge(l)
            if "stop_merge" in dbg:
                break
            phase_ffn(l, last)
        nsem = P.emit()
        print("built: ops", {e: len(P.ops[e]) for e in ENGS}, "nsem", nsem, flush=True)
    return nc


def make_inputs(inputs, b, consts):
    f = np.float32
    m = {}
    m["xs"] = np.ascontiguousarray(np.concatenate([inputs["ctx"][b], inputs["x"][b]], axis=0), dtype=f)
    ccv = np.stack([inputs["c"][b], inputs["c_ctx"]], axis=-1)
    m["cc"] = np.ascontiguousarray(ccv.reshape(16, 128, 2).transpose(1, 0, 2).reshape(128, 32), dtype=f)
    for k in ("w_mod", "b_mod", "w_in", "w_uq", "w_ukv", "w_br_a", "w_br_b", "w_out", "ln1_g", "ln1_b", "ln2_g", "ln2_b",
              "ffn_w1", "ffn_w3", "ffn_w2", "moe_w1", "moe_w3", "moe_w2", "gdn_norm"):
        m[k] = np.ascontiguousarray(inputs[k], dtype=f)
    m["convw"] = np.ascontiguousarray(inputs["conv_w"].reshape(2, 5, 48, 128).transpose(0, 3, 2, 1).reshape(2, 128, 240), dtype=f)
    m["a_log"] = np.ascontiguousarray(inputs["a_log"].reshape(2, 32), dtype=f)
    m["dt_bias"] = np.ascontiguousarray(inputs["dt_bias"].reshape(2, 32), dtype=f)
    qn = inputs["q_norm"].reshape(2, 4, 128).transpose(0, 2, 1)
    kn = inputs["kv_norm"].reshape(2, 4, 128).transpose(0, 2, 1)
    m["qkn"] = np.ascontiguousarray(np.concatenate([qn, kn], axis=2), dtype=f)
    m["router"] = np.ascontiguousarray(inputs["moe_router"][0].reshape(16, 128, 8).transpose(1, 0, 2).reshape(128, 128), dtype=f)
    m.update(consts)
    return m


_NC = None


def kernel(**inputs):
    global _NC
    inputs = {k: np.asarray(v) for k, v in inputs.items()}
    consts = host_consts()
    if _NC is None:
        _NC = build(bass.Bass("TRN2", target_bir_lowering=False))
    in_maps = [make_inputs(inputs, b, consts) for b in range(8)]
    res = run_bass_kernel_spmd(_NC, in_maps, core_ids=list(range(8)))
    return np.stack([np.asarray(r["out"], dtype=np.float32) for r in res.results], axis=0)
```

```python
import numpy as np
import concourse.bass as bass
import concourse.mybir as mybir
from concourse.bass_utils import run_bass_kernel_spmd
from contextlib import ExitStack

F32 = mybir.dt.float32
BF16 = mybir.dt.bfloat16
AF = mybir.ActivationFunctionType
ALU = mybir.AluOpType
AX = mybir.AxisListType

ENGS = ("pe", "act", "dve", "pool", "sp")
EPOCH = 12000
NDS = 12
DEPOCH = 1500

D = 2048
NT = 2304
NTILE = 18
CTXT = 2
H = 16
PROJ_W = 13440
FF_DENSE = 5632
FF_EXP = 7168
NEXP = 8
ALPHA = 4 ** 0.25
EPS = 1e-6
MLA_SCALE = 192 ** -0.5
NEG = -1.0e6


class Res:
    __slots__ = ("w", "rd", "name", "excl")

    def __init__(self, name="", excl=False):
        self.w = None
        self.rd = {}
        self.name = name
        self.excl = excl


class Prog:
    def __init__(self, nc, same_engine_sync=("act", "dve", "pool")):
        self.nc = nc
        self.ops = {e: [] for e in ENGS}
        self.cnt = {e: 0 for e in ENGS}
        self.dcnt = {e: 0 for e in ENGS}
        self.know = {e: {} for e in ENGS}
        self.last = {}
        self.semkeys = {}
        self.same = set(same_engine_sync)

    def _mkwaits(self, eng, deps):
        waits = {}
        kn = self.know[eng]
        for (sk, v, e) in deps:
            if e == eng and sk[0] == "c" and eng not in self.same:
                continue
            if kn.get(sk, 0) >= v:
                continue
            kn[sk] = v
            if waits.get(sk, 0) < v:
                waits[sk] = v
        return list(waits.items())

    def _deps(self, eng, reads, writes):
        deps = []
        for r in reads:
            if r.w is not None:
                deps.append(r.w)
            if r.excl:
                for sk, (v, e) in r.rd.items():
                    if e != eng:
                        deps.append((sk, v, e))
        for r in writes:
            if r.w is not None:
                deps.append(r.w)
            for sk, (v, e) in r.rd.items():
                deps.append((sk, v, e))
        return self._mkwaits(eng, deps)

    def _record(self, ident, reads, writes):
        sk, v, e = ident
        self.last[sk] = (v, e)
        for r in writes:
            r.w = ident
            r.rd = {}
        for r in reads:
            cur = r.rd.get(sk)
            if cur is None or cur[0] < v:
                r.rd[sk] = (v, e)

    def op(self, eng, fn, reads=(), writes=()):
        waits = self._deps(eng, reads, writes)
        k = self.cnt[eng]
        self.cnt[eng] = k + 1
        sk = ("c", eng, k // EPOCH)
        v = k % EPOCH + 1
        self.ops[eng].append((waits, fn, (sk, v)))
        self._record((sk, v, eng), reads, writes)

    def dma(self, q, out, in_, reads=(), writes=(), **kw):
        waits = self._deps(q, reads, writes)
        j = self.dcnt[q]
        self.dcnt[q] = j + 1
        slot = j % NDS
        use = j // NDS
        sk = ("d", q, slot, use // DEPOCH)
        v = 16 * (use % DEPOCH + 1)
        if use % DEPOCH > 0:
            pv = v - 16
            kn = self.know[q]
            if kn.get(sk, 0) < pv:
                kn[sk] = pv
                waits.append((sk, pv))

        def fn(eng, out=out, in_=in_, kw=kw):
            return eng.dma_start(out=out, in_=in_, **kw)
        self.ops[q].append((waits, fn, (sk, v)))
        self._record((sk, v, q), reads, writes)

    def barrier(self):
        deps = [(sk, v, e) for sk, (v, e) in self.last.items()]
        for eng in ENGS:
            kn = self.know[eng]
            waits = {}
            for (sk, v, e) in deps:
                if kn.get(sk, 0) >= v:
                    continue
                kn[sk] = v
                waits[sk] = v
            if waits:
                self.ops[eng].append((list(waits.items()), None, None))

    def emit(self):
        nc = self.nc
        self.barrier()
        allkeys = set()
        for e in ENGS:
            for waits, fn, inc in self.ops[e]:
                if inc is not None:
                    allkeys.add(inc[0])
        allkeys = sorted(allkeys)
        with ExitStack() as st:
            for i, sk in enumerate(allkeys):
                self.semkeys[sk] = st.enter_context(nc.semaphore("s%d" % i))
            block = st.enter_context(nc.Block())
            sem = self.semkeys

            def run(engname, eng):
                for waits, fn, inc in self.ops[engname]:
                    for sk, v in waits:
                        eng.wait_ge(sem[sk], v)
                    if fn is None:
                        continue
                    ins = fn(eng)
                    ins.then_inc(sem[inc[0]], 16 if inc[0][0] == "d" else 1)

            @block.tensor
            def _(e):
                run("pe", e)

            @block.scalar
            def _(e):
                run("act", e)

            @block.vector
            def _(e):
                run("dve", e)

            @block.gpsimd
            def _(e):
                run("pool", e)

            @block.sync
            def _(e):
                run("sp", e)
        return len(allkeys)


class Arena:
    def __init__(self, ap, nwords):
        self.ap = ap
        self.n = nwords
        self.off = 0
        self.marks = []

    def alloc(self, free_elems, dtype):
        words = (free_elems * (2 if dtype == BF16 else 4) + 3) // 4
        words = (words + 7) // 8 * 8
        assert self.off + words <= self.n, ("arena overflow", self.off, words, self.n)
        a = self.ap[:, self.off:self.off + words]
        self.off += words
        if dtype == BF16:
            a = a.bitcast(BF16)
        return a[:, 0:free_elems]

    def mark(self):
        self.marks.append(self.off)

    def release(self):
        self.off = self.marks.pop()


class Ring:
    def __init__(self, arena, n, free_elems, dtype):
        self.bufs = [(arena.alloc(free_elems, dtype), Res()) for _ in range(n)]
        self.i = 0

    def next(self):
        b = self.bufs[self.i % len(self.bufs)]
        self.i += 1
        return b


def host_consts():
    c = {}
    c["identf"] = np.eye(128, dtype=np.float32)
    idx = np.arange(128)
    same = (idx[:, None] // 64) == (idx[None, :] // 64)
    masks = np.zeros((12, 128, 128), np.float32)
    for d in range(2):
        aft = (idx[:, None] >= idx[None, :]) if d == 0 else (idx[:, None] <= idx[None, :])
        aft_s = (idx[:, None] > idx[None, :]) if d == 0 else (idx[:, None] < idx[None, :])
        masks[0 + d] = np.where((aft & same).T, 0.0, NEG)
        masks[2 + d] = np.where((aft_s & same), 0.0, NEG)
        masks[4 + d] = np.where((aft & same).T, 1.0, 0.0)
    masks[6] = np.where(idx[:, None] < 64, 1.0, 0.0) * np.ones((1, 128))
    masks[7] = np.where(idx[:, None] >= 64, 1.0, 0.0) * np.ones((1, 128))
    masks[8] = 1.0
    c["masks"] = masks.astype(np.float32)
    rows = 2048 // 64
    row = np.repeat(np.arange(rows), 64).astype(np.float32)
    col = np.tile(np.arange(64), rows).astype(np.float32)
    inv_freq = (10000.0 ** (-np.arange(16, dtype=np.float32) / 16)).astype(np.float32)
    cosT = np.zeros((64, 2048), np.float32)
    sinT = np.zeros((64, 2048), np.float32)
    for ax, pos in enumerate((row, col)):
        ang = (pos[None, :] * inv_freq[:, None]).astype(np.float32)
        for half in range(2):
            p0 = ax * 32 + half * 16
            cosT[p0:p0 + 16] = np.cos(ang)
            sinT[p0:p0 + 16] = np.sin(ang) * (-1.0 if half == 0 else 1.0)
    c["ropec"] = np.stack([cosT, sinT]).astype(np.float32)
    return c


def build(nc, nlayers=2, dbg=()):
    def din(name, shape):
        return nc.dram_tensor(name, list(shape), F32, kind="ExternalInput").ap()

    def dscr(name, shape, dt=F32):
        kind = "ExternalOutput" if name in dbg else "Internal"
        return nc.dram_tensor(name, list(shape), dt, kind=kind).ap()

    xs_in = din("xs", [NT, D])
    cc = din("cc", [128, 32])
    w_mod = din("w_mod", [2, D, 6 * D]); b_mod = din("b_mod", [2, 6 * D])
    w_in = din("w_in", [2, D, PROJ_W])
    convw = din("convw", [2, 128, 48 * 5])
    a_log = din("a_log", [2, 32]); dt_bias = din("dt_bias", [2, 32])
    gdn_norm = din("gdn_norm", [2, 128])
    qkn = din("qkn", [2, 128, 8])
    w_uq = din("w_uq", [2, 512, 3072]); w_ukv = din("w_ukv", [2, 512, 4096])
    w_br_a = din("w_br_a", [2, D, D]); w_br_b = din("w_br_b", [2, D, D]); w_out = din("w_out", [2, D, D])
    ln1_g = din("ln1_g", [2, D]); ln1_b = din("ln1_b", [2, D]); ln2_g = din("ln2_g", [2, D]); ln2_b = din("ln2_b", [2, D])
    ffn_w1 = din("ffn_w1", [1, D, FF_DENSE]); ffn_w3 = din("ffn_w3", [1, D, FF_DENSE]); ffn_w2 = din("ffn_w2", [1, FF_DENSE, D])
    router = din("router", [128, 16 * 8])
    moe_w1 = din("moe_w1", [1, NEXP, D, FF_EXP]); moe_w3 = din("moe_w3", [1, NEXP, D, FF_EXP]); moe_w2 = din("moe_w2", [1, NEXP, FF_EXP, D])
    identf_d = din("identf", [128, 128]); masks_d = din("masks", [12, 128, 128]); ropec_d = din("ropec", [2, 64, 2048])
    out_d = nc.dram_tensor("out", [2048, D], F32, kind="ExternalOutput").ap()

    XS = dscr("XS", [NT, D])
    MODV = dscr("MODV", [2, 2, 6 * D])
    QT = dscr("QT", [H, 128, NT], BF16); KT = dscr("KT", [H, 128, NT], BF16); KT32 = dscr("KT32", [H, 128, NT])
    KM = dscr("KM", [NT, H, 128], BF16); VM = dscr("VM", [NT, H, 128], BF16)
    ZS = dscr("ZS", [NT, D])
    GT = dscr("GT", [32, 128, NT], BF16)
    QNT = dscr("QNT", [H, 128, NT], BF16); QRT = dscr("QRT", [H, 64, NT], BF16)
    KNT = dscr("KNT", [H, 128, NT], BF16); VM2 = dscr("VM2", [H, NT, 128], BF16)
    OFB = dscr("OFB", [2, NT, H * 128])
    YAT = dscr("YAT", [H, 128, NT], BF16); YBT = dscr("YBT", [H, 128, NT], BF16)
    ATS = dscr("ATS", [NEXP, 56, 128, NT], BF16)
    FO = dscr("FO", [NT, D])

    NW = 47000
    with ExitStack() as st:
        arena_t = st.enter_context(nc.sbuf_tensor("arena", [128, NW], F32))
        ps_all = st.enter_context(nc.psum_tensor("psall", [128, 4096], F32))
        A = Arena(arena_t[:], NW)
        P = Prog(nc)
        ps = [ps_all[:, b * 512:(b + 1) * 512] for b in range(8)]
        psb = [ps_all[:, b * 512:(b + 1) * 512].bitcast(BF16) for b in range(8)]
        rps = [Res("ps%d" % b, excl=True) for b in range(8)]

        identf = A.alloc(128, F32); identb = A.alloc(128, BF16); r_const = Res("const")
        masks = [A.alloc(128, F32) for _ in range(9)]
        onecol = A.alloc(1, F32); epscol = A.alloc(1, F32)
        P.dma("sp", identf, identf_d, writes=[r_const])
        for i in range(9):
            P.dma("sp", masks[i], masks_d[i], writes=[r_const])
        P.op("dve", lambda e: e.tensor_copy(identb, identf), reads=[r_const], writes=[r_const])
        P.op("dve", lambda e: e.memset(onecol, 1.0), writes=[r_const])
        P.op("dve", lambda e: e.memset(epscol, EPS), writes=[r_const])
        NEGM_AT, NEGM_P, MCUM, MCH, ONESF = masks[0:2], masks[2:4], masks[4:6], masks[6:8], masks[8]
        g_all = A.alloc(NTILE * 32, F32).rearrange("p (t c) -> p t c", c=32); r_gall = Res("gall")
        lnb_all = A.alloc(NTILE * 32, F32).rearrange("p (t c) -> p t c", c=32)
        KRT = A.alloc(NT, BF16); r_krt = Res("krt")
        comb = A.alloc(16 * 8, F32).rearrange("p (t e) -> p t e", e=8); r_comb = Res("comb")
        P.barrier()

        def release():
            A.release()
            P.barrier()

        def mm(out, lhsT, rhs, start, stop, reads, writes):
            P.op("pe", lambda e: e.matmul(out, lhsT, rhs, start=start, stop=stop), reads=reads, writes=writes)

        def tr(out, in_, reads, writes, f32=False):
            idn = identf if f32 else identb
            P.op("pe", lambda e: e.transpose(out, in_, idn), reads=list(reads) + [r_const], writes=writes)

        def act(out, in_, func, reads, writes, **kw):
            P.op("act", lambda e: e.activation(out, in_, func, **kw), reads=reads, writes=writes)

        def tt(eng, out, in0, in1, op, reads, writes):
            P.op(eng, lambda e: e.tensor_tensor(out, in0, in1, op), reads=reads, writes=writes)

        def ts(eng, out, in0, s1, s2, op0, op1, reads, writes):
            if op1 is None:
                P.op(eng, lambda e: e.tensor_scalar(out, in0, s1, None, op0), reads=reads, writes=writes)
            else:
                P.op(eng, lambda e: e.tensor_scalar(out, in0, s1, s2, op0, op1), reads=reads, writes=writes)

        def stt(eng, out, in0, scalar, in1, op0, op1, reads, writes):
            P.op(eng, lambda e: e.scalar_tensor_tensor(out, in0, scalar, in1, op0, op1), reads=reads, writes=writes)

        def wcast(dst3, src2, rw):
            P.dma("pool", dst3, src2.rearrange("(k p) n -> p k n", p=128), writes=[rw])

        TOKG = [(0, 256), (256, 512), (768, 512), (1280, 512), (1792, 512)]

        def phase_init():
            A.mark()
            for t in range(NTILE):
                P.dma("sp", XS[t * 128:(t + 1) * 128, :], xs_in[t * 128:(t + 1) * 128, :])
            cct = A.alloc(32, F32); r_cc = Res()
            P.dma("sp", cct, cc, writes=[r_cc])
            act(cct, cct, AF.Silu, [r_cc], [r_cc])
            cv = cct.rearrange("p (k t) -> p k t", t=2)
            wr = Ring(A, 2, 16 * 512, F32)
            br = Ring(A, 2, 512, F32)
            orr = Ring(A, 2, 512, F32)
            i = 0
            for l in range(2):
                for nb in range(24):
                    wt, rw = wr.next()
                    wt3 = wt.rearrange("p (k n) -> p k n", k=16)
                    P.dma("sp", wt3, w_mod[l, :, nb * 512:(nb + 1) * 512].rearrange("(k p) n -> p k n", p=128), writes=[rw])
                    bt, rb = br.next()
                    P.dma("sp", bt[0:2, :], b_mod[l, nb * 512:(nb + 1) * 512].partition_broadcast(2), writes=[rb])
                    bank = i % 2
                    i += 1
                    for k in range(16):
                        mm(ps[bank][0:2, :], cv[:, k, :], wt3[:, k, :], k == 0, k == 15, [r_cc, rw], [rps[bank]])
                    ot, ro = orr.next()
                    tt("dve", ot[0:2, :], ps[bank][0:2, :], bt[0:2, :], ALU.add, [rps[bank], rb], [ro])
                    if nb // 4 in (1, 4):
                        ts("dve", ot[0:2, :], ot[0:2, :], 1.0, None, ALU.add, None, [ro], [ro])
                    P.dma("sp", MODV[l, :, nb * 512:(nb + 1) * 512], ot[0:2, :], reads=[ro])
            release()

        def ln_mod_T(l, which, tiles, hT, r_hT, col0, extra=None):
            shi, sci = (0, 1) if which == 0 else (3, 4)
            bA, bB = [], []
            r_b = Res()
            for r in (0, 1):
                a = A.alloc(2048, F32); b = A.alloc(2048, F32)
                P.dma("sp", a, MODV[l, r, sci * 2048:(sci + 1) * 2048].partition_broadcast(128), writes=[r_b])
                P.dma("sp", b, MODV[l, r, shi * 2048:(shi + 1) * 2048].partition_broadcast(128), writes=[r_b])
                bA.append(a); bB.append(b)
            xr = Ring(A, 2, 2048, F32)
            hbr = Ring(A, 2, 2048, BF16)
            sr = Ring(A, 2, 32, F32)
            for t in tiles:
                r = 1 if t < CTXT else 0
                xt, rx = xr.next()
                P.dma("sp", xt, XS[t * 128:(t + 1) * 128, :], writes=[rx])
                s, rs = sr.next()
                st6 = s[:, 0:24].rearrange("p (c s) -> p c s", s=6)
                xv = xt.rearrange("p (c f) -> p c f", f=512)
                for c in range(4):
                    P.op("dve", lambda e, c=c, st6=st6, xv=xv: e.bn_stats(st6[:, c, :], xv[:, c, :]), reads=[rx], writes=[rs])
                P.op("dve", lambda e, s=s, st6=st6: e.bn_aggr(s[:, 24:26], st6), reads=[rs], writes=[rs])
                ts("dve", s[:, 26:27], s[:, 25:26], EPS, None, ALU.add, None, [rs], [rs])
                act(s[:, 26:27], s[:, 26:27], AF.Sqrt, [rs], [rs])
                P.op("dve", lambda e, s=s: e.reciprocal(s[:, 26:27], s[:, 26:27]), reads=[rs], writes=[rs])
                ts("dve", xt, xt, s[:, 24:25], s[:, 26:27], ALU.subtract, ALU.mult, [rx, rs], [rx])
                tt("pool", xt, xt, bA[r], ALU.mult, [rx, r_b], [rx])
                tt("dve", xt, xt, bB[r], ALU.add, [rx, r_b], [rx])
                hb, rhb = hbr.next()
                act(hb, xt, AF.Copy, [rx], [rhb])
                if extra is not None:
                    extra(t, xt, rx)
                c = col0(t)
                for g in range(4):
                    bank = 6 + (g % 2)
                    for j in range(4):
                        k = g * 4 + j
                        tr(psb[bank][:, j * 128:(j + 1) * 128], hb[:, k * 128:(k + 1) * 128], [rhb], [rps[bank]])
                    P.op("dve" if g % 2 else "act",
                         (lambda e, g=g, bank=bank, c=c: e.tensor_copy(hT[:, g * 4:(g + 1) * 4, c:c + 128], psb[bank][:, 0:512].rearrange("p (j t) -> p j t", j=4))) if g % 2 else
                         (lambda e, g=g, bank=bank, c=c: e.activation(hT[:, g * 4:(g + 1) * 4, c:c + 128], psb[bank][:, 0:512].rearrange("p (j t) -> p j t", j=4), AF.Copy)),
                         reads=[rps[bank]], writes=[r_hT])

        def phase_proj(l):
            A.mark()
            DQK = [A.alloc(4 * NT, BF16).rearrange("p (k t) -> p k t", k=4) for _ in range(2)]; r_dqk = [Res(), Res()]
            qk = A.alloc(8, F32); r_qk = Res()
            P.dma("sp", qk, qkn[l], writes=[r_qk])
            RC = {}

            def load_rope():
                ropec = A.alloc(2 * 2048, F32); RC["r"] = Res()
                RC["COS"] = ropec[0:64, 0:2048]; RC["SINS"] = ropec[0:64, 2048:4096]
                P.dma("sp", RC["COS"], ropec_d[0], writes=[RC["r"]])
                P.dma("sp", RC["SINS"], ropec_d[1], writes=[RC["r"]])
                RC["t12"] = Ring(A, 2, 1024, F32)
            A.mark()
            hT = A.alloc(16 * NT, BF16).rearrange("p (k t) -> p k t", k=16); r_hT = Res("hT")
            A.mark()
            ln_mod_T(l, 0, range(NTILE), hT, r_hT, lambda t: t * 128)
            release()
            wfm = Ring(A, 2, 16 * 128, BF16)
            cw = A.alloc(240, F32); r_cw = Res()
            P.dma("sp", cw, convw[l], writes=[r_cw])
            cwv = cw.rearrange("p (c j) -> p c j", j=5)
            W = w_in[l]

            def lin_fm(wt3, rw, kc, m, tok0, ntok, bank, inT, r_in, m0=0, po=0):
                for k in range(kc):
                    mm(ps[bank][po:po + m, 0:ntok], wt3[:, k, m0:m0 + m], inT[:, k, tok0:tok0 + ntok], k == 0, k == kc - 1, [rw, r_in], [rps[bank]])

            A.mark()
            rawr = Ring(A, 1, 2312, F32)
            for rb, rr in rawr.bufs:
                P.op("dve", lambda e, rb=rb: e.memset(rb, 0.0), writes=[rr])
            accr = Ring(A, 1, NT, F32)
            sqr = Ring(A, 1, NT, F32)
            rnr = Ring(A, 2, 512, F32)
            fmr = Ring(A, 2, NT, BF16)
            tmr = Ring(A, 1, NT, BF16)
            bi = 0
            for ch in range(48):
                kind, h = ch // 16, ch % 16
                wt, rw = wfm.next()
                wt3 = wt.rearrange("p (k n) -> p k n", k=16)
                wcast(wt3, W[:, ch * 128:(ch + 1) * 128], rw)
                raw, rr = rawr.next()
                for gi, (t0, n) in enumerate(TOKG):
                    bank = bi % 4; bi += 1
                    lin_fm(wt3, rw, 16, 128, t0, n, bank, hT, r_hT)
                    dst = raw[:, 2:258] if gi == 0 else raw[:, 262 + (t0 - 256):262 + (t0 - 256) + n]
                    act(dst, ps[bank][:, 0:n], AF.Copy, [rps[bank]], [rr])
                acc, ra = accr.next()
                for (ro, n, oo) in ((2, 256, 0), (262, 2048, 256)):
                    ts("dve", acc[:, oo:oo + n], raw[:, ro - 2:ro - 2 + n], cwv[:, ch, 0:1], None, ALU.mult, None, [rr, r_cw], [ra])
                    for j in range(1, 5):
                        stt("dve", acc[:, oo:oo + n], raw[:, ro - 2 + j:ro - 2 + j + n], cwv[:, ch, j:j + 1], acc[:, oo:oo + n], ALU.mult, ALU.add, [rr, r_cw, ra], [ra])
                fm, rf = fmr.next()
                if kind == 2:
                    act(fm, acc, AF.Silu, [ra], [rf])
                else:
                    act(acc, acc, AF.Silu, [ra], [ra])
                    sq, rs = sqr.next()
                    act(sq, acc, AF.Square, [ra], [rs])
                    for (t0, n) in TOKG:
                        bank = bi % 4; bi += 1
                        mm(ps[bank][:, 0:n], ONESF, sq[:, t0:t0 + n], True, True, [rs, r_const], [rps[bank]])
                        rn, rrn = rnr.next()
                        act(rn[:, 0:n], ps[bank][:, 0:n], AF.Sqrt, [rps[bank], r_const], [rrn], bias=epscol)
                        P.op("dve", lambda e, rn=rn, n=n: e.reciprocal(rn[:, 0:n], rn[:, 0:n]), reads=[rrn], writes=[rrn])
                        if kind == 0:
                            stt("dve", fm[:, t0:t0 + n], acc[:, t0:t0 + n], 128 ** -0.5, rn[:, 0:n], ALU.mult, ALU.mult, [ra, rrn], [rf])
                        else:
                            tt("dve", sq[:, t0:t0 + n], acc[:, t0:t0 + n], rn[:, 0:n], ALU.mult, [ra, rrn, rs], [rs])
                            act(fm[:, t0:t0 + n], sq[:, t0:t0 + n], AF.Copy, [rs], [rf])
                    P.dma("sp", (QT if kind == 0 else KT)[h], fm, reads=[rf])
                    if kind == 1:
                        P.dma("sp", KT32[h], sq, reads=[rs])
                if kind >= 1:
                    tm, rt = tmr.next()
                    tm3 = tm.rearrange("p (t d) -> p t d", d=128)
                    for g in range(5):
                        bank = 6 + (g % 2)
                        nn = 4 if g < 4 else 2
                        for j in range(nn):
                            t = g * 4 + j
                            tr(psb[bank][:, j * 128:(j + 1) * 128], fm[:, t * 128:(t + 1) * 128], [rf], [rps[bank]])
                        src = psb[bank][:, 0:nn * 128].rearrange("p (j t) -> p j t", j=nn)
                        if g % 2:
                            P.op("dve", lambda e, tm3=tm3, g=g, nn=nn, src=src: e.tensor_copy(tm3[:, g * 4:g * 4 + nn, :], src), reads=[rps[bank]], writes=[rt])
                        else:
                            act(tm3[:, g * 4:g * 4 + nn, :], src, AF.Copy, [rps[bank]], [rt])
                    dst = (KM if kind == 1 else VM).rearrange("(t p) h d -> p t h d", p=128)[:, :, h, :]
                    P.dma("sp", dst, tm3, reads=[rt])
            release()

            A.mark()
            wtm = Ring(A, 2, 16 * 512, BF16)
            zr = Ring(A, 3, 512, F32)
            for nb in range(4):
                wt, rw = wtm.next()
                wt3 = wt.rearrange("p (k n) -> p k n", k=16)
                wcast(wt3, W[:, 6144 + nb * 512:6144 + (nb + 1) * 512], rw)
                for t in range(NTILE):
                    bank = bi % 4; bi += 1
                    for k in range(16):
                        mm(ps[bank], hT[:, k, t * 128:(t + 1) * 128], wt3[:, k, :], k == 0, k == 15, [rw, r_hT], [rps[bank]])
                    z, rz = zr.next()
                    act(z, ps[bank], AF.Silu, [rps[bank]], [rz])
                    P.dma("sp", ZS[t * 128:(t + 1) * 128, nb * 512:(nb + 1) * 512], z, reads=[rz])

            wt, rw = wtm.next()
            wab = wt[:, 0:16 * 64].rearrange("p (k n) -> p k n", k=16)
            wcast(wab, W[:, 8192:8256], rw)
            negA = A.alloc(32, F32); dtb = A.alloc(32, F32); r_ab = Res()
            P.dma("sp", negA, a_log[l].partition_broadcast(128), writes=[r_ab])
            P.dma("sp", dtb, dt_bias[l].partition_broadcast(128), writes=[r_ab])
            act(negA, negA, AF.Exp, [r_ab], [r_ab])
            ts("dve", negA, negA, -1.0, None, ALU.mult, None, [r_ab], [r_ab])
            tmpr = Ring(A, 2, 64, F32)
            for t in range(NTILE):
                bank = bi % 4; bi += 1
                for k in range(16):
                    mm(ps[bank][:, 0:64], hT[:, k, t * 128:(t + 1) * 128], wab[:, k, :], k == 0, k == 15, [rw, r_hT], [rps[bank]])
                tp, rtp = tmpr.next()
                tt("dve", tp[:, 0:32], ps[bank][:, 0:32], dtb, ALU.add, [rps[bank], r_ab], [rtp])
                act(tp[:, 0:32], tp[:, 0:32], AF.Exp, [rtp], [rtp])
                act(tp[:, 32:64], ps[bank][:, 32:64], AF.Exp, [rps[bank], rtp], [rtp], scale=-1.0)
                act(tp, tp, AF.Ln, [rtp, r_const], [rtp], bias=onecol)
                tt("dve", g_all[:, t, :], tp[:, 0:32], negA, ALU.mult, [rtp, r_ab], [r_gall])
                ts("dve", lnb_all[:, t, :], tp[:, 32:64], -1.0, None, ALU.mult, None, [rtp], [r_gall])

            xnr = Ring(A, 2, 512, BF16)
            ssr = Ring(A, 2, 8, F32)
            junk = A.alloc(512, F32); r_junk = Res()
            for which in range(2):
                wt, rw = wtm.next()
                wt3 = wt.rearrange("p (k n) -> p k n", k=16)
                wcast(wt3, W[:, 8256 + which * 512:8256 + (which + 1) * 512], rw)
                for t in range(NTILE):
                    bank = bi % 4; bi += 1
                    for k in range(16):
                        mm(ps[bank], hT[:, k, t * 128:(t + 1) * 128], wt3[:, k, :], k == 0, k == 15, [rw, r_hT], [rps[bank]])
                    s, rs = ssr.next()
                    act(junk, ps[bank], AF.Square, [rps[bank]], [r_junk, rs], accum_out=s[:, 0:1])
                    ts("dve", s[:, 1:2], s[:, 0:1], 1.0 / 512, EPS, ALU.mult, ALU.add, [rs], [rs])
                    act(s[:, 1:2], s[:, 1:2], AF.Sqrt, [rs], [rs])
                    P.op("dve", lambda e, s=s: e.reciprocal(s[:, 1:2], s[:, 1:2]), reads=[rs], writes=[rs])
                    xn, rxn = xnr.next()
                    act(xn, ps[bank], AF.Copy, [rps[bank], rs], [rxn], scale=s[:, 1:2])
                    tb = 6 + (t % 2)
                    for j in range(4):
                        tr(psb[tb][:, j * 128:(j + 1) * 128], xn[:, j * 128:(j + 1) * 128], [rxn], [rps[tb]])
                    for j in range(4):
                        ts("dve", DQK[which][:, j, t * 128:(t + 1) * 128], psb[tb][:, j * 128:(j + 1) * 128], qk[:, which * 4 + j:which * 4 + j + 1], None, ALU.mult, None, [rps[tb], r_qk], [r_dqk[which]])

            release()
            load_rope()
            PERM = [(0, 16), (16, 0), (32, 48), (48, 32)]
            wt, rw = wfm.next()
            wkr = wt.rearrange("p (k n) -> p k n", k=16)
            P.dma("pool", wkr[:, :, 0:64], W[:, 9280:9344].rearrange("(k p) n -> p k n", p=128), writes=[rw])
            for (dc, sc) in PERM:
                P.dma("pool", wkr[:, :, 64 + dc:64 + dc + 16], W[:, 9280 + sc:9280 + sc + 16].rearrange("(k p) n -> p k n", p=128), writes=[rw])

            def rope_evac(bank_a, bank_b, n, t0, dst, r_dst, scale):
                if t0 == 0:
                    act(dst[0:64, t0:t0 + n], ps[bank_a][0:64, 0:n], AF.Copy, [rps[bank_a]], [r_dst], scale=scale)
                    return
                tb, rtb = RC["t12"].next()
                COS, SINS, r_rope = RC["COS"], RC["SINS"], RC["r"]
                c0 = t0 - 256
                stt("dve", tb[0:64, 0:n], ps[bank_a][0:64, 0:n], scale, COS[:, c0:c0 + n], ALU.mult, ALU.mult, [rps[bank_a], r_rope], [rtb])
                stt("dve", tb[0:64, 512:512 + n], ps[bank_b][0:64, 0:n], scale, SINS[:, c0:c0 + n], ALU.mult, ALU.mult, [rps[bank_b], r_rope, rtb], [rtb])
                tt("dve", dst[0:64, t0:t0 + n], tb[0:64, 0:n], tb[0:64, 512:512 + n], ALU.add, [rtb], [r_dst])

            for (t0, n) in TOKG:
                ba = bi % 4; bi += 1
                bb = bi % 4; bi += 1
                lin_fm(wkr, rw, 16, 64, t0, n, ba, hT, r_hT, m0=0)
                lin_fm(wkr, rw, 16, 64, t0, n, bb, hT, r_hT, m0=64)
                rope_evac(ba, bb, n, t0, KRT, r_krt, 1.0)

            gr = Ring(A, 2, NT, BF16)
            for ch in range(32):
                wt, rw = wfm.next()
                wt3 = wt.rearrange("p (k n) -> p k n", k=16)
                wcast(wt3, W[:, 9344 + ch * 128:9344 + (ch + 1) * 128], rw)
                gt, rg = gr.next()
                for (t0, n) in TOKG:
                    bank = bi % 4; bi += 1
                    lin_fm(wt3, rw, 16, 128, t0, n, bank, hT, r_hT)
                    act(gt[:, t0:t0 + n], ps[bank][:, 0:n], AF.Sigmoid, [rps[bank]], [rg])
                P.dma("sp", GT[ch], gt, reads=[rg])

            release()
            load_rope()
            wqr = Ring(A, 2, 4 * 256, BF16)
            wkr2 = Ring(A, 2, 4 * 256, BF16)
            str_ = Ring(A, 3, NT, BF16)
            vstr = Ring(A, 2, NT, BF16)
            for h in range(H):
                wq, rwq = wqr.next()
                wq3 = wq.rearrange("p (k n) -> p k n", k=4)
                src = w_uq[l]
                P.dma("pool", wq3[:, :, 0:192], src[:, h * 192:(h + 1) * 192].rearrange("(k p) n -> p k n", p=128), writes=[rwq])
                for (dc, sc) in PERM:
                    P.dma("pool", wq3[:, :, 192 + dc:192 + dc + 16], src[:, h * 192 + 128 + sc:h * 192 + 128 + sc + 16].rearrange("(k p) n -> p k n", p=128), writes=[rwq])
                wk, rwk = wkr2.next()
                wk3 = wk.rearrange("p (k n) -> p k n", k=4)
                P.dma("pool", wk3, w_ukv[l][:, h * 256:(h + 1) * 256].rearrange("(k p) n -> p k n", p=128), writes=[rwk])
                sqn, rsqn = str_.next()
                sqr_, rsqr = str_.next()
                skn, rskn = str_.next()
                for (t0, n) in TOKG:
                    bank = bi % 4; bi += 1
                    lin_fm(wq3, rwq, 4, 128, t0, n, bank, DQK[0], r_dqk[0])
                    act(sqn[:, t0:t0 + n], ps[bank][:, 0:n], AF.Copy, [rps[bank]], [rsqn], scale=MLA_SCALE)
                    ba = bi % 4; bi += 1
                    bb = bi % 4; bi += 1
                    lin_fm(wq3, rwq, 4, 64, t0, n, ba, DQK[0], r_dqk[0], m0=128)
                    lin_fm(wq3, rwq, 4, 64, t0, n, bb, DQK[0], r_dqk[0], m0=192)
                    rope_evac(ba, bb, n, t0, sqr_, rsqr, MLA_SCALE)
                    bank = bi % 4; bi += 1
                    lin_fm(wk3, rwk, 4, 128, t0, n, bank, DQK[1], r_dqk[1])
                    P.op("dve", lambda e, skn=skn, t0=t0, n=n, bank=bank: e.tensor_copy(skn[:, t0:t0 + n], ps[bank][:, 0:n]), reads=[rps[bank]], writes=[rskn])
                P.dma("sp", QNT[h], sqn, reads=[rsqn])
                P.dma("sp", QRT[h], sqr_[0:64, :], reads=[rsqr])
                P.dma("sp", KNT[h], skn, reads=[rskn])
                vs, rvs = vstr.next()
                vs3 = vs.rearrange("p (t d) -> p t d", d=128)
                for t in range(NTILE):
                    bank = 4 + (t % 2)
                    for k in range(4):
                        mm(ps[bank][:, 0:128], DQK[1][:, k, t * 128:(t + 1) * 128], wk3[:, k, 128:256], k == 0, k == 3, [rwk, r_dqk[1]], [rps[bank]])
                    act(vs3[:, t, :], ps[bank][:, 0:128], AF.Copy, [rps[bank]], [rvs])
                P.dma("sp", VM2[h].rearrange("(t p) d -> p t d", p=128), vs3, reads=[rvs])
            release()

        def phase_mla(l):
            A.mark()
            qtiles = list(range(NTILE)) if l == 0 else list(range(CTXT, NTILE))
            kn_ = Ring(A, 2, NT, BF16); qn_ = Ring(A, 2, NT, BF16); qr_ = Ring(A, 2, NT, BF16); v_ = Ring(A, 2, NT, BF16)
            p_ = Ring(A, 2, NT, BF16); pt_ = Ring(A, 2, NT, BF16); yb_ = Ring(A, 2, NT, BF16)
            sm_ = Ring(A, 3, 8, F32); ob_ = Ring(A, 2, 128, BF16)
            ei = 0
            for h in range(H):
                knt, rkn = kn_.next(); qnt, rqn = qn_.next(); qrt, rqr = qr_.next(); v, rv = v_.next()
                P.dma("sp", knt, KNT[h], writes=[rkn])
                P.dma("sp", qnt, QNT[h], writes=[rqn])
                P.dma("sp", qrt[0:64, :], QRT[h], writes=[rqr])
                v3 = v.rearrange("p (t d) -> p t d", d=128)
                P.dma("sp", v3, VM2[h].rearrange("(t p) d -> p t d", p=128), writes=[rv])
                yb, ryb = yb_.next()
                for qt in qtiles:
                    qc = slice(qt * 128, (qt + 1) * 128)
                    nk = 256 if qt < CTXT else NT
                    groups = [(0, 256)] if qt < CTXT else [(0, 512), (512, 512), (1024, 512), (1536, 512), (2048, 256)]
                    for gi, (k0, n) in enumerate(groups):
                        mm(ps[gi][:, 0:n], qnt[:, qc], knt[:, k0:k0 + n], True, False, [rqn, rkn], [rps[gi]])
                        mm(ps[gi][:, 0:n], qrt[0:64, qc], KRT[0:64, k0:k0 + n], False, True, [rqr, r_krt], [rps[gi]])
                    S = ps_all[:, 0:nk]
                    rS = [rps[gi] for gi in range(len(groups))]
                    s, rs = sm_.next()
                    P.op("dve", lambda e, s=s, S=S: e.reduce_max(s[:, 0:1], S, AX.X), reads=rS, writes=[rs])
                    ts("dve", s[:, 1:2], s[:, 0:1], -1.0, None, ALU.mult, None, [rs], [rs])
                    p, rp = p_.next()
                    act(p[:, 0:nk], S, AF.Exp, rS + [rs], [rp, rs], bias=s[:, 1:2], scale=1.0, accum_out=s[:, 2:3])
                    P.op("dve", lambda e, s=s: e.reciprocal(s[:, 3:4], s[:, 2:3]), reads=[rs], writes=[rs])
                    pt, rpt = pt_.next()
                    pt3 = pt.rearrange("p (t q) -> p t q", q=128)
                    nkt = nk // 128
                    for g in range((nkt + 3) // 4):
                        bank = 5 + (g % 2)
                        nn = min(4, nkt - 4 * g)
                        for j in range(nn):
                            kt = 4 * g + j
                            tr(psb[bank][:, j * 128:(j + 1) * 128], p[:, kt * 128:(kt + 1) * 128], [rp], [rps[bank]])
                        src = psb[bank][:, 0:nn * 128].rearrange("p (j t) -> p j t", j=nn)
                        ei += 1
                        if ei % 2:
                            P.op("dve", lambda e, pt3=pt3, g=g, nn=nn, src=src: e.tensor_copy(pt3[:, 4 * g:4 * g + nn, :], src), reads=[rps[bank]], writes=[rpt])
                        else:
                            act(pt3[:, 4 * g:4 * g + nn, :], src, AF.Copy, [rps[bank]], [rpt])
                    for kt in range(nkt):
                        mm(ps[7][:, 0:128], pt3[:, kt, :], v3[:, kt, :], kt == 0, kt == nkt - 1, [rpt, rv], [rps[7]])
                    ob, rob = ob_.next()
                    act(ob, ps[7][:, 0:128], AF.Copy, [rps[7], rs], [rob], scale=s[:, 3:4])
                    tr(psb[7][:, 512:640], ob, [rob], [rps[7]])
                    P.op("dve", lambda e, yb=yb, qc=qc: e.tensor_copy(yb[:, qc], psb[7][:, 512:640]), reads=[rps[7]], writes=[ryb])
                if l == 1:
                    P.op("dve", lambda e, yb=yb: e.memset(yb[:, 0:256], 0.0), writes=[ryb])
                P.dma("sp", YBT[h], yb, reads=[ryb])
            release()

        def phase_gdn(l):
            A.mark()
            G = 4
            S32 = A.alloc(32 * 128, F32).rearrange("p (c v) -> p c v", v=128)
            Sbf = A.alloc(32 * 128, BF16).rearrange("p (c v) -> p c v", v=128)
            r_S = [Res() for _ in range(32)]
            P.op("dve", lambda e: e.memset(S32, 0.0), writes=r_S)
            P.op("dve", lambda e: e.memset(Sbf, 0.0), writes=r_S)
            order = {0: list(range(NTILE)), 1: [1, 0] + list(range(NTILE - 1, 1, -1))}
            qT_ = Ring(A, 2, 2048, BF16); kM_ = Ring(A, 2, 2048, BF16); vM_ = Ring(A, 2, 2048, BF16)
            kF_ = Ring(A, 2, 2048, F32)
            o_ = Ring(A, 2, 2048, F32)
            sm_ = Ring(A, 2, 16 * 10, F32)
            mf_ = Ring(A, 96, 128, F32)
            LLB = [[{k: (A.alloc(128, BF16), Res()) for k in ("AT", "wT", "vnew", "kd0", "kd1")} for _ in range(G)] for _ in range(2)]
            LLF = [[{k: (A.alloc(128, F32), Res()) for k in ("u", "tmp")} for _ in range(G)] for _ in range(2)]
            for b_, r_ in mf_.bufs:
                P.op("dve", lambda e, b_=b_: e.memset(b_, 0.0), writes=[r_])
            for par in LLB + LLF:
                for dct in par:
                    for (b_, r_) in dct.values():
                        P.op("dve", lambda e, b_=b_: e.memset(b_, 0.0), writes=[r_])
            slots = [(ps[b][:, 0:128], rps[b]) for b in range(8)]
            sl = [0]

            def slot():
                x = slots[sl[0] % 8]
                sl[0] += 1
                return x

            def cp_act(dst, rdst, src, rsrc):
                act(dst, src, AF.Copy, [rsrc], [rdst])

            def cp_dve(dst, rdst, src, rsrc):
                P.op("dve", lambda e: e.tensor_copy(dst, src), reads=[rsrc], writes=[rdst])
            gpar = [0]
            for s in range(GDN_STEPS):
                for d in (0, 1):
                    t = order[d][s]
                    tc = slice(t * 128, (t + 1) * 128)
                    qTt, rqT = qT_.next(); kMt, rkM = kM_.next(); vMt, rvM = vM_.next()
                    qT3 = qTt.rearrange("p (h t) -> p h t", h=16)
                    kM3 = kMt.rearrange("p (h t) -> p h t", h=16); vM3 = vMt.rearrange("p (h t) -> p h t", h=16)
                    P.dma("sp", qT3, QT[:, :, tc].rearrange("h p t -> p h t"), writes=[rqT])
                    kFt, rkF = kF_.next()
                    kF3 = kFt.rearrange("p (h t) -> p h t", h=16)
                    P.dma("sp", kF3, KT32[:, :, tc].rearrange("h p t -> p h t"), writes=[rkF])
                    P.dma("sp", kMt, KM[tc].rearrange("p h d -> p (h d)"), writes=[rkM])
                    P.dma("sp", vMt, VM[tc].rearrange("p h d -> p (h d)"), writes=[rvM])
                    sm, rsm = sm_.next()
                    smv = sm.rearrange("p (c h) -> p c h", h=16)
                    GC, GB, NGC, EGC, EGB, KD0, EGL0, EGL1, EB, KD1 = [smv[:, i, :] for i in range(10)]
                    gsl = g_all[:, t, d * 16:(d + 1) * 16]
                    pg, rpg = slot()
                    mm(pg[:, 0:16], MCUM[d], gsl, True, True, [r_const, r_gall], [rpg])
                    mm(pg[:, 16:32], MCH[0], gsl, True, True, [r_const, r_gall], [rpg])
                    mm(pg[:, 32:48], MCH[1], gsl, True, True, [r_const, r_gall], [rpg])
                    P.op("dve", lambda e, GC=GC, pg=pg: e.tensor_copy(GC, pg[:, 0:16]), reads=[rpg], writes=[rsm])
                    tt("dve", GB, GC, lnb_all[:, t, d * 16:(d + 1) * 16], ALU.add, [rsm, r_gall], [rsm])
                    ts("dve", NGC, GC, -1.0, None, ALU.mult, None, [rsm], [rsm])
                    P.op("dve", lambda e, KD0=KD0: e.memset(KD0, 0.0), writes=[rsm])
                    P.op("dve", lambda e, KD1=KD1: e.memset(KD1, 0.0), writes=[rsm])
                    tt("dve", KD0[0:64, :], pg[0:64, 16:32], GC[0:64, :], ALU.subtract, [rpg, rsm], [rsm])
                    tt("dve", KD1[64:128, :], pg[64:128, 32:48], GC[64:128, :], ALU.subtract, [rpg, rsm], [rsm])
                    act(EGC, GC, AF.Exp, [rsm], [rsm])
                    act(EGB, GB, AF.Exp, [rsm], [rsm])
                    act(EB, lnb_all[:, t, d * 16:(d + 1) * 16], AF.Exp, [rsm, r_gall], [rsm])
                    act(KD0[0:64, :], KD0[0:64, :], AF.Exp, [rsm], [rsm])
                    act(KD1[64:128, :], KD1[64:128, :], AF.Exp, [rsm], [rsm])
                    act(EGL0, pg[:, 16:32], AF.Exp, [rpg, rsm], [rsm])
                    act(EGL1, pg[:, 32:48], AF.Exp, [rpg, rsm], [rsm])
                    EGL = (EGL0, EGL1)
                    KD = (KD0, KD1)
                    ot, ro = o_.next()
                    o3 = ot.rearrange("p (h v) -> p h v", h=16)
                    for hg in range(0, GDN_HEADS, G):
                        hs = list(range(hg, min(hg + G, GDN_HEADS)))
                        par = gpar[0] % 2
                        gpar[0] += 1
                        C = {h: {} for h in hs}
                        for gi, h in enumerate(hs):
                            C[h]["B"] = LLB[par][gi]; C[h]["F"] = LLF[par][gi]
                        for h in hs:
                            c = C[h]; hc = slice(h, h + 1)
                            c["dg1"] = mf_.next(); c["dg2"] = mf_.next()
                            act(c["dg1"][0], identf, AF.Copy, [r_const, rsm], [c["dg1"][1]], scale=NGC[:, hc])
                            act(c["dg2"][0], identf, AF.Copy, [r_const, rsm], [c["dg2"][1]], scale=GC[:, hc])
                        for h in hs:
                            c = C[h]; hc = slice(h, h + 1)
                            pD, rD = slot()
                            mm(pD, ONESF, c["dg1"][0], True, False, [r_const, c["dg1"][1]], [rD])
                            mm(pD, identf, NEGM_P[d], False, True, [r_const], [rD])
                            c["DP"] = mf_.next()
                            act(c["DP"][0], pD, AF.Exp, [rD, rsm], [c["DP"][1]], bias=GB[:, hc], scale=1.0)
                        for h in hs:
                            c = C[h]; hc = slice(h, h + 1)
                            pD2, rD2 = slot()
                            mm(pD2, ONESF, c["dg2"][0], True, False, [r_const, c["dg2"][1]], [rD2])
                            mm(pD2, identf, NEGM_AT[d], False, True, [r_const], [rD2])
                            c["DAT"] = mf_.next()
                            act(c["DAT"][0], pD2, AF.Exp, [rD2, rsm], [c["DAT"][1]], bias=NGC[:, hc], scale=1.0)
                        for h in hs:
                            c = C[h]
                            pK, rK = slot(); mm(pK, kF3[:, h, :], kF3[:, h, :], True, True, [rkF], [rK])
                            c["P"] = mf_.next()
                            stt("dve", c["P"][0], pK, -1.0, c["DP"][0], ALU.mult, ALU.mult, [rK, c["DP"][1]], [c["P"][1]])
                        for h in hs:
                            c = C[h]
                            c["kb"] = mf_.next()
                        for h in hs:
                            c = C[h]
                            kb = c["kb"][0].bitcast(BF16)[:, 0:128]
                            cp_act(kb, c["kb"][1], kF3[:, h, :], rkF)
                        for h in hs:
                            c = C[h]
                            kb = c["kb"][0].bitcast(BF16)[:, 0:128]
                            pQ, rQ = slot(); mm(pQ, kb, qT3[:, h, :], True, True, [c["kb"][1], rqT], [rQ])
                            AT, rAT = c["B"]["AT"]
                            tt("dve", AT, pQ, c["DAT"][0], ALU.mult, [rQ, c["DAT"][1]], [rAT])
                        for h in hs:
                            c = C[h]
                            pT, rT = slot()
                            tr(pT, c["P"][0], [c["P"][1]], [rT], f32=True)
                            c["Q"] = mf_.next()
                            cp_act(c["Q"][0], c["Q"][1], pT, rT)
                        for h in hs:
                            c = C[h]
                            c["TT"] = mf_.next()
                            tt("pool", c["TT"][0], c["Q"][0], identf, ALU.add, [c["Q"][1], r_const], [c["TT"][1]])
                        for j in range(5):
                            for h in hs:
                                c = C[h]
                                p1, r1 = slot(); mm(p1, c["Q"][0], c["P"][0], True, True, [c["Q"][1], c["P"][1]], [r1])
                                c["Pn"] = mf_.next()
                                (cp_act if j % 2 else cp_dve)(c["Pn"][0], c["Pn"][1], p1, r1)
                            if j < 4:
                                for h in hs:
                                    c = C[h]
                                    p2, r2 = slot(); mm(p2, c["P"][0], c["Q"][0], True, True, [c["Q"][1], c["P"][1]], [r2])
                                    c["Qn"] = mf_.next()
                                    (cp_dve if j % 2 else cp_act)(c["Qn"][0], c["Qn"][1], p2, r2)
                            for h in hs:
                                c = C[h]
                                p3, r3 = slot(); mm(p3, c["Pn"][0], c["TT"][0], True, True, [c["Pn"][1], c["TT"][1]], [r3])
                                c["TTn"] = mf_.next()
                                tt("dve", c["TTn"][0], p3, c["TT"][0], ALU.add, [r3, c["TT"][1]], [c["TTn"][1]])
                            for h in hs:
                                c = C[h]
                                c["P"] = c["Pn"]; c["TT"] = c["TTn"]
                                if j < 4:
                                    c["Q"] = c["Qn"]
                        for h in hs:
                            c = C[h]; hc = slice(h, h + 1)
                            c["vb"] = mf_.next(); c["kbg"] = mf_.next()
                            act(c["vb"][0], vM3[:, h, :], AF.Copy, [rvM, rsm], [c["vb"][1]], scale=EB[:, hc])
                            act(c["kbg"][0], kM3[:, h, :], AF.Copy, [rkM, rsm], [c["kbg"][1]], scale=EGB[:, hc])
                            for ci in (0, 1):
                                kd, rkd = c["B"]["kd%d" % ci]
                                ts("dve", kd, kM3[:, h, :], KD[ci][:, hc], None, ALU.mult, None, [rkM, rsm], [rkd])
                        for h in hs:
                            c = C[h]
                            pU, rU = slot(); mm(pU, c["TT"][0], c["vb"][0], True, True, [c["TT"][1], c["vb"][1]], [rU])
                            u, ru = c["F"]["u"]
                            cp_dve(u, ru, pU, rU)
                        for h in hs:
                            c = C[h]
                            pW, rW = slot(); mm(pW, c["kbg"][0], c["TT"][0], True, True, [c["TT"][1], c["kbg"][1]], [rW])
                            wT, rwT = c["B"]["wT"]
                            cp_act(wT, rwT, pW, rW)
                        for ci in ((0, 1) if d == 0 else (1, 0)):
                            pr = slice(ci * 64, ci * 64 + 64)
                            for h in hs:
                                c = C[h]; dh = d * 16 + h
                                wT, rwT = c["B"]["wT"]; u, ru = c["F"]["u"]; vnew, rvn = c["B"]["vnew"]
                                pv, rv_ = slot()
                                mm(pv, wT, Sbf[:, dh, :], True, True, [rwT, r_S[dh]], [rv_])
                                tt("dve", vnew[pr, :], u[pr, :], pv[pr, :], ALU.subtract, [ru, rv_], [rvn])
                            for h in hs:
                                c = C[h]; dh = d * 16 + h; hc = slice(h, h + 1)
                                tmp, rtmp = c["F"]["tmp"]
                                po1, ro1 = slot()
                                mm(po1, qT3[:, h, :], Sbf[:, dh, :], True, True, [rqT, r_S[dh]], [ro1])
                                act(tmp[pr, :], po1[pr, :], AF.Copy, [ro1, rsm], [rtmp], scale=EGC[pr, hc])
                            for h in hs:
                                c = C[h]
                                AT, rAT = c["B"]["AT"]; vnew, rvn = c["B"]["vnew"]; tmp, rtmp = c["F"]["tmp"]
                                po2, ro2 = slot()
                                mm(po2, AT, vnew, True, True, [rAT, rvn], [ro2])
                                tt("dve", o3[pr, h, :], tmp[pr, :], po2[pr, :], ALU.add, [rtmp, ro2], [ro])
                            for h in hs:
                                c = C[h]; dh = d * 16 + h; hc = slice(h, h + 1)
                                kd, rkd = c["B"]["kd%d" % ci]; vnew, rvn = c["B"]["vnew"]
                                pS, rS_ = slot()
                                mm(pS, kd, vnew, True, True, [rkd, rvn], [rS_])
                                stt("dve", S32[:, dh, :], S32[:, dh, :], EGL[ci][:, hc], pS, ALU.mult, ALU.add, [r_S[dh], rsm, rS_], [r_S[dh]])
                                act(Sbf[:, dh, :], S32[:, dh, :], AF.Copy, [r_S[dh]], [r_S[dh]])
                    P.dma("sp", OFB[d, tc, :], ot, reads=[ro])
            release()

        def phase_gdn_out(l):
            A.mark()
            tiles = list(range(NTILE)) if l == 0 else list(range(CTXT, NTILE))
            gnb = A.alloc(128, F32); r_gn = Res()
            P.dma("sp", gnb, gdn_norm[l].partition_broadcast(128), writes=[r_gn])
            of_ = Ring(A, 2, 2048, F32); ob_ = Ring(A, 2, 2048, F32); z_ = Ring(A, 2, 2048, F32)
            sq_ = Ring(A, 1, 2048, F32); ss_ = Ring(A, 2, 32, F32); yb_ = Ring(A, 2, 2048, BF16); yt_ = Ring(A, 2, 2048, BF16)
            for t in tiles:
                tc = slice(t * 128, (t + 1) * 128)
                of, rof = of_.next(); ob, rob = ob_.next(); z, rz = z_.next()
                P.dma("sp", of, OFB[0, tc, :], writes=[rof])
                P.dma("sp", ob, OFB[1, tc, :], writes=[rob])
                P.dma("sp", z, ZS[tc, :], writes=[rz])
                tt("pool", of, of, ob, ALU.add, [rof, rob], [rof])
                sq, rsq = sq_.next()
                act(sq, of, AF.Square, [rof], [rsq])
                ss, rss = ss_.next()
                P.op("dve", lambda e, ss=ss, sq=sq: e.reduce_sum(ss[:, 0:16], sq.rearrange("p (h v) -> p h v", h=16), AX.X), reads=[rsq], writes=[rss])
                ts("dve", ss[:, 0:16], ss[:, 0:16], 1.0 / 128, EPS, ALU.mult, ALU.add, [rss], [rss])
                act(ss[:, 0:16], ss[:, 0:16], AF.Sqrt, [rss], [rss])
                P.op("dve", lambda e, ss=ss: e.reciprocal(ss[:, 0:16], ss[:, 0:16]), reads=[rss], writes=[rss])
                for h in range(H):
                    hs = slice(h * 128, (h + 1) * 128)
                    stt("dve", of[:, hs], of[:, hs], ss[:, h:h + 1], gnb, ALU.mult, ALU.mult, [rof, rss, r_gn], [rof])
                yb, ryb = yb_.next()
                tt("dve", yb, of, z, ALU.mult, [rof, rz], [ryb])
                yt, ryt = yt_.next()
                yt3 = yt.rearrange("p (h t) -> p h t", h=16)
                for g in range(4):
                    bank = 6 + (g % 2)
                    for j in range(4):
                        k = g * 4 + j
                        tr(psb[bank][:, j * 128:(j + 1) * 128], yb[:, k * 128:(k + 1) * 128], [ryb], [rps[bank]])
                    act(yt3[:, g * 4:(g + 1) * 4, :], psb[bank][:, 0:512].rearrange("p (j t) -> p j t", j=4), AF.Copy, [rps[bank]], [ryt])
                P.dma("sp", YAT[:, :, tc].rearrange("h p t -> p h t"), yt3, reads=[ryt])
            release()

        def resid_ln(xt, rx, r_, rr, gtb, gb_, bb_, r_b, dst, sr):
            tt("pool", r_, r_, gtb, ALU.mult, [rr, r_b], [rr])
            stt("dve", xt, xt, ALPHA, r_, ALU.mult, ALU.add, [rx, rr], [rx])
            s, rs = sr.next()
            st6 = s[:, 0:24].rearrange("p (c s) -> p c s", s=6)
            xv = xt.rearrange("p (c f) -> p c f", f=512)
            for c in range(4):
                P.op("dve", lambda e, c=c: e.bn_stats(st6[:, c, :], xv[:, c, :]), reads=[rx], writes=[rs])
            P.op("dve", lambda e: e.bn_aggr(s[:, 24:26], st6), reads=[rs], writes=[rs])
            ts("dve", s[:, 26:27], s[:, 25:26], EPS, None, ALU.add, None, [rs], [rs])
            act(s[:, 26:27], s[:, 26:27], AF.Sqrt, [rs], [rs])
            P.op("dve", lambda e: e.reciprocal(s[:, 26:27], s[:, 26:27]), reads=[rs], writes=[rs])
            ts("dve", xt, xt, s[:, 24:25], s[:, 26:27], ALU.subtract, ALU.mult, [rx, rs], [rx])
            tt("pool", xt, xt, gb_, ALU.mult, [rx, r_b], [rx])
            tt("dve", xt, xt, bb_, ALU.add, [rx, r_b], [rx])
            P.dma("sp", dst, xt, reads=[rx])

        def load_bcast(l, gt_idx, g_d, b_d):
            r_b = Res()
            gts = []
            for r in (0, 1):
                a = A.alloc(2048, F32)
                P.dma("sp", a, MODV[l, r, gt_idx * 2048:(gt_idx + 1) * 2048].partition_broadcast(128), writes=[r_b])
                gts.append(a)
            gb_ = A.alloc(2048, F32); bb_ = A.alloc(2048, F32)
            P.dma("sp", gb_, g_d[l].partition_broadcast(128), writes=[r_b])
            P.dma("sp", bb_, b_d[l].partition_broadcast(128), writes=[r_b])
            return gts, gb_, bb_, r_b

        def phase_merge(l):
            A.mark()
            groups = TOKG if l == 0 else TOKG[1:]
            gts, gb_, bb_, r_b = load_bcast(l, 2, ln1_g, ln1_b)
            ya_ = Ring(A, 1, 16 * 512, BF16); yb_ = Ring(A, 1, 16 * 512, BF16); yT_ = Ring(A, 1, 16 * 512, BF16)
            w_ = Ring(A, 4, 16 * 128, BF16); g_ = Ring(A, 4, 512, BF16); t_ = Ring(A, 4, 512, F32)
            wo_ = Ring(A, 2, 16 * 256, BF16)
            m_ = Ring(A, 4, 2048, F32); x_ = Ring(A, 1, 2048, F32); sr = Ring(A, 2, 32, F32)
            bi = 0
            for (t0, n) in groups:
                ya, rya = ya_.next(); yb, ryb = yb_.next(); yT, ryT = yT_.next()
                ya3 = ya.rearrange("p (k t) -> p k t", k=16)[:, :, 0:n]; yb3 = yb.rearrange("p (k t) -> p k t", k=16)[:, :, 0:n]
                yT3 = yT.rearrange("p (k t) -> p k t", k=16)
                P.dma("sp", ya3, YAT[:, :, t0:t0 + n].rearrange("h p t -> p h t"), writes=[rya])
                P.dma("sp", yb3, YBT[:, :, t0:t0 + n].rearrange("h p t -> p h t"), writes=[ryb])
                for m in range(16):
                    wa, rwa = w_.next(); wb, rwb = w_.next()
                    wa3 = wa.rearrange("p (k n) -> p k n", k=16); wb3 = wb.rearrange("p (k n) -> p k n", k=16)
                    wcast(wa3, w_br_a[l][:, m * 128:(m + 1) * 128], rwa)
                    wcast(wb3, w_br_b[l][:, m * 128:(m + 1) * 128], rwb)
                    ga, rga = g_.next(); gb2, rgb = g_.next()
                    P.dma("sp", ga[:, 0:n], GT[m][:, t0:t0 + n], writes=[rga])
                    P.dma("sp", gb2[:, 0:n], GT[16 + m][:, t0:t0 + n], writes=[rgb])
                    ba = bi % 6; bi += 1
                    bb = bi % 6; bi += 1
                    for k in range(16):
                        mm(ps[ba][:, 0:n], wa3[:, k, :], ya3[:, k, :], k == 0, k == 15, [rwa, rya], [rps[ba]])
                    for k in range(16):
                        mm(ps[bb][:, 0:n], wb3[:, k, :], yb3[:, k, :], k == 0, k == 15, [rwb, ryb], [rps[bb]])
                    t1, rt1 = t_.next(); t2, rt2 = t_.next()
                    tt("dve", t1[:, 0:n], ps[ba][:, 0:n], ga[:, 0:n], ALU.mult, [rps[ba], rga], [rt1])
                    tt("dve", t2[:, 0:n], ps[bb][:, 0:n], gb2[:, 0:n], ALU.mult, [rps[bb], rgb], [rt2])
                    tt("pool", yT3[:, m, 0:n], t1[:, 0:n], t2[:, 0:n], ALU.add, [rt1, rt2], [ryT])
                ntl = n // 128
                ms = [m_.next() for _ in range(ntl)]
                for cb in range(8):
                    wo, rwo = wo_.next()
                    wo3 = wo.rearrange("p (k n) -> p k n", k=16)
                    wcast(wo3, w_out[l][:, cb * 256:(cb + 1) * 256], rwo)
                    for ti in range(ntl):
                        bank = bi % 6; bi += 1
                        for k in range(16):
                            mm(ps[bank][:, 0:256], yT3[:, k, ti * 128:(ti + 1) * 128], wo3[:, k, :], k == 0, k == 15, [rwo, ryT], [rps[bank]])
                        act(ms[ti][0][:, cb * 256:(cb + 1) * 256], ps[bank][:, 0:256], AF.Copy, [rps[bank]], [ms[ti][1]])
                for ti in range(ntl):
                    t = t0 // 128 + ti
                    xt, rx = x_.next()
                    P.dma("sp", xt, XS[t * 128:(t + 1) * 128, :], writes=[rx])
                    resid_ln(xt, rx, ms[ti][0], ms[ti][1], gts[1 if t < CTXT else 0], gb_, bb_, r_b, XS[t * 128:(t + 1) * 128, :], sr)
            release()

        def phase_ffn(l, last):
            moe = (l % 2 == 1)
            tiles = list(range(NTILE)) if not last else list(range(CTXT, NTILE))
            ntl = len(tiles); ntok = ntl * 128; tb = tiles[0]
            E = NEXP if moe else 1
            FFC = (FF_EXP if moe else FF_DENSE) // 128
            A.mark()
            hT = A.alloc(16 * ntok, BF16).rearrange("p (k t) -> p k t", k=16); r_hT = Res()
            A.mark()
            extra = None
            if moe:
                rwt = A.alloc(128, F32); r_rw = Res()
                P.dma("sp", rwt, router, writes=[r_rw])
                rw3 = rwt.rearrange("p (k e) -> p k e", e=8)
                hf_ = Ring(A, 1, 2048, F32); lg_ = Ring(A, 2, 40, F32)

                def extra(t, xt, rx):
                    ti = t - tb
                    hf, rhf = hf_.next()
                    hf3 = hf.rearrange("p (k t) -> p k t", k=16)
                    for g in range(4):
                        bank = g % 4
                        for j in range(4):
                            k = g * 4 + j
                            tr(ps[bank][:, j * 128:(j + 1) * 128], xt[:, k * 128:(k + 1) * 128], [rx], [rps[bank]], f32=True)
                        act(hf3[:, g * 4:(g + 1) * 4, :], ps[bank].rearrange("p (j t) -> p j t", j=4), AF.Copy, [rps[bank]], [rhf])
                    for k in range(16):
                        mm(ps[4][:, 0:8], hf3[:, k, :], rw3[:, k, :], k == 0, k == 15, [rhf, r_rw], [rps[4]])
                    lg, rlg = lg_.next()
                    P.op("dve", lambda e: e.tensor_copy(lg[:, 0:8], ps[4][:, 0:8]), reads=[rps[4]], writes=[rlg])
                    P.op("dve", lambda e: e.max(lg[:, 8:16], lg[:, 0:8]), reads=[rlg], writes=[rlg])
                    ts("dve", lg[:, 16:24], lg[:, 0:8], lg[:, 9:10], None, ALU.is_ge, None, [rlg], [rlg])
                    ts("dve", lg[:, 32:33], lg[:, 8:9], -1.0, None, ALU.mult, None, [rlg], [rlg])
                    act(lg[:, 24:32], lg[:, 0:8], AF.Exp, [rlg], [rlg], bias=lg[:, 32:33], scale=1.0)
                    tt("dve", lg[:, 24:32], lg[:, 24:32], lg[:, 16:24], ALU.mult, [rlg], [rlg])
                    P.op("dve", lambda e: e.reduce_sum(lg[:, 33:34], lg[:, 24:32], AX.X), reads=[rlg], writes=[rlg])
                    P.op("dve", lambda e: e.reciprocal(lg[:, 33:34], lg[:, 33:34]), reads=[rlg], writes=[rlg])
                    ts("dve", comb[:, ti, :], lg[:, 24:32], lg[:, 33:34], None, ALU.mult, None, [rlg], [r_comb])
            ln_mod_T(l, 1, tiles, hT, r_hT, lambda t: (t - tb) * 128, extra=extra)
            release()
            A.mark()
            w1_ = Ring(A, 2, 16 * 512, BF16); w3_ = Ring(A, 2, 16 * 512, BF16)
            s1_ = Ring(A, 3, 512, F32); st_ = Ring(A, 3, ntok, BF16)
            tgs = [(c0, min(512, ntok - c0)) for c0 in range(0, ntok, 512)]
            bi = 0
            for e in range(E):
                W1 = moe_w1[0, e] if moe else ffn_w1[0]
                W3 = moe_w3[0, e] if moe else ffn_w3[0]
                for fb in range((FFC + 3) // 4):
                    nfc = min(4, FFC - 4 * fb)
                    w1, rw1 = w1_.next(); w3, rw3_ = w3_.next()
                    w13 = w1.rearrange("p (k n) -> p k n", k=16)[:, :, 0:nfc * 128]; w33 = w3.rearrange("p (k n) -> p k n", k=16)[:, :, 0:nfc * 128]
                    wcast(w13, W1[:, fb * 512:fb * 512 + nfc * 128], rw1)
                    wcast(w33, W3[:, fb * 512:fb * 512 + nfc * 128], rw3_)
                    for fc in range(nfc):
                        stg, rst = st_.next()
                        for (c0, n) in tgs:
                            b1 = bi % 8; bi += 1
                            b3 = bi % 8; bi += 1
                            for k in range(16):
                                mm(ps[b1][:, 0:n], w13[:, k, fc * 128:(fc + 1) * 128], hT[:, k, c0:c0 + n], k == 0, k == 15, [rw1, r_hT], [rps[b1]])
                            for k in range(16):
                                mm(ps[b3][:, 0:n], w33[:, k, fc * 128:(fc + 1) * 128], hT[:, k, c0:c0 + n], k == 0, k == 15, [rw3_, r_hT], [rps[b3]])
                            s1, rs1 = s1_.next()
                            act(s1[:, 0:n], ps[b1][:, 0:n], AF.Silu, [rps[b1]], [rs1])
                            tt("dve", stg[:, c0:c0 + n], s1[:, 0:n], ps[b3][:, 0:n], ALU.mult, [rs1, rps[b3]], [rst])
                        P.dma("sp", ATS[e, fb * 4 + fc, :, 0:ntok], stg, reads=[rst])
            release()
            release()
            A.mark()
            HF = FFC // 2
            w2_ = Ring(A, 2, HF * 512, BF16); at_ = Ring(A, 3, HF * 128, BF16)
            acc = A.alloc(ntl * 512, F32).rearrange("p (t n) -> p t n", n=512); r_acc = [Res() for _ in range(ntl)]
            for cb in range(4):
                for e in range(E):
                    W2 = moe_w2[0, e] if moe else ffn_w2[0]
                    for half in range(2):
                        w2, rw2 = w2_.next()
                        w23 = w2.rearrange("p (f n) -> p f n", n=512)
                        wcast(w23, W2[half * HF * 128:(half + 1) * HF * 128, cb * 512:(cb + 1) * 512], rw2)
                        for ti in range(ntl):
                            at, rat = at_.next()
                            at3 = at.rearrange("p (f t) -> p f t", t=128)
                            P.dma("sp", at3, ATS[e, half * HF:(half + 1) * HF, :, ti * 128:(ti + 1) * 128].rearrange("f p t -> p f t"), writes=[rat])
                            bank = bi % 8; bi += 1
                            for f in range(HF):
                                mm(ps[bank], at3[:, f, :], w23[:, f, :], f == 0, f == HF - 1, [rat, rw2], [rps[bank]])
                            first = (e == 0 and half == 0)
                            if moe:
                                if first:
                                    ts("dve", acc[:, ti, :], ps[bank], comb[:, ti, e:e + 1], None, ALU.mult, None, [rps[bank], r_comb], [r_acc[ti]])
                                else:
                                    stt("dve", acc[:, ti, :], ps[bank], comb[:, ti, e:e + 1], acc[:, ti, :], ALU.mult, ALU.add, [rps[bank], r_comb, r_acc[ti]], [r_acc[ti]])
                            else:
                                if first:
                                    act(acc[:, ti, :], ps[bank], AF.Copy, [rps[bank]], [r_acc[ti]])
                                else:
                                    tt("dve", acc[:, ti, :], acc[:, ti, :], ps[bank], ALU.add, [rps[bank], r_acc[ti]], [r_acc[ti]])
                for ti in range(ntl):
                    t = tiles[ti]
                    P.dma("sp", FO[t * 128:(t + 1) * 128, cb * 512:(cb + 1) * 512], acc[:, ti, :], reads=[r_acc[ti]])
            release()
            A.mark()
            gts, gb_, bb_, r_b = load_bcast(l, 5, ln2_g, ln2_b)
            x_ = Ring(A, 2, 2048, F32); f_ = Ring(A, 2, 2048, F32); sr = Ring(A, 2, 32, F32)
            for t in tiles:
                xt, rx = x_.next(); ft, rf = f_.next()
                P.dma("sp", xt, XS[t * 128:(t + 1) * 128, :], writes=[rx])
                P.dma("sp", ft, FO[t * 128:(t + 1) * 128, :], writes=[rf])
                dst = out_d[(t - CTXT) * 128:(t - CTXT + 1) * 128, :] if last else XS[t * 128:(t + 1) * 128, :]
                resid_ln(xt, rx, ft, rf, gts[1 if t < CTXT else 0], gb_, bb_, r_b, dst, sr)
            release()

        GDN_STEPS = 1 if 'gdn_small' in dbg else NTILE
        GDN_HEADS = 1 if 'gdn_small' in dbg else H
        GDN_LVL = 99
        for x in dbg:
            if x.startswith('gdnlvl'):
                GDN_LVL = int(x[6:])
        phase_init()
        for l in range(nlayers):
            last = (l == 1)
            phase_proj(l)
            if "stop_proj" in dbg:
                break
            phase_mla(l)
            if "stop_mla" in dbg:
                break
            phase_gdn(l)
            phase_gdn_out(l)
            if "stop_gdn" in dbg:
                break
            phase_merge(l)
            if "stop_merge" in dbg:
                break
            phase_ffn(l, last)
        nsem = P.emit()
        print("built: ops", {e: len(P.ops[e]) for e in ENGS}, "nsem", nsem, flush=True)
    return nc


def make_inputs(inputs, b, consts):
    f = np.float32
    m = {}
    m["xs"] = np.ascontiguousarray(np.concatenate([inputs["ctx"][b], inputs["x"][b]], axis=0), dtype=f)
    ccv = np.stack([inputs["c"][b], inputs["c_ctx"]], axis=-1)
    m["cc"] = np.ascontiguousarray(ccv.reshape(16, 128, 2).transpose(1, 0, 2).reshape(128, 32), dtype=f)
    for k in ("w_mod", "b_mod", "w_in", "w_uq", "w_ukv", "w_br_a", "w_br_b", "w_out", "ln1_g", "ln1_b", "ln2_g", "ln2_b",
              "ffn_w1", "ffn_w3", "ffn_w2", "moe_w1", "moe_w3", "moe_w2", "gdn_norm"):
        m[k] = np.ascontiguousarray(inputs[k], dtype=f)
    m["convw"] = np.ascontiguousarray(inputs["conv_w"].reshape(2, 5, 48, 128).transpose(0, 3, 2, 1).reshape(2, 128, 240), dtype=f)
    m["a_log"] = np.ascontiguousarray(inputs["a_log"].reshape(2, 32), dtype=f)
    m["dt_bias"] = np.ascontiguousarray(inputs["dt_bias"].reshape(2, 32), dtype=f)
    qn = inputs["q_norm"].reshape(2, 4, 128).transpose(0, 2, 1)
    kn = inputs["kv_norm"].reshape(2, 4, 128).transpose(0, 2, 1)
    m["qkn"] = np.ascontiguousarray(np.concatenate([qn, kn], axis=2), dtype=f)
    m["router"] = np.ascontiguousarray(inputs["moe_router"][0].reshape(16, 128, 8).transpose(1, 0, 2).reshape(128, 128), dtype=f)
    m.update(consts)
    return m


_NC = None


def kernel(**inputs):
    global _NC
    inputs = {k: np.asarray(v) for k, v in inputs.items()}
    consts = host_consts()
    if _NC is None:
        _NC = build(bass.Bass("TRN2", target_bir_lowering=False))
    in_maps = [make_inputs(inputs, b, consts) for b in range(8)]
    res = run_bass_kernel_spmd(_NC, in_maps, core_ids=list(range(8)))
    return np.stack([np.asarray(r["out"], dtype=np.float32) for r in res.results], axis=0)
```

```python
import numpy as np
import concourse.bass as bass
import concourse.mybir as mybir
from concourse.bass_utils import run_bass_kernel_spmd
from contextlib import ExitStack

F32 = mybir.dt.float32
BF16 = mybir.dt.bfloat16
AF = mybir.ActivationFunctionType
ALU = mybir.AluOpType
AX = mybir.AxisListType

ENGS = ("pe", "act", "dve", "pool", "sp")
EPOCH = 12000
NDS = 12
DEPOCH = 1500

D = 2048
NT = 2304
NTILE = 18
CTXT = 2
H = 16
PROJ_W = 13440
FF_DENSE = 5632
FF_EXP = 7168
NEXP = 8
ALPHA = 4 ** 0.25
EPS = 1e-6
MLA_SCALE = 192 ** -0.5
NEG = -1.0e6


class Res:
    __slots__ = ("w", "rd", "name", "excl")

    def __init__(self, name="", excl=False):
        self.w = None
        self.rd = {}
        self.name = name
        self.excl = excl


class Prog:
    def __init__(self, nc, same_engine_sync=("act", "dve", "pool")):
        self.nc = nc
        self.ops = {e: [] for e in ENGS}
        self.cnt = {e: 0 for e in ENGS}
        self.dcnt = {e: 0 for e in ENGS}
        self.know = {e: {} for e in ENGS}
        self.last = {}
        self.semkeys = {}
        self.same = set(same_engine_sync)

    def _mkwaits(self, eng, deps):
        waits = {}
        kn = self.know[eng]
        for (sk, v, e) in deps:
            if e == eng and sk[0] == "c" and eng not in self.same:
                continue
            if kn.get(sk, 0) >= v:
                continue
            kn[sk] = v
            if waits.get(sk, 0) < v:
                waits[sk] = v
        return list(waits.items())

    def _deps(self, eng, reads, writes):
        deps = []
        for r in reads:
            if r.w is not None:
                deps.append(r.w)
            if r.excl:
                for sk, (v, e) in r.rd.items():
                    if e != eng:
                        deps.append((sk, v, e))
        for r in writes:
            if r.w is not None:
                deps.append(r.w)
            for sk, (v, e) in r.rd.items():
                deps.append((sk, v, e))
        return self._mkwaits(eng, deps)

    def _record(self, ident, reads, writes):
        sk, v, e = ident
        self.last[sk] = (v, e)
        for r in writes:
            r.w = ident
            r.rd = {}
        for r in reads:
            cur = r.rd.get(sk)
            if cur is None or cur[0] < v:
                r.rd[sk] = (v, e)

    def op(self, eng, fn, reads=(), writes=()):
        waits = self._deps(eng, reads, writes)
        k = self.cnt[eng]
        self.cnt[eng] = k + 1
        sk = ("c", eng, k // EPOCH)
        v = k % EPOCH + 1
        self.ops[eng].append((waits, fn, (sk, v)))
        self._record((sk, v, eng), reads, writes)

    def dma(self, q, out, in_, reads=(), writes=(), **kw):
        waits = self._deps(q, reads, writes)
        j = self.dcnt[q]
        self.dcnt[q] = j + 1
        slot = j % NDS
        use = j // NDS
        sk = ("d", q, slot, use // DEPOCH)
        v = 16 * (use % DEPOCH + 1)
        if use % DEPOCH > 0:
            pv = v - 16
            kn = self.know[q]
            if kn.get(sk, 0) < pv:
                kn[sk] = pv
                waits.append((sk, pv))

        def fn(eng, out=out, in_=in_, kw=kw):
            return eng.dma_start(out=out, in_=in_, **kw)
        self.ops[q].append((waits, fn, (sk, v)))
        self._record((sk, v, q), reads, writes)

    def barrier(self):
        deps = [(sk, v, e) for sk, (v, e) in self.last.items()]
        for eng in ENGS:
            kn = self.know[eng]
            waits = {}
            for (sk, v, e) in deps:
                if kn.get(sk, 0) >= v:
                    continue
                kn[sk] = v
                waits[sk] = v
            if waits:
                self.ops[eng].append((list(waits.items()), None, None))

    def emit(self):
        nc = self.nc
        self.barrier()
        allkeys = set()
        for e in ENGS:
            for waits, fn, inc in self.ops[e]:
                if inc is not None:
                    allkeys.add(inc[0])
        allkeys = sorted(allkeys)
        with ExitStack() as st:
            for i, sk in enumerate(allkeys):
                self.semkeys[sk] = st.enter_context(nc.semaphore("s%d" % i))
            block = st.enter_context(nc.Block())
            sem = self.semkeys

            def run(engname, eng):
                for waits, fn, inc in self.ops[engname]:
                    for sk, v in waits:
                        eng.wait_ge(sem[sk], v)
                    if fn is None:
                        continue
                    ins = fn(eng)
                    ins.then_inc(sem[inc[0]], 16 if inc[0][0] == "d" else 1)

            @block.tensor
            def _(e):
                run("pe", e)

            @block.scalar
            def _(e):
                run("act", e)

            @block.vector
            def _(e):
                run("dve", e)

            @block.gpsimd
            def _(e):
                run("pool", e)

            @block.sync
            def _(e):
                run("sp", e)
        return len(allkeys)


class Arena:
    def __init__(self, ap, nwords):
        self.ap = ap
        self.n = nwords
        self.off = 0
        self.marks = []

    def alloc(self, free_elems, dtype):
        words = (free_elems * (2 if dtype == BF16 else 4) + 3) // 4
        words = (words + 7) // 8 * 8
        assert self.off + words <= self.n, ("arena overflow", self.off, words, self.n)
        a = self.ap[:, self.off:self.off + words]
        self.off += words
        if dtype == BF16:
            a = a.bitcast(BF16)
        return a[:, 0:free_elems]

    def mark(self):
        self.marks.append(self.off)

    def release(self):
        self.off = self.marks.pop()


class Ring:
    def __init__(self, arena, n, free_elems, dtype):
        self.bufs = [(arena.alloc(free_elems, dtype), Res()) for _ in range(n)]
        self.i = 0

    def next(self):
        b = self.bufs[self.i % len(self.bufs)]
        self.i += 1
        return b


def interleave(gens, k):
    it = iter(gens)
    active = []
    more = True
    while True:
        while more and len(active) < k:
            try:
                active.append(next(it))
            except StopIteration:
                more = False
        if not active:
            break
        for g in list(active):
            try:
                next(g)
            except StopIteration:
                active.remove(g)


def host_consts():
    c = {}
    c["identf"] = np.eye(128, dtype=np.float32)
    idx = np.arange(128)
    same = (idx[:, None] // 64) == (idx[None, :] // 64)
    masks = np.zeros((12, 128, 128), np.float32)
    for d in range(2):
        aft = (idx[:, None] >= idx[None, :]) if d == 0 else (idx[:, None] <= idx[None, :])
        aft_s = (idx[:, None] > idx[None, :]) if d == 0 else (idx[:, None] < idx[None, :])
        masks[0 + d] = np.where((aft & same).T, 0.0, NEG)
        masks[2 + d] = np.where((aft_s & same), 0.0, NEG)
        masks[4 + d] = np.where((aft & same).T, 1.0, 0.0)
    masks[6] = np.where(idx[:, None] < 64, 1.0, 0.0) * np.ones((1, 128))
    masks[7] = np.where(idx[:, None] >= 64, 1.0, 0.0) * np.ones((1, 128))
    masks[8] = 1.0
    c["masks"] = masks.astype(np.float32)
    rows = 2048 // 64
    row = np.repeat(np.arange(rows), 64).astype(np.float32)
    col = np.tile(np.arange(64), rows).astype(np.float32)
    inv_freq = (10000.0 ** (-np.arange(16, dtype=np.float32) / 16)).astype(np.float32)
    cosT = np.zeros((64, 2048), np.float32)
    sinT = np.zeros((64, 2048), np.float32)
    for ax, pos in enumerate((row, col)):
        ang = (pos[None, :] * inv_freq[:, None]).astype(np.float32)
        for half in range(2):
            p0 = ax * 32 + half * 16
            cosT[p0:p0 + 16] = np.cos(ang)
            sinT[p0:p0 + 16] = np.sin(ang) * (-1.0 if half == 0 else 1.0)
    c["ropec"] = np.stack([cosT, sinT]).astype(np.float32)
    return c


def build(nc, nlayers=2, dbg=()):
    def din(name, shape):
        return nc.dram_tensor(name, list(shape), F32, kind="ExternalInput").ap()

    def dscr(name, shape, dt=F32):
        kind = "ExternalOutput" if name in dbg else "Internal"
        return nc.dram_tensor(name, list(shape), dt, kind=kind).ap()

    xs_in = din("xs", [NT, D])
    cc = din("cc", [128, 32])
    w_mod = din("w_mod", [2, D, 6 * D]); b_mod = din("b_mod", [2, 6 * D])
    w_in = din("w_in", [2, D, PROJ_W])
    convw = din("convw", [2, 128, 48 * 5])
    a_log = din("a_log", [2, 32]); dt_bias = din("dt_bias", [2, 32])
    gdn_norm = din("gdn_norm", [2, 128])
    qkn = din("qkn", [2, 128, 8])
    w_uq = din("w_uq", [2, 512, 3072]); w_ukv = din("w_ukv", [2, 512, 4096])
    w_br_a = din("w_br_a", [2, D, D]); w_br_b = din("w_br_b", [2, D, D]); w_out = din("w_out", [2, D, D])
    ln1_g = din("ln1_g", [2, D]); ln1_b = din("ln1_b", [2, D]); ln2_g = din("ln2_g", [2, D]); ln2_b = din("ln2_b", [2, D])
    ffn_w1 = din("ffn_w1", [1, D, FF_DENSE]); ffn_w3 = din("ffn_w3", [1, D, FF_DENSE]); ffn_w2 = din("ffn_w2", [1, FF_DENSE, D])
    router = din("router", [128, 16 * 8])
    moe_w1 = din("moe_w1", [1, NEXP, D, FF_EXP]); moe_w3 = din("moe_w3", [1, NEXP, D, FF_EXP]); moe_w2 = din("moe_w2", [1, NEXP, FF_EXP, D])
    identf_d = din("identf", [128, 128]); masks_d = din("masks", [12, 128, 128]); ropec_d = din("ropec", [2, 64, 2048])
    out_d = nc.dram_tensor("out", [2048, D], F32, kind="ExternalOutput").ap()

    XS = dscr("XS", [NT, D])
    MODV = dscr("MODV", [2, 2, 6 * D])
    QT = dscr("QT", [H, 128, NT], BF16); KT = dscr("KT", [H, 128, NT], BF16); KT32 = dscr("KT32", [H, 128, NT])
    KM = dscr("KM", [NT, H, 128], BF16); VM = dscr("VM", [NT, H, 128], BF16)
    ZS = dscr("ZS", [NT, D])
    GT = dscr("GT", [32, 128, NT], BF16)
    QNT = dscr("QNT", [H, 128, NT], BF16); QRT = dscr("QRT", [H, 64, NT], BF16)
    KNT = dscr("KNT", [H, 128, NT], BF16); VM2 = dscr("VM2", [H, NT, 128], BF16)
    OFB = dscr("OFB", [2, NT, H * 128])
    YAT = dscr("YAT", [H, 128, NT], BF16); YBT = dscr("YBT", [H, 128, NT], BF16)
    ATS = dscr("ATS", [NEXP, 56, 128, NT], BF16)
    FO = dscr("FO", [NT, D])

    NW = 47000
    with ExitStack() as st:
        arena_t = st.enter_context(nc.sbuf_tensor("arena", [128, NW], F32))
        ps_all = st.enter_context(nc.psum_tensor("psall", [128, 4096], F32))
        A = Arena(arena_t[:], NW)
        P = Prog(nc)
        ps = [ps_all[:, b * 512:(b + 1) * 512] for b in range(8)]
        psb = [ps_all[:, b * 512:(b + 1) * 512].bitcast(BF16) for b in range(8)]
        rps = [Res("ps%d" % b, excl=True) for b in range(8)]

        identf = A.alloc(128, F32); identb = A.alloc(128, BF16); r_const = Res("const")
        masks = [A.alloc(128, F32) for _ in range(9)]
        onecol = A.alloc(1, F32); epscol = A.alloc(1, F32)
        P.dma("sp", identf, identf_d, writes=[r_const])
        for i in range(9):
            P.dma("sp", masks[i], masks_d[i], writes=[r_const])
        P.op("dve", lambda e: e.tensor_copy(identb, identf), reads=[r_const], writes=[r_const])
        P.op("dve", lambda e: e.memset(onecol, 1.0), writes=[r_const])
        P.op("dve", lambda e: e.memset(epscol, EPS), writes=[r_const])
        NEGM_AT, NEGM_P, MCUM, MCH, ONESF = masks[0:2], masks[2:4], masks[4:6], masks[6:8], masks[8]
        g_all = A.alloc(NTILE * 32, F32).rearrange("p (t c) -> p t c", c=32); r_gall = Res("gall")
        lnb_all = A.alloc(NTILE * 32, F32).rearrange("p (t c) -> p t c", c=32)
        KRT = A.alloc(NT, BF16); r_krt = Res("krt")
        comb = A.alloc(16 * 8, F32).rearrange("p (t e) -> p t e", e=8); r_comb = Res("comb")
        P.barrier()

        def release():
            A.release()
            P.barrier()

        def mm(out, lhsT, rhs, start, stop, reads, writes):
            P.op("pe", lambda e: e.matmul(out, lhsT, rhs, start=start, stop=stop), reads=reads, writes=writes)

        def tr(out, in_, reads, writes, f32=False):
            idn = identf if f32 else identb
            P.op("pe", lambda e: e.transpose(out, in_, idn), reads=list(reads) + [r_const], writes=writes)

        def act(out, in_, func, reads, writes, **kw):
            P.op("act", lambda e: e.activation(out, in_, func, **kw), reads=reads, writes=writes)

        def tt(eng, out, in0, in1, op, reads, writes):
            P.op(eng, lambda e: e.tensor_tensor(out, in0, in1, op), reads=reads, writes=writes)

        def ts(eng, out, in0, s1, s2, op0, op1, reads, writes):
            if op1 is None:
                P.op(eng, lambda e: e.tensor_scalar(out, in0, s1, None, op0), reads=reads, writes=writes)
            else:
                P.op(eng, lambda e: e.tensor_scalar(out, in0, s1, s2, op0, op1), reads=reads, writes=writes)

        def stt(eng, out, in0, scalar, in1, op0, op1, reads, writes):
            P.op(eng, lambda e: e.scalar_tensor_tensor(out, in0, scalar, in1, op0, op1), reads=reads, writes=writes)

        def wcast(dst3, src2, rw):
            P.dma("pool", dst3, src2.rearrange("(k p) n -> p k n", p=128), writes=[rw])

        TOKG = [(0, 256), (256, 512), (768, 512), (1280, 512), (1792, 512)]

        def phase_init():
            A.mark()
            for t in range(NTILE):
                P.dma("sp", XS[t * 128:(t + 1) * 128, :], xs_in[t * 128:(t + 1) * 128, :])
            cct = A.alloc(32, F32); r_cc = Res()
            P.dma("sp", cct, cc, writes=[r_cc])
            act(cct, cct, AF.Silu, [r_cc], [r_cc])
            cv = cct.rearrange("p (k t) -> p k t", t=2)
            wr = Ring(A, 2, 16 * 512, F32)
            br = Ring(A, 2, 512, F32)
            orr = Ring(A, 2, 512, F32)
            i = 0
            for l in range(2):
                for nb in range(24):
                    wt, rw = wr.next()
                    wt3 = wt.rearrange("p (k n) -> p k n", k=16)
                    P.dma("sp", wt3, w_mod[l, :, nb * 512:(nb + 1) * 512].rearrange("(k p) n -> p k n", p=128), writes=[rw])
                    bt, rb = br.next()
                    P.dma("sp", bt[0:2, :], b_mod[l, nb * 512:(nb + 1) * 512].partition_broadcast(2), writes=[rb])
                    bank = i % 2
                    i += 1
                    for k in range(16):
                        mm(ps[bank][0:2, :], cv[:, k, :], wt3[:, k, :], k == 0, k == 15, [r_cc, rw], [rps[bank]])
                    ot, ro = orr.next()
                    tt("dve", ot[0:2, :], ps[bank][0:2, :], bt[0:2, :], ALU.add, [rps[bank], rb], [ro])
                    if nb // 4 in (1, 4):
                        ts("dve", ot[0:2, :], ot[0:2, :], 1.0, None, ALU.add, None, [ro], [ro])
                    P.dma("sp", MODV[l, :, nb * 512:(nb + 1) * 512], ot[0:2, :], reads=[ro])
            release()

        def ln_mod_T(l, which, tiles, hT, r_hT, col0, extra=None):
            shi, sci = (0, 1) if which == 0 else (3, 4)
            bA, bB = [], []
            r_b = Res()
            for r in (0, 1):
                a = A.alloc(2048, F32); b = A.alloc(2048, F32)
                P.dma("sp", a, MODV[l, r, sci * 2048:(sci + 1) * 2048].partition_broadcast(128), writes=[r_b])
                P.dma("sp", b, MODV[l, r, shi * 2048:(shi + 1) * 2048].partition_broadcast(128), writes=[r_b])
                bA.append(a); bB.append(b)
            xr = Ring(A, 2, 2048, F32)
            hbr = Ring(A, 2, 2048, BF16)
            sr = Ring(A, 2, 32, F32)
            def tile_gen(t):
                r = 1 if t < CTXT else 0
                xt, rx = xr.next()
                P.dma("sp", xt, XS[t * 128:(t + 1) * 128, :], writes=[rx])
                s, rs = sr.next()
                st6 = s[:, 0:24].rearrange("p (c s) -> p c s", s=6)
                xv = xt.rearrange("p (c f) -> p c f", f=512)
                yield
                for c in range(4):
                    P.op("dve", lambda e, c=c, st6=st6, xv=xv: e.bn_stats(st6[:, c, :], xv[:, c, :]), reads=[rx], writes=[rs])
                P.op("dve", lambda e, s=s, st6=st6: e.bn_aggr(s[:, 24:26], st6), reads=[rs], writes=[rs])
                ts("dve", s[:, 26:27], s[:, 25:26], EPS, None, ALU.add, None, [rs], [rs])
                yield
                act(s[:, 26:27], s[:, 26:27], AF.Sqrt, [rs], [rs])
                yield
                P.op("dve", lambda e, s=s: e.reciprocal(s[:, 26:27], s[:, 26:27]), reads=[rs], writes=[rs])
                ts("dve", xt, xt, s[:, 24:25], s[:, 26:27], ALU.subtract, ALU.mult, [rx, rs], [rx])
                yield
                tt("pool", xt, xt, bA[r], ALU.mult, [rx, r_b], [rx])
                yield
                tt("dve", xt, xt, bB[r], ALU.add, [rx, r_b], [rx])
                yield
                hb, rhb = hbr.next()
                act(hb, xt, AF.Copy, [rx], [rhb])
                yield
                if extra is not None:
                    extra(t, xt, rx)
                    yield
                c = col0(t)
                for g in range(4):
                    bank = 6 + (g % 2)
                    for j in range(4):
                        k = g * 4 + j
                        tr(psb[bank][:, j * 128:(j + 1) * 128], hb[:, k * 128:(k + 1) * 128], [rhb], [rps[bank]])
                    src = psb[bank][:, 0:512].rearrange("p (j t) -> p j t", j=4)
                    dst = hT[:, g * 4:(g + 1) * 4, c:c + 128]
                    if g % 2:
                        P.op("dve", lambda e, dst=dst, src=src: e.tensor_copy(dst, src), reads=[rps[bank]], writes=[r_hT])
                    else:
                        act(dst, src, AF.Copy, [rps[bank]], [r_hT])
                    yield
            interleave((tile_gen(t) for t in tiles), 2)

        def phase_proj(l):
            A.mark()
            DQK = [A.alloc(4 * NT, BF16).rearrange("p (k t) -> p k t", k=4) for _ in range(2)]; r_dqk = [Res(), Res()]
            qk = A.alloc(8, F32); r_qk = Res()
            P.dma("sp", qk, qkn[l], writes=[r_qk])
            RC = {}

            def load_rope():
                ropec = A.alloc(2 * 2048, F32); RC["r"] = Res()
                RC["COS"] = ropec[0:64, 0:2048]; RC["SINS"] = ropec[0:64, 2048:4096]
                P.dma("sp", RC["COS"], ropec_d[0], writes=[RC["r"]])
                P.dma("sp", RC["SINS"], ropec_d[1], writes=[RC["r"]])
                RC["t12"] = Ring(A, 2, 1024, F32)
            A.mark()
            hT = A.alloc(16 * NT, BF16).rearrange("p (k t) -> p k t", k=16); r_hT = Res("hT")
            A.mark()
            ln_mod_T(l, 0, range(NTILE), hT, r_hT, lambda t: t * 128)
            release()
            wfm = Ring(A, 2, 16 * 128, BF16)
            cw = A.alloc(240, F32); r_cw = Res()
            P.dma("sp", cw, convw[l], writes=[r_cw])
            cwv = cw.rearrange("p (c j) -> p c j", j=5)
            W = w_in[l]

            def lin_fm(wt3, rw, kc, m, tok0, ntok, bank, inT, r_in, m0=0, po=0):
                for k in range(kc):
                    mm(ps[bank][po:po + m, 0:ntok], wt3[:, k, m0:m0 + m], inT[:, k, tok0:tok0 + ntok], k == 0, k == kc - 1, [rw, r_in], [rps[bank]])

            A.mark()
            rawr = Ring(A, 1, 2312, F32)
            for rb, rr in rawr.bufs:
                P.op("dve", lambda e, rb=rb: e.memset(rb, 0.0), writes=[rr])
            accr = Ring(A, 1, NT, F32)
            sqr = Ring(A, 1, NT, F32)
            rnr = Ring(A, 2, 512, F32)
            dgr = Ring(A, 10, 128, F32)
            fmr = Ring(A, 2, NT, BF16)
            tmr = Ring(A, 1, NT, BF16)
            bi = 0
            for ch in range(48):
                kind, h = ch // 16, ch % 16
                wt, rw = wfm.next()
                wt3 = wt.rearrange("p (k n) -> p k n", k=16)
                wcast(wt3, W[:, ch * 128:(ch + 1) * 128], rw)
                raw, rr = rawr.next()
                for gi, (t0, n) in enumerate(TOKG):
                    bank = bi % 4; bi += 1
                    lin_fm(wt3, rw, 16, 128, t0, n, bank, hT, r_hT)
                    dst = raw[:, 2:258] if gi == 0 else raw[:, 262 + (t0 - 256):262 + (t0 - 256) + n]
                    act(dst, ps[bank][:, 0:n], AF.Copy, [rps[bank]], [rr])
                acc, ra = accr.next()
                fm, rf = fmr.next()
                dgs = []
                for j in range(5):
                    dg, rdg = dgr.next()
                    act(dg, identf, AF.Copy, [r_const, r_cw], [rdg], scale=cwv[:, ch, j:j + 1])
                    dgs.append((dg, rdg))
                for gi, (t0, n) in enumerate(TOKG):
                    bank = bi % 4; bi += 1
                    ro = 2 if gi == 0 else 262 + (t0 - 256)
                    for j in range(5):
                        mm(ps[bank][:, 0:n], dgs[j][0], raw[:, ro - 2 + j:ro - 2 + j + n], j == 0, j == 4, [dgs[j][1], rr], [rps[bank]])
                    if kind == 2:
                        act(fm[:, t0:t0 + n], ps[bank][:, 0:n], AF.Silu, [rps[bank]], [rf])
                    else:
                        act(acc[:, t0:t0 + n], ps[bank][:, 0:n], AF.Silu, [rps[bank]], [ra])
                if kind != 2:
                    sq, rs = sqr.next()
                    act(sq, acc, AF.Square, [ra], [rs])
                    for (t0, n) in TOKG:
                        bank = bi % 4; bi += 1
                        mm(ps[bank][:, 0:n], ONESF, sq[:, t0:t0 + n], True, True, [rs, r_const], [rps[bank]])
                        rn, rrn = rnr.next()
                        act(rn[:, 0:n], ps[bank][:, 0:n], AF.Sqrt, [rps[bank], r_const], [rrn], bias=epscol)
                        P.op("dve", lambda e, rn=rn, n=n: e.reciprocal(rn[:, 0:n], rn[:, 0:n]), reads=[rrn], writes=[rrn])
                        if kind == 0:
                            stt("dve", fm[:, t0:t0 + n], acc[:, t0:t0 + n], 128 ** -0.5, rn[:, 0:n], ALU.mult, ALU.mult, [ra, rrn], [rf])
                        else:
                            tt("dve", sq[:, t0:t0 + n], acc[:, t0:t0 + n], rn[:, 0:n], ALU.mult, [ra, rrn, rs], [rs])
                            act(fm[:, t0:t0 + n], sq[:, t0:t0 + n], AF.Copy, [rs], [rf])
                    P.dma("sp", (QT if kind == 0 else KT)[h], fm, reads=[rf])
                    if kind == 1:
                        P.dma("sp", KT32[h], sq, reads=[rs])
                if kind >= 1:
                    tm, rt = tmr.next()
                    tm3 = tm.rearrange("p (t d) -> p t d", d=128)
                    for g in range(5):
                        bank = 6 + (g % 2)
                        nn = 4 if g < 4 else 2
                        for j in range(nn):
                            t = g * 4 + j
                            tr(psb[bank][:, j * 128:(j + 1) * 128], fm[:, t * 128:(t + 1) * 128], [rf], [rps[bank]])
                        src = psb[bank][:, 0:nn * 128].rearrange("p (j t) -> p j t", j=nn)
                        if g % 2:
                            P.op("dve", lambda e, tm3=tm3, g=g, nn=nn, src=src: e.tensor_copy(tm3[:, g * 4:g * 4 + nn, :], src), reads=[rps[bank]], writes=[rt])
                        else:
                            act(tm3[:, g * 4:g * 4 + nn, :], src, AF.Copy, [rps[bank]], [rt])
                    dst = (KM if kind == 1 else VM).rearrange("(t p) h d -> p t h d", p=128)[:, :, h, :]
                    P.dma("sp", dst, tm3, reads=[rt])
            release()

            A.mark()
            wtm = Ring(A, 2, 16 * 512, BF16)
            zr = Ring(A, 3, 512, F32)
            for nb in range(4):
                wt, rw = wtm.next()
                wt3 = wt.rearrange("p (k n) -> p k n", k=16)
                wcast(wt3, W[:, 6144 + nb * 512:6144 + (nb + 1) * 512], rw)
                for t in range(NTILE):
                    bank = bi % 4; bi += 1
                    for k in range(16):
                        mm(ps[bank], hT[:, k, t * 128:(t + 1) * 128], wt3[:, k, :], k == 0, k == 15, [rw, r_hT], [rps[bank]])
                    z, rz = zr.next()
                    act(z, ps[bank], AF.Silu, [rps[bank]], [rz])
                    P.dma("sp", ZS[t * 128:(t + 1) * 128, nb * 512:(nb + 1) * 512], z, reads=[rz])

            wt, rw = wtm.next()
            wab = wt[:, 0:16 * 64].rearrange("p (k n) -> p k n", k=16)
            wcast(wab, W[:, 8192:8256], rw)
            negA = A.alloc(32, F32); dtb = A.alloc(32, F32); r_ab = Res()
            P.dma("sp", negA, a_log[l].partition_broadcast(128), writes=[r_ab])
            P.dma("sp", dtb, dt_bias[l].partition_broadcast(128), writes=[r_ab])
            act(negA, negA, AF.Exp, [r_ab], [r_ab])
            ts("dve", negA, negA, -1.0, None, ALU.mult, None, [r_ab], [r_ab])
            tmpr = Ring(A, 2, 64, F32)
            for t in range(NTILE):
                bank = bi % 4; bi += 1
                for k in range(16):
                    mm(ps[bank][:, 0:64], hT[:, k, t * 128:(t + 1) * 128], wab[:, k, :], k == 0, k == 15, [rw, r_hT], [rps[bank]])
                tp, rtp = tmpr.next()
                tt("dve", tp[:, 0:32], ps[bank][:, 0:32], dtb, ALU.add, [rps[bank], r_ab], [rtp])
                act(tp[:, 0:32], tp[:, 0:32], AF.Exp, [rtp], [rtp])
                act(tp[:, 32:64], ps[bank][:, 32:64], AF.Exp, [rps[bank], rtp], [rtp], scale=-1.0)
                act(tp, tp, AF.Ln, [rtp, r_const], [rtp], bias=onecol)
                tt("dve", g_all[:, t, :], tp[:, 0:32], negA, ALU.mult, [rtp, r_ab], [r_gall])
                ts("dve", lnb_all[:, t, :], tp[:, 32:64], -1.0, None, ALU.mult, None, [rtp], [r_gall])

            xnr = Ring(A, 2, 512, BF16)
            ssr = Ring(A, 2, 8, F32)
            junk = A.alloc(512, F32); r_junk = Res()
            for which in range(2):
                wt, rw = wtm.next()
                wt3 = wt.rearrange("p (k n) -> p k n", k=16)
                wcast(wt3, W[:, 8256 + which * 512:8256 + (which + 1) * 512], rw)
                for t in range(NTILE):
                    bank = bi % 4; bi += 1
                    for k in range(16):
                        mm(ps[bank], hT[:, k, t * 128:(t + 1) * 128], wt3[:, k, :], k == 0, k == 15, [rw, r_hT], [rps[bank]])
                    s, rs = ssr.next()
                    act(junk, ps[bank], AF.Square, [rps[bank]], [r_junk, rs], accum_out=s[:, 0:1])
                    ts("dve", s[:, 1:2], s[:, 0:1], 1.0 / 512, EPS, ALU.mult, ALU.add, [rs], [rs])
                    act(s[:, 1:2], s[:, 1:2], AF.Sqrt, [rs], [rs])
                    P.op("dve", lambda e, s=s: e.reciprocal(s[:, 1:2], s[:, 1:2]), reads=[rs], writes=[rs])
                    xn, rxn = xnr.next()
                    act(xn, ps[bank], AF.Copy, [rps[bank], rs], [rxn], scale=s[:, 1:2])
                    tb = 6 + (t % 2)
                    for j in range(4):
                        tr(psb[tb][:, j * 128:(j + 1) * 128], xn[:, j * 128:(j + 1) * 128], [rxn], [rps[tb]])
                    for j in range(4):
                        ts("dve", DQK[which][:, j, t * 128:(t + 1) * 128], psb[tb][:, j * 128:(j + 1) * 128], qk[:, which * 4 + j:which * 4 + j + 1], None, ALU.mult, None, [rps[tb], r_qk], [r_dqk[which]])

            release()
            load_rope()
            PERM = [(0, 16), (16, 0), (32, 48), (48, 32)]
            wt, rw = wfm.next()
            wkr = wt.rearrange("p (k n) -> p k n", k=16)
            P.dma("pool", wkr[:, :, 0:64], W[:, 9280:9344].rearrange("(k p) n -> p k n", p=128), writes=[rw])
            for (dc, sc) in PERM:
                P.dma("pool", wkr[:, :, 64 + dc:64 + dc + 16], W[:, 9280 + sc:9280 + sc + 16].rearrange("(k p) n -> p k n", p=128), writes=[rw])

            def rope_evac(bank_a, bank_b, n, t0, dst, r_dst, scale):
                if t0 == 0:
                    act(dst[0:64, t0:t0 + n], ps[bank_a][0:64, 0:n], AF.Copy, [rps[bank_a]], [r_dst], scale=scale)
                    return
                tb, rtb = RC["t12"].next()
                COS, SINS, r_rope = RC["COS"], RC["SINS"], RC["r"]
                c0 = t0 - 256
                stt("dve", tb[0:64, 0:n], ps[bank_a][0:64, 0:n], scale, COS[:, c0:c0 + n], ALU.mult, ALU.mult, [rps[bank_a], r_rope], [rtb])
                stt("dve", tb[0:64, 512:512 + n], ps[bank_b][0:64, 0:n], scale, SINS[:, c0:c0 + n], ALU.mult, ALU.mult, [rps[bank_b], r_rope, rtb], [rtb])
                tt("dve", dst[0:64, t0:t0 + n], tb[0:64, 0:n], tb[0:64, 512:512 + n], ALU.add, [rtb], [r_dst])

            for (t0, n) in TOKG:
                ba = bi % 4; bi += 1
                bb = bi % 4; bi += 1
                lin_fm(wkr, rw, 16, 64, t0, n, ba, hT, r_hT, m0=0)
                lin_fm(wkr, rw, 16, 64, t0, n, bb, hT, r_hT, m0=64)
                rope_evac(ba, bb, n, t0, KRT, r_krt, 1.0)

            gr = Ring(A, 2, NT, BF16)
            for ch in range(32):
                wt, rw = wfm.next()
                wt3 = wt.rearrange("p (k n) -> p k n", k=16)
                wcast(wt3, W[:, 9344 + ch * 128:9344 + (ch + 1) * 128], rw)
                gt, rg = gr.next()
                for (t0, n) in TOKG:
                    bank = bi % 4; bi += 1
                    lin_fm(wt3, rw, 16, 128, t0, n, bank, hT, r_hT)
                    act(gt[:, t0:t0 + n], ps[bank][:, 0:n], AF.Sigmoid, [rps[bank]], [rg])
                P.dma("sp", GT[ch], gt, reads=[rg])

            release()
            load_rope()
            wqr = Ring(A, 2, 4 * 256, BF16)
            wkr2 = Ring(A, 2, 4 * 256, BF16)
            str_ = Ring(A, 3, NT, BF16)
            vstr = Ring(A, 2, NT, BF16)
            for h in range(H):
                wq, rwq = wqr.next()
                wq3 = wq.rearrange("p (k n) -> p k n", k=4)
                src = w_uq[l]
                P.dma("pool", wq3[:, :, 0:192], src[:, h * 192:(h + 1) * 192].rearrange("(k p) n -> p k n", p=128), writes=[rwq])
                for (dc, sc) in PERM:
                    P.dma("pool", wq3[:, :, 192 + dc:192 + dc + 16], src[:, h * 192 + 128 + sc:h * 192 + 128 + sc + 16].rearrange("(k p) n -> p k n", p=128), writes=[rwq])
                wk, rwk = wkr2.next()
                wk3 = wk.rearrange("p (k n) -> p k n", k=4)
                P.dma("pool", wk3, w_ukv[l][:, h * 256:(h + 1) * 256].rearrange("(k p) n -> p k n", p=128), writes=[rwk])
                sqn, rsqn = str_.next()
                sqr_, rsqr = str_.next()
                skn, rskn = str_.next()
                for (t0, n) in TOKG:
                    bank = bi % 4; bi += 1
                    lin_fm(wq3, rwq, 4, 128, t0, n, bank, DQK[0], r_dqk[0])
                    act(sqn[:, t0:t0 + n], ps[bank][:, 0:n], AF.Copy, [rps[bank]], [rsqn], scale=MLA_SCALE)
                    ba = bi % 4; bi += 1
                    bb = bi % 4; bi += 1
                    lin_fm(wq3, rwq, 4, 64, t0, n, ba, DQK[0], r_dqk[0], m0=128)
                    lin_fm(wq3, rwq, 4, 64, t0, n, bb, DQK[0], r_dqk[0], m0=192)
                    rope_evac(ba, bb, n, t0, sqr_, rsqr, MLA_SCALE)
                    bank = bi % 4; bi += 1
                    lin_fm(wk3, rwk, 4, 128, t0, n, bank, DQK[1], r_dqk[1])
                    P.op("dve", lambda e, skn=skn, t0=t0, n=n, bank=bank: e.tensor_copy(skn[:, t0:t0 + n], ps[bank][:, 0:n]), reads=[rps[bank]], writes=[rskn])
                P.dma("sp", QNT[h], sqn, reads=[rsqn])
                P.dma("sp", QRT[h], sqr_[0:64, :], reads=[rsqr])
                P.dma("sp", KNT[h], skn, reads=[rskn])
                vs, rvs = vstr.next()
                vs3 = vs.rearrange("p (t d) -> p t d", d=128)
                for t in range(NTILE):
                    bank = 4 + (t % 2)
                    for k in range(4):
                        mm(ps[bank][:, 0:128], DQK[1][:, k, t * 128:(t + 1) * 128], wk3[:, k, 128:256], k == 0, k == 3, [rwk, r_dqk[1]], [rps[bank]])
                    act(vs3[:, t, :], ps[bank][:, 0:128], AF.Copy, [rps[bank]], [rvs])
                P.dma("sp", VM2[h].rearrange("(t p) d -> p t d", p=128), vs3, reads=[rvs])
            release()

        def phase_mla(l):
            A.mark()
            qtiles = list(range(NTILE)) if l == 0 else list(range(CTXT, NTILE))
            kn_ = Ring(A, 2, NT, BF16); qn_ = Ring(A, 2, NT, BF16); qr_ = Ring(A, 2, NT, BF16); v_ = Ring(A, 2, NT, BF16)
            p_ = Ring(A, 2, NT, BF16); pt_ = Ring(A, 2, NT, BF16); yb_ = Ring(A, 2, NT, BF16)
            sm_ = Ring(A, 3, 8, F32); ob_ = Ring(A, 2, 128, BF16)
            ei = 0
            for h in range(H):
                knt, rkn = kn_.next(); qnt, rqn = qn_.next(); qrt, rqr = qr_.next(); v, rv = v_.next()
                P.dma("sp", knt, KNT[h], writes=[rkn])
                P.dma("sp", qnt, QNT[h], writes=[rqn])
                P.dma("sp", qrt[0:64, :], QRT[h], writes=[rqr])
                v3 = v.rearrange("p (t d) -> p t d", d=128)
                P.dma("sp", v3, VM2[h].rearrange("(t p) d -> p t d", p=128), writes=[rv])
                yb, ryb = yb_.next()
                for qt in qtiles:
                    qc = slice(qt * 128, (qt + 1) * 128)
                    nk = 256 if qt < CTXT else NT
                    groups = [(0, 256)] if qt < CTXT else [(0, 512), (512, 512), (1024, 512), (1536, 512), (2048, 256)]
                    for gi, (k0, n) in enumerate(groups):
                        mm(ps[gi][:, 0:n], qnt[:, qc], knt[:, k0:k0 + n], True, False, [rqn, rkn], [rps[gi]])
                        mm(ps[gi][:, 0:n], qrt[0:64, qc], KRT[0:64, k0:k0 + n], False, True, [rqr, r_krt], [rps[gi]])
                    S = ps_all[:, 0:nk]
                    rS = [rps[gi] for gi in range(len(groups))]
                    s, rs = sm_.next()
                    P.op("dve", lambda e, s=s, S=S: e.reduce_max(s[:, 0:1], S, AX.X), reads=rS, writes=[rs])
                    ts("dve", s[:, 1:2], s[:, 0:1], -1.0, None, ALU.mult, None, [rs], [rs])
                    p, rp = p_.next()
                    act(p[:, 0:nk], S, AF.Exp, rS + [rs], [rp, rs], bias=s[:, 1:2], scale=1.0, accum_out=s[:, 2:3])
                    P.op("dve", lambda e, s=s: e.reciprocal(s[:, 3:4], s[:, 2:3]), reads=[rs], writes=[rs])
                    pt, rpt = pt_.next()
                    pt3 = pt.rearrange("p (t q) -> p t q", q=128)
                    nkt = nk // 128
                    for g in range((nkt + 3) // 4):
                        bank = 5 + (g % 2)
                        nn = min(4, nkt - 4 * g)
                        for j in range(nn):
                            kt = 4 * g + j
                            tr(psb[bank][:, j * 128:(j + 1) * 128], p[:, kt * 128:(kt + 1) * 128], [rp], [rps[bank]])
                        src = psb[bank][:, 0:nn * 128].rearrange("p (j t) -> p j t", j=nn)
                        ei += 1
                        if ei % 2:
                            P.op("dve", lambda e, pt3=pt3, g=g, nn=nn, src=src: e.tensor_copy(pt3[:, 4 * g:4 * g + nn, :], src), reads=[rps[bank]], writes=[rpt])
                        else:
                            act(pt3[:, 4 * g:4 * g + nn, :], src, AF.Copy, [rps[bank]], [rpt])
                    for kt in range(nkt):
                        mm(ps[7][:, 0:128], pt3[:, kt, :], v3[:, kt, :], kt == 0, kt == nkt - 1, [rpt, rv], [rps[7]])
                    ob, rob = ob_.next()
                    act(ob, ps[7][:, 0:128], AF.Copy, [rps[7], rs], [rob], scale=s[:, 3:4])
                    tr(psb[7][:, 512:640], ob, [rob], [rps[7]])
                    P.op("dve", lambda e, yb=yb, qc=qc: e.tensor_copy(yb[:, qc], psb[7][:, 512:640]), reads=[rps[7]], writes=[ryb])
                if l == 1:
                    P.op("dve", lambda e, yb=yb: e.memset(yb[:, 0:256], 0.0), writes=[ryb])
                P.dma("sp", YBT[h], yb, reads=[ryb])
            release()

        def phase_gdn(l):
            A.mark()
            G = 4
            S32 = A.alloc(32 * 128, F32).rearrange("p (c v) -> p c v", v=128)
            Sbf = A.alloc(32 * 128, BF16).rearrange("p (c v) -> p c v", v=128)
            r_S = [Res() for _ in range(32)]
            P.op("dve", lambda e: e.memset(S32, 0.0), writes=r_S)
            P.op("dve", lambda e: e.memset(Sbf, 0.0), writes=r_S)
            order = {0: list(range(NTILE)), 1: [1, 0] + list(range(NTILE - 1, 1, -1))}
            qT_ = Ring(A, 2, 2048, BF16); kM_ = Ring(A, 2, 2048, BF16); vM_ = Ring(A, 2, 2048, BF16)
            kF_ = Ring(A, 2, 2048, F32)
            o_ = Ring(A, 2, 2048, F32)
            sm_ = Ring(A, 2, 16 * 10, F32)
            mf_ = Ring(A, 96, 128, F32)
            LLB = [[{k: (A.alloc(128, BF16), Res()) for k in ("AT", "wT", "vnew", "kd0", "kd1")} for _ in range(G)] for _ in range(2)]
            LLF = [[{k: (A.alloc(128, F32), Res()) for k in ("u", "tmp")} for _ in range(G)] for _ in range(2)]
            for b_, r_ in mf_.bufs:
                P.op("dve", lambda e, b_=b_: e.memset(b_, 0.0), writes=[r_])
            for par in LLB + LLF:
                for dct in par:
                    for (b_, r_) in dct.values():
                        P.op("dve", lambda e, b_=b_: e.memset(b_, 0.0), writes=[r_])
            slots = [(ps[b][:, 0:128], rps[b]) for b in range(8)]
            sl = [0]

            def slot():
                x = slots[sl[0] % 8]
                sl[0] += 1
                return x

            def cp_act(dst, rdst, src, rsrc):
                act(dst, src, AF.Copy, [rsrc], [rdst])

            def cp_dve(dst, rdst, src, rsrc):
                P.op("dve", lambda e: e.tensor_copy(dst, src), reads=[rsrc], writes=[rdst])
            gpar = [0]
            for s in range(GDN_STEPS):
                for d in (0, 1):
                    t = order[d][s]
                    tc = slice(t * 128, (t + 1) * 128)
                    qTt, rqT = qT_.next(); kMt, rkM = kM_.next(); vMt, rvM = vM_.next()
                    qT3 = qTt.rearrange("p (h t) -> p h t", h=16)
                    kM3 = kMt.rearrange("p (h t) -> p h t", h=16); vM3 = vMt.rearrange("p (h t) -> p h t", h=16)
                    P.dma("sp", qT3, QT[:, :, tc].rearrange("h p t -> p h t"), writes=[rqT])
                    kFt, rkF = kF_.next()
                    kF3 = kFt.rearrange("p (h t) -> p h t", h=16)
                    P.dma("sp", kF3, KT32[:, :, tc].rearrange("h p t -> p h t"), writes=[rkF])
                    P.dma("sp", kMt, KM[tc].rearrange("p h d -> p (h d)"), writes=[rkM])
                    P.dma("sp", vMt, VM[tc].rearrange("p h d -> p (h d)"), writes=[rvM])
                    sm, rsm = sm_.next()
                    smv = sm.rearrange("p (c h) -> p c h", h=16)
                    GC, GB, NGC, EGC, EGB, KD0, EGL0, EGL1, EB, KD1 = [smv[:, i, :] for i in range(10)]
                    gsl = g_all[:, t, d * 16:(d + 1) * 16]
                    pg, rpg = slot()
                    mm(pg[:, 0:16], MCUM[d], gsl, True, True, [r_const, r_gall], [rpg])
                    mm(pg[:, 16:32], MCH[0], gsl, True, True, [r_const, r_gall], [rpg])
                    mm(pg[:, 32:48], MCH[1], gsl, True, True, [r_const, r_gall], [rpg])
                    P.op("dve", lambda e, GC=GC, pg=pg: e.tensor_copy(GC, pg[:, 0:16]), reads=[rpg], writes=[rsm])
                    tt("dve", GB, GC, lnb_all[:, t, d * 16:(d + 1) * 16], ALU.add, [rsm, r_gall], [rsm])
                    ts("dve", NGC, GC, -1.0, None, ALU.mult, None, [rsm], [rsm])
                    P.op("dve", lambda e, KD0=KD0: e.memset(KD0, 0.0), writes=[rsm])
                    P.op("dve", lambda e, KD1=KD1: e.memset(KD1, 0.0), writes=[rsm])
                    tt("dve", KD0[0:64, :], pg[0:64, 16:32], GC[0:64, :], ALU.subtract, [rpg, rsm], [rsm])
                    tt("dve", KD1[64:128, :], pg[64:128, 32:48], GC[64:128, :], ALU.subtract, [rpg, rsm], [rsm])
                    act(EGC, GC, AF.Exp, [rsm], [rsm])
                    act(EGB, GB, AF.Exp, [rsm], [rsm])
                    act(EB, lnb_all[:, t, d * 16:(d + 1) * 16], AF.Exp, [rsm, r_gall], [rsm])
                    act(KD0[0:64, :], KD0[0:64, :], AF.Exp, [rsm], [rsm])
                    act(KD1[64:128, :], KD1[64:128, :], AF.Exp, [rsm], [rsm])
                    act(EGL0, pg[:, 16:32], AF.Exp, [rpg, rsm], [rsm])
                    act(EGL1, pg[:, 32:48], AF.Exp, [rpg, rsm], [rsm])
                    EGL = (EGL0, EGL1)
                    KD = (KD0, KD1)
                    ot, ro = o_.next()
                    o3 = ot.rearrange("p (h v) -> p h v", h=16)
                    for hg in range(0, GDN_HEADS, G):
                        hs = list(range(hg, min(hg + G, GDN_HEADS)))
                        par = gpar[0] % 2
                        gpar[0] += 1
                        C = {h: {} for h in hs}
                        for gi, h in enumerate(hs):
                            C[h]["B"] = LLB[par][gi]; C[h]["F"] = LLF[par][gi]
                        for h in hs:
                            c = C[h]; hc = slice(h, h + 1)
                            c["dg1"] = mf_.next(); c["dg2"] = mf_.next()
                            act(c["dg1"][0], identf, AF.Copy, [r_const, rsm], [c["dg1"][1]], scale=NGC[:, hc])
                            act(c["dg2"][0], identf, AF.Copy, [r_const, rsm], [c["dg2"][1]], scale=GC[:, hc])
                        for h in hs:
                            c = C[h]; hc = slice(h, h + 1)
                            pD, rD = slot()
                            mm(pD, ONESF, c["dg1"][0], True, False, [r_const, c["dg1"][1]], [rD])
                            mm(pD, identf, NEGM_P[d], False, True, [r_const], [rD])
                            c["DP"] = mf_.next()
                            act(c["DP"][0], pD, AF.Exp, [rD, rsm], [c["DP"][1]], bias=GB[:, hc], scale=1.0)
                        for h in hs:
                            c = C[h]; hc = slice(h, h + 1)
                            pD2, rD2 = slot()
                            mm(pD2, ONESF, c["dg2"][0], True, False, [r_const, c["dg2"][1]], [rD2])
                            mm(pD2, identf, NEGM_AT[d], False, True, [r_const], [rD2])
                            c["DAT"] = mf_.next()
                            act(c["DAT"][0], pD2, AF.Exp, [rD2, rsm], [c["DAT"][1]], bias=NGC[:, hc], scale=1.0)
                        for h in hs:
                            c = C[h]
                            pK, rK = slot(); mm(pK, kF3[:, h, :], kF3[:, h, :], True, True, [rkF], [rK])
                            c["P"] = mf_.next()
                            stt("dve", c["P"][0], pK, -1.0, c["DP"][0], ALU.mult, ALU.mult, [rK, c["DP"][1]], [c["P"][1]])
                        for h in hs:
                            c = C[h]
                            c["kb"] = mf_.next()
                        for h in hs:
                            c = C[h]
                            kb = c["kb"][0].bitcast(BF16)[:, 0:128]
                            cp_act(kb, c["kb"][1], kF3[:, h, :], rkF)
                        for h in hs:
                            c = C[h]
                            kb = c["kb"][0].bitcast(BF16)[:, 0:128]
                            pQ, rQ = slot(); mm(pQ, kb, qT3[:, h, :], True, True, [c["kb"][1], rqT], [rQ])
                            AT, rAT = c["B"]["AT"]
                            tt("dve", AT, pQ, c["DAT"][0], ALU.mult, [rQ, c["DAT"][1]], [rAT])
                        for h in hs:
                            c = C[h]
                            pT, rT = slot()
                            tr(pT, c["P"][0], [c["P"][1]], [rT], f32=True)
                            c["Q"] = mf_.next()
                            cp_act(c["Q"][0], c["Q"][1], pT, rT)
                        for h in hs:
                            c = C[h]
                            c["TT"] = mf_.next()
                            tt("pool", c["TT"][0], c["Q"][0], identf, ALU.add, [c["Q"][1], r_const], [c["TT"][1]])
                        for j in range(5):
                            for h in hs:
                                c = C[h]
                                p1, r1 = slot(); mm(p1, c["Q"][0], c["P"][0], True, True, [c["Q"][1], c["P"][1]], [r1])
                                c["Pn"] = mf_.next()
                                (cp_act if j % 2 else cp_dve)(c["Pn"][0], c["Pn"][1], p1, r1)
                            if j < 4:
                                for h in hs:
                                    c = C[h]
                                    p2, r2 = slot(); mm(p2, c["P"][0], c["Q"][0], True, True, [c["Q"][1], c["P"][1]], [r2])
                                    c["Qn"] = mf_.next()
                                    (cp_dve if j % 2 else cp_act)(c["Qn"][0], c["Qn"][1], p2, r2)
                            for h in hs:
                                c = C[h]
                                p3, r3 = slot(); mm(p3, c["Pn"][0], c["TT"][0], True, True, [c["Pn"][1], c["TT"][1]], [r3])
                                c["TTn"] = mf_.next()
                                tt("dve", c["TTn"][0], p3, c["TT"][0], ALU.add, [r3, c["TT"][1]], [c["TTn"][1]])
                            for h in hs:
                                c = C[h]
                                c["P"] = c["Pn"]; c["TT"] = c["TTn"]
                                if j < 4:
                                    c["Q"] = c["Qn"]
                        for h in hs:
                            c = C[h]; hc = slice(h, h + 1)
                            c["vb"] = mf_.next(); c["kbg"] = mf_.next()
                            act(c["vb"][0], vM3[:, h, :], AF.Copy, [rvM, rsm], [c["vb"][1]], scale=EB[:, hc])
                            act(c["kbg"][0], kM3[:, h, :], AF.Copy, [rkM, rsm], [c["kbg"][1]], scale=EGB[:, hc])
                            for ci in (0, 1):
                                kd, rkd = c["B"]["kd%d" % ci]
                                ts("dve", kd, kM3[:, h, :], KD[ci][:, hc], None, ALU.mult, None, [rkM, rsm], [rkd])
                        for h in hs:
                            c = C[h]
                            pU, rU = slot(); mm(pU, c["TT"][0], c["vb"][0], True, True, [c["TT"][1], c["vb"][1]], [rU])
                            u, ru = c["F"]["u"]
                            cp_dve(u, ru, pU, rU)
                        for h in hs:
                            c = C[h]
                            pW, rW = slot(); mm(pW, c["kbg"][0], c["TT"][0], True, True, [c["TT"][1], c["kbg"][1]], [rW])
                            wT, rwT = c["B"]["wT"]
                            cp_act(wT, rwT, pW, rW)
                        for ci in ((0, 1) if d == 0 else (1, 0)):
                            pr = slice(ci * 64, ci * 64 + 64)
                            for h in hs:
                                c = C[h]; dh = d * 16 + h
                                wT, rwT = c["B"]["wT"]; u, ru = c["F"]["u"]; vnew, rvn = c["B"]["vnew"]
                                pv, rv_ = slot()
                                mm(pv, wT, Sbf[:, dh, :], True, True, [rwT, r_S[dh]], [rv_])
                                tt("dve", vnew[pr, :], u[pr, :], pv[pr, :], ALU.subtract, [ru, rv_], [rvn])
                            for h in hs:
                                c = C[h]; dh = d * 16 + h; hc = slice(h, h + 1)
                                tmp, rtmp = c["F"]["tmp"]
                                po1, ro1 = slot()
                                mm(po1, qT3[:, h, :], Sbf[:, dh, :], True, True, [rqT, r_S[dh]], [ro1])
                                act(tmp[pr, :], po1[pr, :], AF.Copy, [ro1, rsm], [rtmp], scale=EGC[pr, hc])
                            for h in hs:
                                c = C[h]
                                AT, rAT = c["B"]["AT"]; vnew, rvn = c["B"]["vnew"]; tmp, rtmp = c["F"]["tmp"]
                                po2, ro2 = slot()
                                mm(po2, AT, vnew, True, True, [rAT, rvn], [ro2])
                                tt("dve", o3[pr, h, :], tmp[pr, :], po2[pr, :], ALU.add, [rtmp, ro2], [ro])
                            for h in hs:
                                c = C[h]; dh = d * 16 + h; hc = slice(h, h + 1)
                                kd, rkd = c["B"]["kd%d" % ci]; vnew, rvn = c["B"]["vnew"]
                                pS, rS_ = slot()
                                mm(pS, kd, vnew, True, True, [rkd, rvn], [rS_])
                                stt("dve", S32[:, dh, :], S32[:, dh, :], EGL[ci][:, hc], pS, ALU.mult, ALU.add, [r_S[dh], rsm, rS_], [r_S[dh]])
                                act(Sbf[:, dh, :], S32[:, dh, :], AF.Copy, [r_S[dh]], [r_S[dh]])
                    P.dma("sp", OFB[d, tc, :], ot, reads=[ro])
            release()

        def phase_gdn_out(l):
            A.mark()
            tiles = list(range(NTILE)) if l == 0 else list(range(CTXT, NTILE))
            gnb = A.alloc(128, F32); r_gn = Res()
            P.dma("sp", gnb, gdn_norm[l].partition_broadcast(128), writes=[r_gn])
            of_ = Ring(A, 2, 2048, F32); ob_ = Ring(A, 2, 2048, F32); z_ = Ring(A, 2, 2048, F32)
            sq_ = Ring(A, 1, 2048, F32); ss_ = Ring(A, 2, 32, F32); yb_ = Ring(A, 2, 2048, BF16); yt_ = Ring(A, 2, 2048, BF16)
            for t in tiles:
                tc = slice(t * 128, (t + 1) * 128)
                of, rof = of_.next(); ob, rob = ob_.next(); z, rz = z_.next()
                P.dma("sp", of, OFB[0, tc, :], writes=[rof])
                P.dma("sp", ob, OFB[1, tc, :], writes=[rob])
                P.dma("sp", z, ZS[tc, :], writes=[rz])
                tt("pool", of, of, ob, ALU.add, [rof, rob], [rof])
                sq, rsq = sq_.next()
                act(sq, of, AF.Square, [rof], [rsq])
                ss, rss = ss_.next()
                P.op("dve", lambda e, ss=ss, sq=sq: e.reduce_sum(ss[:, 0:16], sq.rearrange("p (h v) -> p h v", h=16), AX.X), reads=[rsq], writes=[rss])
                ts("dve", ss[:, 0:16], ss[:, 0:16], 1.0 / 128, EPS, ALU.mult, ALU.add, [rss], [rss])
                act(ss[:, 0:16], ss[:, 0:16], AF.Sqrt, [rss], [rss])
                P.op("dve", lambda e, ss=ss: e.reciprocal(ss[:, 0:16], ss[:, 0:16]), reads=[rss], writes=[rss])
                for h in range(H):
                    hs = slice(h * 128, (h + 1) * 128)
                    stt("dve", of[:, hs], of[:, hs], ss[:, h:h + 1], gnb, ALU.mult, ALU.mult, [rof, rss, r_gn], [rof])
                yb, ryb = yb_.next()
                tt("dve", yb, of, z, ALU.mult, [rof, rz], [ryb])
                yt, ryt = yt_.next()
                yt3 = yt.rearrange("p (h t) -> p h t", h=16)
                for g in range(4):
                    bank = 6 + (g % 2)
                    for j in range(4):
                        k = g * 4 + j
                        tr(psb[bank][:, j * 128:(j + 1) * 128], yb[:, k * 128:(k + 1) * 128], [ryb], [rps[bank]])
                    act(yt3[:, g * 4:(g + 1) * 4, :], psb[bank][:, 0:512].rearrange("p (j t) -> p j t", j=4), AF.Copy, [rps[bank]], [ryt])
                P.dma("sp", YAT[:, :, tc].rearrange("h p t -> p h t"), yt3, reads=[ryt])
            release()

        def resid_ln(xt, rx, r_, rr, gtb, gb_, bb_, r_b, dst, sr):
            tt("pool", r_, r_, gtb, ALU.mult, [rr, r_b], [rr])
            stt("dve", xt, xt, ALPHA, r_, ALU.mult, ALU.add, [rx, rr], [rx])
            s, rs = sr.next()
            st6 = s[:, 0:24].rearrange("p (c s) -> p c s", s=6)
            xv = xt.rearrange("p (c f) -> p c f", f=512)
            for c in range(4):
                P.op("dve", lambda e, c=c: e.bn_stats(st6[:, c, :], xv[:, c, :]), reads=[rx], writes=[rs])
            P.op("dve", lambda e: e.bn_aggr(s[:, 24:26], st6), reads=[rs], writes=[rs])
            ts("dve", s[:, 26:27], s[:, 25:26], EPS, None, ALU.add, None, [rs], [rs])
            act(s[:, 26:27], s[:, 26:27], AF.Sqrt, [rs], [rs])
            P.op("dve", lambda e: e.reciprocal(s[:, 26:27], s[:, 26:27]), reads=[rs], writes=[rs])
            ts("dve", xt, xt, s[:, 24:25], s[:, 26:27], ALU.subtract, ALU.mult, [rx, rs], [rx])
            tt("pool", xt, xt, gb_, ALU.mult, [rx, r_b], [rx])
            tt("dve", xt, xt, bb_, ALU.add, [rx, r_b], [rx])
            P.dma("sp", dst, xt, reads=[rx])

        def load_bcast(l, gt_idx, g_d, b_d):
            r_b = Res()
            gts = []
            for r in (0, 1):
                a = A.alloc(2048, F32)
                P.dma("sp", a, MODV[l, r, gt_idx * 2048:(gt_idx + 1) * 2048].partition_broadcast(128), writes=[r_b])
                gts.append(a)
            gb_ = A.alloc(2048, F32); bb_ = A.alloc(2048, F32)
            P.dma("sp", gb_, g_d[l].partition_broadcast(128), writes=[r_b])
            P.dma("sp", bb_, b_d[l].partition_broadcast(128), writes=[r_b])
            return gts, gb_, bb_, r_b

        def phase_merge(l):
            A.mark()
            groups = TOKG if l == 0 else TOKG[1:]
            gts, gb_, bb_, r_b = load_bcast(l, 2, ln1_g, ln1_b)
            ya_ = Ring(A, 1, 16 * 512, BF16); yb_ = Ring(A, 1, 16 * 512, BF16); yT_ = Ring(A, 1, 16 * 512, BF16)
            w_ = Ring(A, 4, 16 * 128, BF16); g_ = Ring(A, 4, 512, BF16); t_ = Ring(A, 4, 512, F32)
            wo_ = Ring(A, 2, 16 * 256, BF16)
            m_ = Ring(A, 4, 2048, F32); x_ = Ring(A, 1, 2048, F32); sr = Ring(A, 2, 32, F32)
            bi = 0
            for (t0, n) in groups:
                ya, rya = ya_.next(); yb, ryb = yb_.next(); yT, ryT = yT_.next()
                ya3 = ya.rearrange("p (k t) -> p k t", k=16)[:, :, 0:n]; yb3 = yb.rearrange("p (k t) -> p k t", k=16)[:, :, 0:n]
                yT3 = yT.rearrange("p (k t) -> p k t", k=16)
                P.dma("sp", ya3, YAT[:, :, t0:t0 + n].rearrange("h p t -> p h t"), writes=[rya])
                P.dma("sp", yb3, YBT[:, :, t0:t0 + n].rearrange("h p t -> p h t"), writes=[ryb])
                for m in range(16):
                    wa, rwa = w_.next(); wb, rwb = w_.next()
                    wa3 = wa.rearrange("p (k n) -> p k n", k=16); wb3 = wb.rearrange("p (k n) -> p k n", k=16)
                    wcast(wa3, w_br_a[l][:, m * 128:(m + 1) * 128], rwa)
                    wcast(wb3, w_br_b[l][:, m * 128:(m + 1) * 128], rwb)
                    ga, rga = g_.next(); gb2, rgb = g_.next()
                    P.dma("sp", ga[:, 0:n], GT[m][:, t0:t0 + n], writes=[rga])
                    P.dma("sp", gb2[:, 0:n], GT[16 + m][:, t0:t0 + n], writes=[rgb])
                    ba = bi % 6; bi += 1
                    bb = bi % 6; bi += 1
                    for k in range(16):
                        mm(ps[ba][:, 0:n], wa3[:, k, :], ya3[:, k, :], k == 0, k == 15, [rwa, rya], [rps[ba]])
                    for k in range(16):
                        mm(ps[bb][:, 0:n], wb3[:, k, :], yb3[:, k, :], k == 0, k == 15, [rwb, ryb], [rps[bb]])
                    t1, rt1 = t_.next(); t2, rt2 = t_.next()
                    tt("dve", t1[:, 0:n], ps[ba][:, 0:n], ga[:, 0:n], ALU.mult, [rps[ba], rga], [rt1])
                    tt("dve", t2[:, 0:n], ps[bb][:, 0:n], gb2[:, 0:n], ALU.mult, [rps[bb], rgb], [rt2])
                    tt("pool", yT3[:, m, 0:n], t1[:, 0:n], t2[:, 0:n], ALU.add, [rt1, rt2], [ryT])
                ntl = n // 128
                ms = [m_.next() for _ in range(ntl)]
                for cb in range(8):
                    wo, rwo = wo_.next()
                    wo3 = wo.rearrange("p (k n) -> p k n", k=16)
                    wcast(wo3, w_out[l][:, cb * 256:(cb + 1) * 256], rwo)
                    for ti in range(ntl):
                        bank = bi % 6; bi += 1
                        for k in range(16):
                            mm(ps[bank][:, 0:256], yT3[:, k, ti * 128:(ti + 1) * 128], wo3[:, k, :], k == 0, k == 15, [rwo, ryT], [rps[bank]])
                        act(ms[ti][0][:, cb * 256:(cb + 1) * 256], ps[bank][:, 0:256], AF.Copy, [rps[bank]], [ms[ti][1]])
                for ti in range(ntl):
                    t = t0 // 128 + ti
                    xt, rx = x_.next()
                    P.dma("sp", xt, XS[t * 128:(t + 1) * 128, :], writes=[rx])
                    resid_ln(xt, rx, ms[ti][0], ms[ti][1], gts[1 if t < CTXT else 0], gb_, bb_, r_b, XS[t * 128:(t + 1) * 128, :], sr)
            release()

        def phase_ffn(l, last):
            moe = (l % 2 == 1)
            tiles = list(range(NTILE)) if not last else list(range(CTXT, NTILE))
            ntl = len(tiles); ntok = ntl * 128; tb = tiles[0]
            E = NEXP if moe else 1
            FFC = (FF_EXP if moe else FF_DENSE) // 128
            A.mark()
            hT = A.alloc(16 * ntok, BF16).rearrange("p (k t) -> p k t", k=16); r_hT = Res()
            A.mark()
            extra = None
            if moe:
                rwt = A.alloc(128, F32); r_rw = Res()
                P.dma("sp", rwt, router, writes=[r_rw])
                rw3 = rwt.rearrange("p (k e) -> p k e", e=8)
                hf_ = Ring(A, 1, 2048, F32); lg_ = Ring(A, 2, 40, F32)

                def extra(t, xt, rx):
                    ti = t - tb
                    hf, rhf = hf_.next()
                    hf3 = hf.rearrange("p (k t) -> p k t", k=16)
                    for g in range(4):
                        bank = g % 4
                        for j in range(4):
                            k = g * 4 + j
                            tr(ps[bank][:, j * 128:(j + 1) * 128], xt[:, k * 128:(k + 1) * 128], [rx], [rps[bank]], f32=True)
                        act(hf3[:, g * 4:(g + 1) * 4, :], ps[bank].rearrange("p (j t) -> p j t", j=4), AF.Copy, [rps[bank]], [rhf])
                    for k in range(16):
                        mm(ps[4][:, 0:8], hf3[:, k, :], rw3[:, k, :], k == 0, k == 15, [rhf, r_rw], [rps[4]])
                    lg, rlg = lg_.next()
                    P.op("dve", lambda e: e.tensor_copy(lg[:, 0:8], ps[4][:, 0:8]), reads=[rps[4]], writes=[rlg])
                    P.op("dve", lambda e: e.max(lg[:, 8:16], lg[:, 0:8]), reads=[rlg], writes=[rlg])
                    ts("dve", lg[:, 16:24], lg[:, 0:8], lg[:, 9:10], None, ALU.is_ge, None, [rlg], [rlg])
                    ts("dve", lg[:, 32:33], lg[:, 8:9], -1.0, None, ALU.mult, None, [rlg], [rlg])
                    act(lg[:, 24:32], lg[:, 0:8], AF.Exp, [rlg], [rlg], bias=lg[:, 32:33], scale=1.0)
                    tt("dve", lg[:, 24:32], lg[:, 24:32], lg[:, 16:24], ALU.mult, [rlg], [rlg])
                    P.op("dve", lambda e: e.reduce_sum(lg[:, 33:34], lg[:, 24:32], AX.X), reads=[rlg], writes=[rlg])
                    P.op("dve", lambda e: e.reciprocal(lg[:, 33:34], lg[:, 33:34]), reads=[rlg], writes=[rlg])
                    ts("dve", comb[:, ti, :], lg[:, 24:32], lg[:, 33:34], None, ALU.mult, None, [rlg], [r_comb])
            ln_mod_T(l, 1, tiles, hT, r_hT, lambda t: (t - tb) * 128, extra=extra)
            release()
            A.mark()
            w1_ = Ring(A, 2, 16 * 512, BF16); w3_ = Ring(A, 2, 16 * 512, BF16)
            s1_ = Ring(A, 3, 512, F32); st_ = Ring(A, 3, ntok, BF16)
            tgs = [(c0, min(512, ntok - c0)) for c0 in range(0, ntok, 512)]
            bi = 0
            for e in range(E):
                W1 = moe_w1[0, e] if moe else ffn_w1[0]
                W3 = moe_w3[0, e] if moe else ffn_w3[0]
                for fb in range((FFC + 3) // 4):
                    nfc = min(4, FFC - 4 * fb)
                    w1, rw1 = w1_.next(); w3, rw3_ = w3_.next()
                    w13 = w1.rearrange("p (k n) -> p k n", k=16)[:, :, 0:nfc * 128]; w33 = w3.rearrange("p (k n) -> p k n", k=16)[:, :, 0:nfc * 128]
                    wcast(w13, W1[:, fb * 512:fb * 512 + nfc * 128], rw1)
                    wcast(w33, W3[:, fb * 512:fb * 512 + nfc * 128], rw3_)
                    for fc in range(nfc):
                        stg, rst = st_.next()
                        for (c0, n) in tgs:
                            b1 = bi % 8; bi += 1
                            b3 = bi % 8; bi += 1
                            for k in range(16):
                                mm(ps[b1][:, 0:n], w13[:, k, fc * 128:(fc + 1) * 128], hT[:, k, c0:c0 + n], k == 0, k == 15, [rw1, r_hT], [rps[b1]])
                            for k in range(16):
                                mm(ps[b3][:, 0:n], w33[:, k, fc * 128:(fc + 1) * 128], hT[:, k, c0:c0 + n], k == 0, k == 15, [rw3_, r_hT], [rps[b3]])
                            s1, rs1 = s1_.next()
                            act(s1[:, 0:n], ps[b1][:, 0:n], AF.Silu, [rps[b1]], [rs1])
                            tt("dve", stg[:, c0:c0 + n], s1[:, 0:n], ps[b3][:, 0:n], ALU.mult, [rs1, rps[b3]], [rst])
                        P.dma("sp", ATS[e, fb * 4 + fc, :, 0:ntok], stg, reads=[rst])
            release()
            release()
            A.mark()
            HF = FFC // 2
            w2_ = Ring(A, 2, HF * 512, BF16); at_ = Ring(A, 3, HF * 128, BF16)
            acc = A.alloc(ntl * 512, F32).rearrange("p (t n) -> p t n", n=512); r_acc = [Res() for _ in range(ntl)]
            for cb in range(4):
                for e in range(E):
                    W2 = moe_w2[0, e] if moe else ffn_w2[0]
                    for half in range(2):
                        w2, rw2 = w2_.next()
                        w23 = w2.rearrange("p (f n) -> p f n", n=512)
                        wcast(w23, W2[half * HF * 128:(half + 1) * HF * 128, cb * 512:(cb + 1) * 512], rw2)
                        for ti in range(ntl):
                            at, rat = at_.next()
                            at3 = at.rearrange("p (f t) -> p f t", t=128)
                            P.dma("sp", at3, ATS[e, half * HF:(half + 1) * HF, :, ti * 128:(ti + 1) * 128].rearrange("f p t -> p f t"), writes=[rat])
                            bank = bi % 8; bi += 1
                            for f in range(HF):
                                mm(ps[bank], at3[:, f, :], w23[:, f, :], f == 0, f == HF - 1, [rat, rw2], [rps[bank]])
                            first = (e == 0 and half == 0)
                            if moe:
                                if first:
                                    ts("dve", acc[:, ti, :], ps[bank], comb[:, ti, e:e + 1], None, ALU.mult, None, [rps[bank], r_comb], [r_acc[ti]])
                                else:
                                    stt("dve", acc[:, ti, :], ps[bank], comb[:, ti, e:e + 1], acc[:, ti, :], ALU.mult, ALU.add, [rps[bank], r_comb, r_acc[ti]], [r_acc[ti]])
                            else:
                                if first:
                                    act(acc[:, ti, :], ps[bank], AF.Copy, [rps[bank]], [r_acc[ti]])
                                else:
                                    tt("dve", acc[:, ti, :], acc[:, ti, :], ps[bank], ALU.add, [rps[bank], r_acc[ti]], [r_acc[ti]])
                for ti in range(ntl):
                    t = tiles[ti]
                    P.dma("sp", FO[t * 128:(t + 1) * 128, cb * 512:(cb + 1) * 512], acc[:, ti, :], reads=[r_acc[ti]])
            release()
            A.mark()
            gts, gb_, bb_, r_b = load_bcast(l, 5, ln2_g, ln2_b)
            x_ = Ring(A, 2, 2048, F32); f_ = Ring(A, 2, 2048, F32); sr = Ring(A, 2, 32, F32)
            for t in tiles:
                xt, rx = x_.next(); ft, rf = f_.next()
                P.dma("sp", xt, XS[t * 128:(t + 1) * 128, :], writes=[rx])
                P.dma("sp", ft, FO[t * 128:(t + 1) * 128, :], writes=[rf])
                dst = out_d[(t - CTXT) * 128:(t - CTXT + 1) * 128, :] if last else XS[t * 128:(t + 1) * 128, :]
                resid_ln(xt, rx, ft, rf, gts[1 if t < CTXT else 0], gb_, bb_, r_b, dst, sr)
            release()

        GDN_STEPS = 1 if 'gdn_small' in dbg else NTILE
        GDN_HEADS = 1 if 'gdn_small' in dbg else H
        GDN_LVL = 99
        for x in dbg:
            if x.startswith('gdnlvl'):
                GDN_LVL = int(x[6:])
        phase_init()
        for l in range(nlayers):
            last = (l == 1)
            phase_proj(l)
            if "stop_proj" in dbg:
                break
            phase_mla(l)
            if "stop_mla" in dbg:
                break
            phase_gdn(l)
            phase_gdn_out(l)
            if "stop_gdn" in dbg:
                break
            phase_merge(l)
            if "stop_merge" in dbg:
                break
            phase_ffn(l, last)
        nsem = P.emit()
        print("built: ops", {e: len(P.ops[e]) for e in ENGS}, "nsem", nsem, flush=True)
    return nc


def make_inputs(inputs, b, consts):
    f = np.float32
    m = {}
    m["xs"] = np.ascontiguousarray(np.concatenate([inputs["ctx"][b], inputs["x"][b]], axis=0), dtype=f)
    ccv = np.stack([inputs["c"][b], inputs["c_ctx"]], axis=-1)
    m["cc"] = np.ascontiguousarray(ccv.reshape(16, 128, 2).transpose(1, 0, 2).reshape(128, 32), dtype=f)
    for k in ("w_mod", "b_mod", "w_in", "w_uq", "w_ukv", "w_br_a", "w_br_b", "w_out", "ln1_g", "ln1_b", "ln2_g", "ln2_b",
              "ffn_w1", "ffn_w3", "ffn_w2", "moe_w1", "moe_w3", "moe_w2", "gdn_norm"):
        m[k] = np.ascontiguousarray(inputs[k], dtype=f)
    m["convw"] = np.ascontiguousarray(inputs["conv_w"].reshape(2, 5, 48, 128).transpose(0, 3, 2, 1).reshape(2, 128, 240), dtype=f)
    m["a_log"] = np.ascontiguousarray(inputs["a_log"].reshape(2, 32), dtype=f)
    m["dt_bias"] = np.ascontiguousarray(inputs["dt_bias"].reshape(2, 32), dtype=f)
    qn = inputs["q_norm"].reshape(2, 4, 128).transpose(0, 2, 1)
    kn = inputs["kv_norm"].reshape(2, 4, 128).transpose(0, 2, 1)
    m["qkn"] = np.ascontiguousarray(np.concatenate([qn, kn], axis=2), dtype=f)
    m["router"] = np.ascontiguousarray(inputs["moe_router"][0].reshape(16, 128, 8).transpose(1, 0, 2).reshape(128, 128), dtype=f)
    m.update(consts)
    return m


_NC = None


def kernel(**inputs):
    global _NC
    inputs = {k: np.asarray(v) for k, v in inputs.items()}
    consts = host_consts()
    if _NC is None:
        _NC = build(bass.Bass("TRN2", target_bir_lowering=False))
    in_maps = [make_inputs(inputs, b, consts) for b in range(8)]
    res = run_bass_kernel_spmd(_NC, in_maps, core_ids=list(range(8)))
    return np.stack([np.asarray(r["out"], dtype=np.float32) for r in res.results], axis=0)
```

```python
import numpy as np
import concourse.bass as bass
import concourse.mybir as mybir
from concourse.bass_utils import run_bass_kernel_spmd
from contextlib import ExitStack

F32 = mybir.dt.float32
BF16 = mybir.dt.bfloat16
AF = mybir.ActivationFunctionType
ALU = mybir.AluOpType
AX = mybir.AxisListType

ENGS = ("pe", "act", "dve", "pool", "sp")
EPOCH = 12000
NDS = 12
DEPOCH = 1500

D = 2048
NT = 2304
NTILE = 18
CTXT = 2
H = 16
PROJ_W = 13440
FF_DENSE = 5632
FF_EXP = 7168
NEXP = 8
ALPHA = 4 ** 0.25
EPS = 1e-6
MLA_SCALE = 192 ** -0.5
NEG = -1.0e6


class Res:
    __slots__ = ("w", "rd", "name", "excl")

    def __init__(self, name="", excl=False):
        self.w = None
        self.rd = {}
        self.name = name
        self.excl = excl


class Prog:
    def __init__(self, nc, same_engine_sync=("act", "dve", "pool")):
        self.nc = nc
        self.ops = {e: [] for e in ENGS}
        self.cnt = {e: 0 for e in ENGS}
        self.dcnt = {e: 0 for e in ENGS}
        self.know = {e: {} for e in ENGS}
        self.last = {}
        self.semkeys = {}
        self.same = set(same_engine_sync)

    def _mkwaits(self, eng, deps):
        waits = {}
        kn = self.know[eng]
        for (sk, v, e) in deps:
            if e == eng and sk[0] == "c" and eng not in self.same:
                continue
            if kn.get(sk, 0) >= v:
                continue
            kn[sk] = v
            if waits.get(sk, 0) < v:
                waits[sk] = v
        return list(waits.items())

    def _deps(self, eng, reads, writes):
        deps = []
        for r in reads:
            if r.w is not None:
                deps.append(r.w)
            if r.excl:
                for sk, (v, e) in r.rd.items():
                    if e != eng:
                        deps.append((sk, v, e))
        for r in writes:
            if r.w is not None:
                deps.append(r.w)
            for sk, (v, e) in r.rd.items():
                deps.append((sk, v, e))
        return self._mkwaits(eng, deps)

    def _record(self, ident, reads, writes):
        sk, v, e = ident
        self.last[sk] = (v, e)
        for r in writes:
            r.w = ident
            r.rd = {}
        for r in reads:
            cur = r.rd.get(sk)
            if cur is None or cur[0] < v:
                r.rd[sk] = (v, e)

    def op(self, eng, fn, reads=(), writes=()):
        waits = self._deps(eng, reads, writes)
        k = self.cnt[eng]
        self.cnt[eng] = k + 1
        sk = ("c", eng, k // EPOCH)
        v = k % EPOCH + 1
        self.ops[eng].append((waits, fn, (sk, v)))
        self._record((sk, v, eng), reads, writes)

    def dma(self, q, out, in_, reads=(), writes=(), **kw):
        waits = self._deps(q, reads, writes)
        j = self.dcnt[q]
        self.dcnt[q] = j + 1
        slot = j % NDS
        use = j // NDS
        sk = ("d", q, slot, use // DEPOCH)
        v = 16 * (use % DEPOCH + 1)
        if use % DEPOCH > 0:
            pv = v - 16
            kn = self.know[q]
            if kn.get(sk, 0) < pv:
                kn[sk] = pv
                waits.append((sk, pv))

        def fn(eng, out=out, in_=in_, kw=kw):
            return eng.dma_start(out=out, in_=in_, **kw)
        self.ops[q].append((waits, fn, (sk, v)))
        self._record((sk, v, q), reads, writes)

    def barrier(self):
        deps = [(sk, v, e) for sk, (v, e) in self.last.items()]
        for eng in ENGS:
            kn = self.know[eng]
            waits = {}
            for (sk, v, e) in deps:
                if kn.get(sk, 0) >= v:
                    continue
                kn[sk] = v
                waits[sk] = v
            if waits:
                self.ops[eng].append((list(waits.items()), None, None))

    def emit(self):
        nc = self.nc
        self.barrier()
        allkeys = set()
        for e in ENGS:
            for waits, fn, inc in self.ops[e]:
                if inc is not None:
                    allkeys.add(inc[0])
        allkeys = sorted(allkeys)
        with ExitStack() as st:
            for i, sk in enumerate(allkeys):
                self.semkeys[sk] = st.enter_context(nc.semaphore("s%d" % i))
            block = st.enter_context(nc.Block())
            sem = self.semkeys

            def run(engname, eng):
                for waits, fn, inc in self.ops[engname]:
                    for sk, v in waits:
                        eng.wait_ge(sem[sk], v)
                    if fn is None:
                        continue
                    ins = fn(eng)
                    ins.then_inc(sem[inc[0]], 16 if inc[0][0] == "d" else 1)

            @block.tensor
            def _(e):
                run("pe", e)

            @block.scalar
            def _(e):
                run("act", e)

            @block.vector
            def _(e):
                run("dve", e)

            @block.gpsimd
            def _(e):
                run("pool", e)

            @block.sync
            def _(e):
                run("sp", e)
        return len(allkeys)


class Arena:
    def __init__(self, ap, nwords):
        self.ap = ap
        self.n = nwords
        self.off = 0
        self.marks = []

    def alloc(self, free_elems, dtype):
        words = (free_elems * (2 if dtype == BF16 else 4) + 3) // 4
        words = (words + 7) // 8 * 8
        assert self.off + words <= self.n, ("arena overflow", self.off, words, self.n)
        a = self.ap[:, self.off:self.off + words]
        self.off += words
        if dtype == BF16:
            a = a.bitcast(BF16)
        return a[:, 0:free_elems]

    def mark(self):
        self.marks.append(self.off)

    def release(self):
        self.off = self.marks.pop()


class Ring:
    def __init__(self, arena, n, free_elems, dtype):
        self.bufs = [(arena.alloc(free_elems, dtype), Res()) for _ in range(n)]
        self.i = 0

    def next(self):
        b = self.bufs[self.i % len(self.bufs)]
        self.i += 1
        return b


def interleave(gens, k):
    it = iter(gens)
    active = []
    more = True
    while True:
        while more and len(active) < k:
            try:
                active.append(next(it))
            except StopIteration:
                more = False
        if not active:
            break
        for g in list(active):
            try:
                next(g)
            except StopIteration:
                active.remove(g)


def host_consts():
    c = {}
    c["identf"] = np.eye(128, dtype=np.float32)
    idx = np.arange(128)
    same = (idx[:, None] // 64) == (idx[None, :] // 64)
    masks = np.zeros((12, 128, 128), np.float32)
    for d in range(2):
        aft = (idx[:, None] >= idx[None, :]) if d == 0 else (idx[:, None] <= idx[None, :])
        aft_s = (idx[:, None] > idx[None, :]) if d == 0 else (idx[:, None] < idx[None, :])
        masks[0 + d] = np.where((aft & same).T, 0.0, NEG)
        masks[2 + d] = np.where((aft_s & same), 0.0, NEG)
        masks[4 + d] = np.where((aft & same).T, 1.0, 0.0)
    masks[6] = np.where(idx[:, None] < 64, 1.0, 0.0) * np.ones((1, 128))
    masks[7] = np.where(idx[:, None] >= 64, 1.0, 0.0) * np.ones((1, 128))
    masks[8] = 1.0
    c["masks"] = masks.astype(np.float32)
    rows = 2048 // 64
    row = np.repeat(np.arange(rows), 64).astype(np.float32)
    col = np.tile(np.arange(64), rows).astype(np.float32)
    inv_freq = (10000.0 ** (-np.arange(16, dtype=np.float32) / 16)).astype(np.float32)
    cosT = np.zeros((64, 2048), np.float32)
    sinT = np.zeros((64, 2048), np.float32)
    for ax, pos in enumerate((row, col)):
        ang = (pos[None, :] * inv_freq[:, None]).astype(np.float32)
        for half in range(2):
            p0 = ax * 32 + half * 16
            cosT[p0:p0 + 16] = np.cos(ang)
            sinT[p0:p0 + 16] = np.sin(ang) * (-1.0 if half == 0 else 1.0)
    c["ropec"] = np.stack([cosT, sinT]).astype(np.float32)
    return c


def build(nc, nlayers=2, dbg=()):
    def din(name, shape):
        return nc.dram_tensor(name, list(shape), F32, kind="ExternalInput").ap()

    def dscr(name, shape, dt=F32):
        kind = "ExternalOutput" if name in dbg else "Internal"
        return nc.dram_tensor(name, list(shape), dt, kind=kind).ap()

    xs_in = din("xs", [NT, D])
    cc = din("cc", [128, 32])
    w_mod = din("w_mod", [2, D, 6 * D]); b_mod = din("b_mod", [2, 6 * D])
    w_in = din("w_in", [2, D, PROJ_W])
    convw = din("convw", [2, 128, 48 * 5])
    a_log = din("a_log", [2, 32]); dt_bias = din("dt_bias", [2, 32])
    gdn_norm = din("gdn_norm", [2, 128])
    qkn = din("qkn", [2, 128, 8])
    w_uq = din("w_uq", [2, 512, 3072]); w_ukv = din("w_ukv", [2, 512, 4096])
    w_br_a = din("w_br_a", [2, D, D]); w_br_b = din("w_br_b", [2, D, D]); w_out = din("w_out", [2, D, D])
    ln1_g = din("ln1_g", [2, D]); ln1_b = din("ln1_b", [2, D]); ln2_g = din("ln2_g", [2, D]); ln2_b = din("ln2_b", [2, D])
    ffn_w1 = din("ffn_w1", [1, D, FF_DENSE]); ffn_w3 = din("ffn_w3", [1, D, FF_DENSE]); ffn_w2 = din("ffn_w2", [1, FF_DENSE, D])
    router = din("router", [128, 16 * 8])
    moe_w1 = din("moe_w1", [1, NEXP, D, FF_EXP]); moe_w3 = din("moe_w3", [1, NEXP, D, FF_EXP]); moe_w2 = din("moe_w2", [1, NEXP, FF_EXP, D])
    identf_d = din("identf", [128, 128]); masks_d = din("masks", [12, 128, 128]); ropec_d = din("ropec", [2, 64, 2048])
    out_d = nc.dram_tensor("out", [2048, D], F32, kind="ExternalOutput").ap()

    XS = dscr("XS", [NT, D])
    MODV = dscr("MODV", [2, 2, 6 * D])
    QT = dscr("QT", [H, 128, NT], BF16); KT = dscr("KT", [H, 128, NT], BF16); KT32 = dscr("KT32", [H, 128, NT])
    KM = dscr("KM", [NT, H, 128], BF16); VM = dscr("VM", [NT, H, 128], BF16)
    ZS = dscr("ZS", [NT, D])
    GT = dscr("GT", [32, 128, NT], BF16)
    QNT = dscr("QNT", [H, 128, NT], BF16); QRT = dscr("QRT", [H, 64, NT], BF16)
    KNT = dscr("KNT", [H, 128, NT], BF16); VM2 = dscr("VM2", [H, NT, 128], BF16)
    OFB = dscr("OFB", [2, NT, H * 128])
    YAT = dscr("YAT", [H, 128, NT], BF16); YBT = dscr("YBT", [H, 128, NT], BF16)
    ATS = dscr("ATS", [NEXP, 56, 128, NT], BF16)
    FO = dscr("FO", [NT, D])

    NW = 47000
    with ExitStack() as st:
        arena_t = st.enter_context(nc.sbuf_tensor("arena", [128, NW], F32))
        ps_all = st.enter_context(nc.psum_tensor("psall", [128, 4096], F32))
        A = Arena(arena_t[:], NW)
        P = Prog(nc)
        ps = [ps_all[:, b * 512:(b + 1) * 512] for b in range(8)]
        psb = [ps_all[:, b * 512:(b + 1) * 512].bitcast(BF16) for b in range(8)]
        rps = [Res("ps%d" % b, excl=True) for b in range(8)]

        identf = A.alloc(128, F32); identb = A.alloc(128, BF16); r_const = Res("const")
        masks = [A.alloc(128, F32) for _ in range(9)]
        onecol = A.alloc(1, F32); epscol = A.alloc(1, F32)
        P.dma("sp", identf, identf_d, writes=[r_const])
        for i in range(9):
            P.dma("sp", masks[i], masks_d[i], writes=[r_const])
        P.op("dve", lambda e: e.tensor_copy(identb, identf), reads=[r_const], writes=[r_const])
        P.op("dve", lambda e: e.memset(onecol, 1.0), writes=[r_const])
        P.op("dve", lambda e: e.memset(epscol, EPS), writes=[r_const])
        NEGM_AT, NEGM_P, MCUM, MCH, ONESF = masks[0:2], masks[2:4], masks[4:6], masks[6:8], masks[8]
        g_all = A.alloc(NTILE * 32, F32).rearrange("p (t c) -> p t c", c=32); r_gall = Res("gall")
        lnb_all = A.alloc(NTILE * 32, F32).rearrange("p (t c) -> p t c", c=32)
        KRT = A.alloc(NT, BF16); r_krt = Res("krt")
        comb = A.alloc(16 * 8, F32).rearrange("p (t e) -> p t e", e=8); r_comb = Res("comb")
        P.barrier()

        def release():
            A.release()
            P.barrier()

        def mm(out, lhsT, rhs, start, stop, reads, writes):
            P.op("pe", lambda e: e.matmul(out, lhsT, rhs, start=start, stop=stop), reads=reads, writes=writes)

        def tr(out, in_, reads, writes, f32=False):
            idn = identf if f32 else identb
            P.op("pe", lambda e: e.transpose(out, in_, idn), reads=list(reads) + [r_const], writes=writes)

        def act(out, in_, func, reads, writes, **kw):
            P.op("act", lambda e: e.activation(out, in_, func, **kw), reads=reads, writes=writes)

        def tt(eng, out, in0, in1, op, reads, writes):
            P.op(eng, lambda e: e.tensor_tensor(out, in0, in1, op), reads=reads, writes=writes)

        def ts(eng, out, in0, s1, s2, op0, op1, reads, writes):
            if op1 is None:
                P.op(eng, lambda e: e.tensor_scalar(out, in0, s1, None, op0), reads=reads, writes=writes)
            else:
                P.op(eng, lambda e: e.tensor_scalar(out, in0, s1, s2, op0, op1), reads=reads, writes=writes)

        def stt(eng, out, in0, scalar, in1, op0, op1, reads, writes):
            P.op(eng, lambda e: e.scalar_tensor_tensor(out, in0, scalar, in1, op0, op1), reads=reads, writes=writes)

        def wcast(dst3, src2, rw):
            P.dma("pool", dst3, src2.rearrange("(k p) n -> p k n", p=128), writes=[rw])

        TOKG = [(0, 256), (256, 512), (768, 512), (1280, 512), (1792, 512)]

        def phase_init():
            A.mark()
            for t in range(NTILE):
                P.dma("sp", XS[t * 128:(t + 1) * 128, :], xs_in[t * 128:(t + 1) * 128, :])
            cct = A.alloc(32, F32); r_cc = Res()
            P.dma("sp", cct, cc, writes=[r_cc])
            act(cct, cct, AF.Silu, [r_cc], [r_cc])
            cv = cct.rearrange("p (k t) -> p k t", t=2)
            wr = Ring(A, 3, 16 * 512, F32)
            br = Ring(A, 3, 512, F32)
            orr = Ring(A, 3, 512, F32)
            i = 0
            for l in range(2):
                for nb in range(24):
                    wt, rw = wr.next()
                    wt3 = wt.rearrange("p (k n) -> p k n", k=16)
                    P.dma("sp", wt3, w_mod[l, :, nb * 512:(nb + 1) * 512].rearrange("(k p) n -> p k n", p=128), writes=[rw])
                    bt, rb = br.next()
                    P.dma("sp", bt[0:2, :], b_mod[l, nb * 512:(nb + 1) * 512].partition_broadcast(2), writes=[rb])
                    bank = i % 2
                    i += 1
                    for k in range(16):
                        mm(ps[bank][0:2, :], cv[:, k, :], wt3[:, k, :], k == 0, k == 15, [r_cc, rw], [rps[bank]])
                    ot, ro = orr.next()
                    tt("dve", ot[0:2, :], ps[bank][0:2, :], bt[0:2, :], ALU.add, [rps[bank], rb], [ro])
                    if nb // 4 in (1, 4):
                        ts("dve", ot[0:2, :], ot[0:2, :], 1.0, None, ALU.add, None, [ro], [ro])
                    P.dma("sp", MODV[l, :, nb * 512:(nb + 1) * 512], ot[0:2, :], reads=[ro])
            release()

        def ln_mod_T(l, which, tiles, hT, r_hT, col0, extra=None):
            shi, sci = (0, 1) if which == 0 else (3, 4)
            bA, bB = [], []
            r_b = Res()
            for r in (0, 1):
                a = A.alloc(2048, F32); b = A.alloc(2048, F32)
                P.dma("sp", a, MODV[l, r, sci * 2048:(sci + 1) * 2048].partition_broadcast(128), writes=[r_b])
                P.dma("sp", b, MODV[l, r, shi * 2048:(shi + 1) * 2048].partition_broadcast(128), writes=[r_b])
                bA.append(a); bB.append(b)
            xr = Ring(A, 2, 2048, F32)
            hbr = Ring(A, 2, 2048, BF16)
            sr = Ring(A, 2, 32, F32)
            def tile_gen(t):
                r = 1 if t < CTXT else 0
                xt, rx = xr.next()
                P.dma("sp", xt, XS[t * 128:(t + 1) * 128, :], writes=[rx])
                s, rs = sr.next()
                st6 = s[:, 0:24].rearrange("p (c s) -> p c s", s=6)
                xv = xt.rearrange("p (c f) -> p c f", f=512)
                yield
                for c in range(4):
                    P.op("dve", lambda e, c=c, st6=st6, xv=xv: e.bn_stats(st6[:, c, :], xv[:, c, :]), reads=[rx], writes=[rs])
                P.op("dve", lambda e, s=s, st6=st6: e.bn_aggr(s[:, 24:26], st6), reads=[rs], writes=[rs])
                ts("dve", s[:, 26:27], s[:, 25:26], EPS, None, ALU.add, None, [rs], [rs])
                yield
                act(s[:, 26:27], s[:, 26:27], AF.Sqrt, [rs], [rs])
                yield
                P.op("dve", lambda e, s=s: e.reciprocal(s[:, 26:27], s[:, 26:27]), reads=[rs], writes=[rs])
                ts("dve", xt, xt, s[:, 24:25], s[:, 26:27], ALU.subtract, ALU.mult, [rx, rs], [rx])
                yield
                tt("pool", xt, xt, bA[r], ALU.mult, [rx, r_b], [rx])
                yield
                tt("dve", xt, xt, bB[r], ALU.add, [rx, r_b], [rx])
                yield
                hb, rhb = hbr.next()
                act(hb, xt, AF.Copy, [rx], [rhb])
                yield
                if extra is not None:
                    extra(t, xt, rx)
                    yield
                c = col0(t)
                for g in range(4):
                    bank = 6 + (g % 2)
                    for j in range(4):
                        k = g * 4 + j
                        tr(psb[bank][:, j * 128:(j + 1) * 128], hb[:, k * 128:(k + 1) * 128], [rhb], [rps[bank]])
                    src = psb[bank][:, 0:512].rearrange("p (j t) -> p j t", j=4)
                    dst = hT[:, g * 4:(g + 1) * 4, c:c + 128]
                    if g % 2:
                        P.op("dve", lambda e, dst=dst, src=src: e.tensor_copy(dst, src), reads=[rps[bank]], writes=[r_hT])
                    else:
                        act(dst, src, AF.Copy, [rps[bank]], [r_hT])
                    yield
            interleave((tile_gen(t) for t in tiles), 2)

        def phase_proj(l):
            A.mark()
            DQK = [A.alloc(4 * NT, BF16).rearrange("p (k t) -> p k t", k=4) for _ in range(2)]; r_dqk = [Res(), Res()]
            qk = A.alloc(8, F32); r_qk = Res()
            P.dma("sp", qk, qkn[l], writes=[r_qk])
            RC = {}

            def load_rope():
                ropec = A.alloc(2 * 2048, F32); RC["r"] = Res()
                RC["COS"] = ropec[0:64, 0:2048]; RC["SINS"] = ropec[0:64, 2048:4096]
                P.dma("sp", RC["COS"], ropec_d[0], writes=[RC["r"]])
                P.dma("sp", RC["SINS"], ropec_d[1], writes=[RC["r"]])
                RC["t12"] = Ring(A, 2, 1024, F32)
            A.mark()
            hT = A.alloc(16 * NT, BF16).rearrange("p (k t) -> p k t", k=16); r_hT = Res("hT")
            A.mark()
            ln_mod_T(l, 0, range(NTILE), hT, r_hT, lambda t: t * 128)
            release()
            wfm = Ring(A, 2, 16 * 128, BF16)
            cw = A.alloc(240, F32); r_cw = Res()
            P.dma("sp", cw, convw[l], writes=[r_cw])
            cwv = cw.rearrange("p (c j) -> p c j", j=5)
            W = w_in[l]

            def lin_fm(wt3, rw, kc, m, tok0, ntok, bank, inT, r_in, m0=0, po=0):
                for k in range(kc):
                    mm(ps[bank][po:po + m, 0:ntok], wt3[:, k, m0:m0 + m], inT[:, k, tok0:tok0 + ntok], k == 0, k == kc - 1, [rw, r_in], [rps[bank]])

            A.mark()
            rawr = Ring(A, 1, 2312, F32)
            for rb, rr in rawr.bufs:
                P.op("dve", lambda e, rb=rb: e.memset(rb, 0.0), writes=[rr])
            accr = Ring(A, 1, NT, F32)
            sqr = Ring(A, 1, NT, F32)
            rnr = Ring(A, 2, 512, F32)
            dgr = Ring(A, 10, 128, F32)
            fmr = Ring(A, 2, NT, BF16)
            tmr = Ring(A, 1, NT, BF16)
            bi = 0
            for ch in range(48):
                kind, h = ch // 16, ch % 16
                wt, rw = wfm.next()
                wt3 = wt.rearrange("p (k n) -> p k n", k=16)
                wcast(wt3, W[:, ch * 128:(ch + 1) * 128], rw)
                raw, rr = rawr.next()
                for gi, (t0, n) in enumerate(TOKG):
                    bank = bi % 4; bi += 1
                    lin_fm(wt3, rw, 16, 128, t0, n, bank, hT, r_hT)
                    dst = raw[:, 2:258] if gi == 0 else raw[:, 262 + (t0 - 256):262 + (t0 - 256) + n]
                    act(dst, ps[bank][:, 0:n], AF.Copy, [rps[bank]], [rr])
                acc, ra = accr.next()
                fm, rf = fmr.next()
                dgs = []
                for j in range(5):
                    dg, rdg = dgr.next()
                    act(dg, identf, AF.Copy, [r_const, r_cw], [rdg], scale=cwv[:, ch, j:j + 1])
                    dgs.append((dg, rdg))
                for gi, (t0, n) in enumerate(TOKG):
                    bank = bi % 4; bi += 1
                    ro = 2 if gi == 0 else 262 + (t0 - 256)
                    for j in range(5):
                        mm(ps[bank][:, 0:n], dgs[j][0], raw[:, ro - 2 + j:ro - 2 + j + n], j == 0, j == 4, [dgs[j][1], rr], [rps[bank]])
                    if kind == 2:
                        act(fm[:, t0:t0 + n], ps[bank][:, 0:n], AF.Silu, [rps[bank]], [rf])
                    else:
                        act(acc[:, t0:t0 + n], ps[bank][:, 0:n], AF.Silu, [rps[bank]], [ra])
                if kind != 2:
                    sq, rs = sqr.next()
                    act(sq, acc, AF.Square, [ra], [rs])
                    for (t0, n) in TOKG:
                        bank = bi % 4; bi += 1
                        mm(ps[bank][:, 0:n], ONESF, sq[:, t0:t0 + n], True, True, [rs, r_const], [rps[bank]])
                        rn, rrn = rnr.next()
                        act(rn[:, 0:n], ps[bank][:, 0:n], AF.Sqrt, [rps[bank], r_const], [rrn], bias=epscol)
                        P.op("dve", lambda e, rn=rn, n=n: e.reciprocal(rn[:, 0:n], rn[:, 0:n]), reads=[rrn], writes=[rrn])
                        if kind == 0:
                            stt("dve", fm[:, t0:t0 + n], acc[:, t0:t0 + n], 128 ** -0.5, rn[:, 0:n], ALU.mult, ALU.mult, [ra, rrn], [rf])
                        else:
                            tt("dve", sq[:, t0:t0 + n], acc[:, t0:t0 + n], rn[:, 0:n], ALU.mult, [ra, rrn, rs], [rs])
                            act(fm[:, t0:t0 + n], sq[:, t0:t0 + n], AF.Copy, [rs], [rf])
                    P.dma("sp", (QT if kind == 0 else KT)[h], fm, reads=[rf])
                    if kind == 1:
                        P.dma("sp", KT32[h], sq, reads=[rs])
                if kind >= 1:
                    tm, rt = tmr.next()
                    tm3 = tm.rearrange("p (t d) -> p t d", d=128)
                    for g in range(5):
                        bank = 6 + (g % 2)
                        nn = 4 if g < 4 else 2
                        for j in range(nn):
                            t = g * 4 + j
                            tr(psb[bank][:, j * 128:(j + 1) * 128], fm[:, t * 128:(t + 1) * 128], [rf], [rps[bank]])
                        src = psb[bank][:, 0:nn * 128].rearrange("p (j t) -> p j t", j=nn)
                        if g % 2:
                            P.op("dve", lambda e, tm3=tm3, g=g, nn=nn, src=src: e.tensor_copy(tm3[:, g * 4:g * 4 + nn, :], src), reads=[rps[bank]], writes=[rt])
                        else:
                            act(tm3[:, g * 4:g * 4 + nn, :], src, AF.Copy, [rps[bank]], [rt])
                    dst = (KM if kind == 1 else VM).rearrange("(t p) h d -> p t h d", p=128)[:, :, h, :]
                    P.dma("sp", dst, tm3, reads=[rt])
            release()

            A.mark()
            wtm = Ring(A, 2, 16 * 512, BF16)
            zr = Ring(A, 3, 512, F32)
            for nb in range(4):
                wt, rw = wtm.next()
                wt3 = wt.rearrange("p (k n) -> p k n", k=16)
                wcast(wt3, W[:, 6144 + nb * 512:6144 + (nb + 1) * 512], rw)
                for t in range(NTILE):
                    bank = bi % 4; bi += 1
                    for k in range(16):
                        mm(ps[bank], hT[:, k, t * 128:(t + 1) * 128], wt3[:, k, :], k == 0, k == 15, [rw, r_hT], [rps[bank]])
                    z, rz = zr.next()
                    act(z, ps[bank], AF.Silu, [rps[bank]], [rz])
                    P.dma("sp", ZS[t * 128:(t + 1) * 128, nb * 512:(nb + 1) * 512], z, reads=[rz])

            wt, rw = wtm.next()
            wab = wt[:, 0:16 * 64].rearrange("p (k n) -> p k n", k=16)
            wcast(wab, W[:, 8192:8256], rw)
            negA = A.alloc(32, F32); dtb = A.alloc(32, F32); r_ab = Res()
            P.dma("sp", negA, a_log[l].partition_broadcast(128), writes=[r_ab])
            P.dma("sp", dtb, dt_bias[l].partition_broadcast(128), writes=[r_ab])
            act(negA, negA, AF.Exp, [r_ab], [r_ab])
            ts("dve", negA, negA, -1.0, None, ALU.mult, None, [r_ab], [r_ab])
            tmpr = Ring(A, 2, 64, F32)
            for t in range(NTILE):
                bank = bi % 4; bi += 1
                for k in range(16):
                    mm(ps[bank][:, 0:64], hT[:, k, t * 128:(t + 1) * 128], wab[:, k, :], k == 0, k == 15, [rw, r_hT], [rps[bank]])
                tp, rtp = tmpr.next()
                tt("dve", tp[:, 0:32], ps[bank][:, 0:32], dtb, ALU.add, [rps[bank], r_ab], [rtp])
                act(tp[:, 0:32], tp[:, 0:32], AF.Exp, [rtp], [rtp])
                act(tp[:, 32:64], ps[bank][:, 32:64], AF.Exp, [rps[bank], rtp], [rtp], scale=-1.0)
                act(tp, tp, AF.Ln, [rtp, r_const], [rtp], bias=onecol)
                tt("dve", g_all[:, t, :], tp[:, 0:32], negA, ALU.mult, [rtp, r_ab], [r_gall])
                ts("dve", lnb_all[:, t, :], tp[:, 32:64], -1.0, None, ALU.mult, None, [rtp], [r_gall])

            xnr = Ring(A, 2, 512, BF16)
            ssr = Ring(A, 2, 8, F32)
            junk = A.alloc(512, F32); r_junk = Res()
            for which in range(2):
                wt, rw = wtm.next()
                wt3 = wt.rearrange("p (k n) -> p k n", k=16)
                wcast(wt3, W[:, 8256 + which * 512:8256 + (which + 1) * 512], rw)
                for t in range(NTILE):
                    bank = bi % 4; bi += 1
                    for k in range(16):
                        mm(ps[bank], hT[:, k, t * 128:(t + 1) * 128], wt3[:, k, :], k == 0, k == 15, [rw, r_hT], [rps[bank]])
                    s, rs = ssr.next()
                    act(junk, ps[bank], AF.Square, [rps[bank]], [r_junk, rs], accum_out=s[:, 0:1])
                    ts("dve", s[:, 1:2], s[:, 0:1], 1.0 / 512, EPS, ALU.mult, ALU.add, [rs], [rs])
                    act(s[:, 1:2], s[:, 1:2], AF.Sqrt, [rs], [rs])
                    P.op("dve", lambda e, s=s: e.reciprocal(s[:, 1:2], s[:, 1:2]), reads=[rs], writes=[rs])
                    xn, rxn = xnr.next()
                    act(xn, ps[bank], AF.Copy, [rps[bank], rs], [rxn], scale=s[:, 1:2])
                    tb = 6 + (t % 2)
                    for j in range(4):
                        tr(psb[tb][:, j * 128:(j + 1) * 128], xn[:, j * 128:(j + 1) * 128], [rxn], [rps[tb]])
                    for j in range(4):
                        ts("dve", DQK[which][:, j, t * 128:(t + 1) * 128], psb[tb][:, j * 128:(j + 1) * 128], qk[:, which * 4 + j:which * 4 + j + 1], None, ALU.mult, None, [rps[tb], r_qk], [r_dqk[which]])

            release()
            load_rope()
            PERM = [(0, 16), (16, 0), (32, 48), (48, 32)]
            wt, rw = wfm.next()
            wkr = wt.rearrange("p (k n) -> p k n", k=16)
            P.dma("pool", wkr[:, :, 0:64], W[:, 9280:9344].rearrange("(k p) n -> p k n", p=128), writes=[rw])
            for (dc, sc) in PERM:
                P.dma("pool", wkr[:, :, 64 + dc:64 + dc + 16], W[:, 9280 + sc:9280 + sc + 16].rearrange("(k p) n -> p k n", p=128), writes=[rw])

            def rope_evac(bank_a, bank_b, n, t0, dst, r_dst, scale):
                if t0 == 0:
                    act(dst[0:64, t0:t0 + n], ps[bank_a][0:64, 0:n], AF.Copy, [rps[bank_a]], [r_dst], scale=scale)
                    return
                tb, rtb = RC["t12"].next()
                COS, SINS, r_rope = RC["COS"], RC["SINS"], RC["r"]
                c0 = t0 - 256
                stt("dve", tb[0:64, 0:n], ps[bank_a][0:64, 0:n], scale, COS[:, c0:c0 + n], ALU.mult, ALU.mult, [rps[bank_a], r_rope], [rtb])
                stt("dve", tb[0:64, 512:512 + n], ps[bank_b][0:64, 0:n], scale, SINS[:, c0:c0 + n], ALU.mult, ALU.mult, [rps[bank_b], r_rope, rtb], [rtb])
                tt("dve", dst[0:64, t0:t0 + n], tb[0:64, 0:n], tb[0:64, 512:512 + n], ALU.add, [rtb], [r_dst])

            for (t0, n) in TOKG:
                ba = bi % 4; bi += 1
                bb = bi % 4; bi += 1
                lin_fm(wkr, rw, 16, 64, t0, n, ba, hT, r_hT, m0=0)
                lin_fm(wkr, rw, 16, 64, t0, n, bb, hT, r_hT, m0=64)
                rope_evac(ba, bb, n, t0, KRT, r_krt, 1.0)

            gr = Ring(A, 2, NT, BF16)
            for ch in range(32):
                wt, rw = wfm.next()
                wt3 = wt.rearrange("p (k n) -> p k n", k=16)
                wcast(wt3, W[:, 9344 + ch * 128:9344 + (ch + 1) * 128], rw)
                gt, rg = gr.next()
                for (t0, n) in TOKG:
                    bank = bi % 4; bi += 1
                    lin_fm(wt3, rw, 16, 128, t0, n, bank, hT, r_hT)
                    act(gt[:, t0:t0 + n], ps[bank][:, 0:n], AF.Sigmoid, [rps[bank]], [rg])
                P.dma("sp", GT[ch], gt, reads=[rg])

            release()
            load_rope()
            wqr = Ring(A, 2, 4 * 256, BF16)
            wkr2 = Ring(A, 2, 4 * 256, BF16)
            str_ = Ring(A, 3, NT, BF16)
            vstr = Ring(A, 2, NT, BF16)
            for h in range(H):
                wq, rwq = wqr.next()
                wq3 = wq.rearrange("p (k n) -> p k n", k=4)
                src = w_uq[l]
                P.dma("pool", wq3[:, :, 0:192], src[:, h * 192:(h + 1) * 192].rearrange("(k p) n -> p k n", p=128), writes=[rwq])
                for (dc, sc) in PERM:
                    P.dma("pool", wq3[:, :, 192 + dc:192 + dc + 16], src[:, h * 192 + 128 + sc:h * 192 + 128 + sc + 16].rearrange("(k p) n -> p k n", p=128), writes=[rwq])
                wk, rwk = wkr2.next()
                wk3 = wk.rearrange("p (k n) -> p k n", k=4)
                P.dma("pool", wk3, w_ukv[l][:, h * 256:(h + 1) * 256].rearrange("(k p) n -> p k n", p=128), writes=[rwk])
                sqn, rsqn = str_.next()
                sqr_, rsqr = str_.next()
                skn, rskn = str_.next()
                for (t0, n) in TOKG:
                    bank = bi % 4; bi += 1
                    lin_fm(wq3, rwq, 4, 128, t0, n, bank, DQK[0], r_dqk[0])
                    act(sqn[:, t0:t0 + n], ps[bank][:, 0:n], AF.Copy, [rps[bank]], [rsqn], scale=MLA_SCALE)
                    ba = bi % 4; bi += 1
                    bb = bi % 4; bi += 1
                    lin_fm(wq3, rwq, 4, 64, t0, n, ba, DQK[0], r_dqk[0], m0=128)
                    lin_fm(wq3, rwq, 4, 64, t0, n, bb, DQK[0], r_dqk[0], m0=192)
                    rope_evac(ba, bb, n, t0, sqr_, rsqr, MLA_SCALE)
                    bank = bi % 4; bi += 1
                    lin_fm(wk3, rwk, 4, 128, t0, n, bank, DQK[1], r_dqk[1])
                    P.op("dve", lambda e, skn=skn, t0=t0, n=n, bank=bank: e.tensor_copy(skn[:, t0:t0 + n], ps[bank][:, 0:n]), reads=[rps[bank]], writes=[rskn])
                P.dma("sp", QNT[h], sqn, reads=[rsqn])
                P.dma("sp", QRT[h], sqr_[0:64, :], reads=[rsqr])
                P.dma("sp", KNT[h], skn, reads=[rskn])
                vs, rvs = vstr.next()
                vs3 = vs.rearrange("p (t d) -> p t d", d=128)
                for t in range(NTILE):
                    bank = 4 + (t % 2)
                    for k in range(4):
                        mm(ps[bank][:, 0:128], DQK[1][:, k, t * 128:(t + 1) * 128], wk3[:, k, 128:256], k == 0, k == 3, [rwk, r_dqk[1]], [rps[bank]])
                    act(vs3[:, t, :], ps[bank][:, 0:128], AF.Copy, [rps[bank]], [rvs])
                P.dma("sp", VM2[h].rearrange("(t p) d -> p t d", p=128), vs3, reads=[rvs])
            release()

        def phase_mla(l):
            A.mark()
            qtiles = list(range(NTILE)) if l == 0 else list(range(CTXT, NTILE))
            kn_ = Ring(A, 2, NT, BF16); qn_ = Ring(A, 2, NT, BF16); qr_ = Ring(A, 2, NT, BF16); v_ = Ring(A, 2, NT, BF16)
            p_ = Ring(A, 2, NT, BF16); pt_ = Ring(A, 2, NT, BF16); yb_ = Ring(A, 2, NT, BF16)
            sm_ = Ring(A, 3, 8, F32); ob_ = Ring(A, 2, 128, BF16)
            ei = 0
            for h in range(H):
                knt, rkn = kn_.next(); qnt, rqn = qn_.next(); qrt, rqr = qr_.next(); v, rv = v_.next()
                P.dma("sp", knt, KNT[h], writes=[rkn])
                P.dma("sp", qnt, QNT[h], writes=[rqn])
                P.dma("sp", qrt[0:64, :], QRT[h], writes=[rqr])
                v3 = v.rearrange("p (t d) -> p t d", d=128)
                P.dma("sp", v3, VM2[h].rearrange("(t p) d -> p t d", p=128), writes=[rv])
                yb, ryb = yb_.next()
                for qt in qtiles:
                    qc = slice(qt * 128, (qt + 1) * 128)
                    nk = 256 if qt < CTXT else NT
                    groups = [(0, 256)] if qt < CTXT else [(0, 512), (512, 512), (1024, 512), (1536, 512), (2048, 256)]
                    for gi, (k0, n) in enumerate(groups):
                        mm(ps[gi][:, 0:n], qnt[:, qc], knt[:, k0:k0 + n], True, False, [rqn, rkn], [rps[gi]])
                        mm(ps[gi][:, 0:n], qrt[0:64, qc], KRT[0:64, k0:k0 + n], False, True, [rqr, r_krt], [rps[gi]])
                    S = ps_all[:, 0:nk]
                    rS = [rps[gi] for gi in range(len(groups))]
                    s, rs = sm_.next()
                    P.op("dve", lambda e, s=s, S=S: e.reduce_max(s[:, 0:1], S, AX.X), reads=rS, writes=[rs])
                    ts("dve", s[:, 1:2], s[:, 0:1], -1.0, None, ALU.mult, None, [rs], [rs])
                    p, rp = p_.next()
                    act(p[:, 0:nk], S, AF.Exp, rS + [rs], [rp, rs], bias=s[:, 1:2], scale=1.0, accum_out=s[:, 2:3])
                    P.op("dve", lambda e, s=s: e.reciprocal(s[:, 3:4], s[:, 2:3]), reads=[rs], writes=[rs])
                    pt, rpt = pt_.next()
                    pt3 = pt.rearrange("p (t q) -> p t q", q=128)
                    nkt = nk // 128
                    for g in range((nkt + 3) // 4):
                        bank = 5 + (g % 2)
                        nn = min(4, nkt - 4 * g)
                        for j in range(nn):
                            kt = 4 * g + j
                            tr(psb[bank][:, j * 128:(j + 1) * 128], p[:, kt * 128:(kt + 1) * 128], [rp], [rps[bank]])
                        src = psb[bank][:, 0:nn * 128].rearrange("p (j t) -> p j t", j=nn)
                        ei += 1
                        if ei % 2:
                            P.op("dve", lambda e, pt3=pt3, g=g, nn=nn, src=src: e.tensor_copy(pt3[:, 4 * g:4 * g + nn, :], src), reads=[rps[bank]], writes=[rpt])
                        else:
                            act(pt3[:, 4 * g:4 * g + nn, :], src, AF.Copy, [rps[bank]], [rpt])
                    for kt in range(nkt):
                        mm(ps[7][:, 0:128], pt3[:, kt, :], v3[:, kt, :], kt == 0, kt == nkt - 1, [rpt, rv], [rps[7]])
                    ob, rob = ob_.next()
                    act(ob, ps[7][:, 0:128], AF.Copy, [rps[7], rs], [rob], scale=s[:, 3:4])
                    tr(psb[7][:, 512:640], ob, [rob], [rps[7]])
                    P.op("dve", lambda e, yb=yb, qc=qc: e.tensor_copy(yb[:, qc], psb[7][:, 512:640]), reads=[rps[7]], writes=[ryb])
                if l == 1:
                    P.op("dve", lambda e, yb=yb: e.memset(yb[:, 0:256], 0.0), writes=[ryb])
                P.dma("sp", YBT[h], yb, reads=[ryb])
            release()

        def phase_gdn(l):
            A.mark()
            G = 4
            S32 = A.alloc(32 * 128, F32).rearrange("p (c v) -> p c v", v=128)
            Sbf = A.alloc(32 * 128, BF16).rearrange("p (c v) -> p c v", v=128)
            r_S = [Res() for _ in range(32)]
            P.op("dve", lambda e: e.memset(S32, 0.0), writes=r_S)
            P.op("dve", lambda e: e.memset(Sbf, 0.0), writes=r_S)
            order = {0: list(range(NTILE)), 1: [1, 0] + list(range(NTILE - 1, 1, -1))}
            qT_ = Ring(A, 2, 2048, BF16); kM_ = Ring(A, 2, 2048, BF16); vM_ = Ring(A, 2, 2048, BF16)
            kF_ = Ring(A, 2, 2048, F32)
            o_ = Ring(A, 2, 2048, F32)
            sm_ = Ring(A, 2, 16 * 10, F32)
            mf_ = Ring(A, 96, 128, F32)
            LLB = [[{k: (A.alloc(128, BF16), Res()) for k in ("AT", "wT", "vnew", "kd0", "kd1")} for _ in range(G)] for _ in range(2)]
            LLF = [[{k: (A.alloc(128, F32), Res()) for k in ("u", "tmp")} for _ in range(G)] for _ in range(2)]
            for b_, r_ in mf_.bufs:
                P.op("dve", lambda e, b_=b_: e.memset(b_, 0.0), writes=[r_])
            for par in LLB + LLF:
                for dct in par:
                    for (b_, r_) in dct.values():
                        P.op("dve", lambda e, b_=b_: e.memset(b_, 0.0), writes=[r_])
            slots = [(ps[b][:, 0:128], rps[b]) for b in range(8)]
            sl = [0]

            def slot():
                x = slots[sl[0] % 8]
                sl[0] += 1
                return x

            def cp_act(dst, rdst, src, rsrc):
                act(dst, src, AF.Copy, [rsrc], [rdst])

            def cp_dve(dst, rdst, src, rsrc):
                P.op("dve", lambda e: e.tensor_copy(dst, src), reads=[rsrc], writes=[rdst])
            gpar = [0]
            for s in range(GDN_STEPS):
                for d in (0, 1):
                    t = order[d][s]
                    tc = slice(t * 128, (t + 1) * 128)
                    qTt, rqT = qT_.next(); kMt, rkM = kM_.next(); vMt, rvM = vM_.next()
                    qT3 = qTt.rearrange("p (h t) -> p h t", h=16)
                    kM3 = kMt.rearrange("p (h t) -> p h t", h=16); vM3 = vMt.rearrange("p (h t) -> p h t", h=16)
                    P.dma("sp", qT3, QT[:, :, tc].rearrange("h p t -> p h t"), writes=[rqT])
                    kFt, rkF = kF_.next()
                    kF3 = kFt.rearrange("p (h t) -> p h t", h=16)
                    P.dma("sp", kF3, KT32[:, :, tc].rearrange("h p t -> p h t"), writes=[rkF])
                    P.dma("sp", kMt, KM[tc].rearrange("p h d -> p (h d)"), writes=[rkM])
                    P.dma("sp", vMt, VM[tc].rearrange("p h d -> p (h d)"), writes=[rvM])
                    sm, rsm = sm_.next()
                    smv = sm.rearrange("p (c h) -> p c h", h=16)
                    GC, GB, NGC, EGC, EGB, KD0, EGL0, EGL1, EB, KD1 = [smv[:, i, :] for i in range(10)]
                    gsl = g_all[:, t, d * 16:(d + 1) * 16]
                    pg, rpg = slot()
                    mm(pg[:, 0:16], MCUM[d], gsl, True, True, [r_const, r_gall], [rpg])
                    mm(pg[:, 16:32], MCH[0], gsl, True, True, [r_const, r_gall], [rpg])
                    mm(pg[:, 32:48], MCH[1], gsl, True, True, [r_const, r_gall], [rpg])
                    P.op("dve", lambda e, GC=GC, pg=pg: e.tensor_copy(GC, pg[:, 0:16]), reads=[rpg], writes=[rsm])
                    tt("dve", GB, GC, lnb_all[:, t, d * 16:(d + 1) * 16], ALU.add, [rsm, r_gall], [rsm])
                    ts("dve", NGC, GC, -1.0, None, ALU.mult, None, [rsm], [rsm])
                    P.op("dve", lambda e, KD0=KD0: e.memset(KD0, 0.0), writes=[rsm])
                    P.op("dve", lambda e, KD1=KD1: e.memset(KD1, 0.0), writes=[rsm])
                    tt("dve", KD0[0:64, :], pg[0:64, 16:32], GC[0:64, :], ALU.subtract, [rpg, rsm], [rsm])
                    tt("dve", KD1[64:128, :], pg[64:128, 32:48], GC[64:128, :], ALU.subtract, [rpg, rsm], [rsm])
                    act(EGC, GC, AF.Exp, [rsm], [rsm])
                    act(EGB, GB, AF.Exp, [rsm], [rsm])
                    act(EB, lnb_all[:, t, d * 16:(d + 1) * 16], AF.Exp, [rsm, r_gall], [rsm])
                    act(KD0[0:64, :], KD0[0:64, :], AF.Exp, [rsm], [rsm])
                    act(KD1[64:128, :], KD1[64:128, :], AF.Exp, [rsm], [rsm])
                    act(EGL0, pg[:, 16:32], AF.Exp, [rpg, rsm], [rsm])
                    act(EGL1, pg[:, 32:48], AF.Exp, [rpg, rsm], [rsm])
                    EGL = (EGL0, EGL1)
                    KD = (KD0, KD1)
                    ot, ro = o_.next()
                    o3 = ot.rearrange("p (h v) -> p h v", h=16)
                    for hg in range(0, GDN_HEADS, G):
                        hs = list(range(hg, min(hg + G, GDN_HEADS)))
                        par = gpar[0] % 2
                        gpar[0] += 1
                        C = {h: {} for h in hs}
                        for gi, h in enumerate(hs):
                            C[h]["B"] = LLB[par][gi]; C[h]["F"] = LLF[par][gi]
                        for h in hs:
                            c = C[h]; hc = slice(h, h + 1)
                            c["dg1"] = mf_.next(); c["dg2"] = mf_.next()
                            act(c["dg1"][0], identf, AF.Copy, [r_const, rsm], [c["dg1"][1]], scale=NGC[:, hc])
                            act(c["dg2"][0], identf, AF.Copy, [r_const, rsm], [c["dg2"][1]], scale=GC[:, hc])
                        for h in hs:
                            c = C[h]; hc = slice(h, h + 1)
                            pD, rD = slot()
                            mm(pD, ONESF, c["dg1"][0], True, False, [r_const, c["dg1"][1]], [rD])
                            mm(pD, identf, NEGM_P[d], False, True, [r_const], [rD])
                            c["DP"] = mf_.next()
                            act(c["DP"][0], pD, AF.Exp, [rD, rsm], [c["DP"][1]], bias=GB[:, hc], scale=1.0)
                        for h in hs:
                            c = C[h]; hc = slice(h, h + 1)
                            pD2, rD2 = slot()
                            mm(pD2, ONESF, c["dg2"][0], True, False, [r_const, c["dg2"][1]], [rD2])
                            mm(pD2, identf, NEGM_AT[d], False, True, [r_const], [rD2])
                            c["DAT"] = mf_.next()
                            act(c["DAT"][0], pD2, AF.Exp, [rD2, rsm], [c["DAT"][1]], bias=NGC[:, hc], scale=1.0)
                        for h in hs:
                            c = C[h]
                            pK, rK = slot(); mm(pK, kF3[:, h, :], kF3[:, h, :], True, True, [rkF], [rK])
                            c["P"] = mf_.next()
                            stt("dve", c["P"][0], pK, -1.0, c["DP"][0], ALU.mult, ALU.mult, [rK, c["DP"][1]], [c["P"][1]])
                        for h in hs:
                            c = C[h]
                            c["kb"] = mf_.next()
                        for h in hs:
                            c = C[h]
                            kb = c["kb"][0].bitcast(BF16)[:, 0:128]
                            cp_act(kb, c["kb"][1], kF3[:, h, :], rkF)
                        for h in hs:
                            c = C[h]
                            kb = c["kb"][0].bitcast(BF16)[:, 0:128]
                            pQ, rQ = slot(); mm(pQ, kb, qT3[:, h, :], True, True, [c["kb"][1], rqT], [rQ])
                            AT, rAT = c["B"]["AT"]
                            tt("dve", AT, pQ, c["DAT"][0], ALU.mult, [rQ, c["DAT"][1]], [rAT])
                        for h in hs:
                            c = C[h]
                            pT, rT = slot()
                            tr(pT, c["P"][0], [c["P"][1]], [rT], f32=True)
                            c["Q"] = mf_.next()
                            cp_act(c["Q"][0], c["Q"][1], pT, rT)
                        for h in hs:
                            c = C[h]
                            c["TT"] = mf_.next()
                            tt("pool", c["TT"][0], c["Q"][0], identf, ALU.add, [c["Q"][1], r_const], [c["TT"][1]])
                        for j in range(5):
                            for h in hs:
                                c = C[h]
                                p1, r1 = slot(); mm(p1, c["Q"][0], c["P"][0], True, True, [c["Q"][1], c["P"][1]], [r1])
                                c["Pn"] = mf_.next()
                                (cp_act if j % 2 else cp_dve)(c["Pn"][0], c["Pn"][1], p1, r1)
                            if j < 4:
                                for h in hs:
                                    c = C[h]
                                    p2, r2 = slot(); mm(p2, c["P"][0], c["Q"][0], True, True, [c["Q"][1], c["P"][1]], [r2])
                                    c["Qn"] = mf_.next()
                                    (cp_dve if j % 2 else cp_act)(c["Qn"][0], c["Qn"][1], p2, r2)
                            for h in hs:
                                c = C[h]
                                p3, r3 = slot(); mm(p3, c["Pn"][0], c["TT"][0], True, True, [c["Pn"][1], c["TT"][1]], [r3])
                                c["TTn"] = mf_.next()
                                tt("dve", c["TTn"][0], p3, c["TT"][0], ALU.add, [r3, c["TT"][1]], [c["TTn"][1]])
                            for h in hs:
                                c = C[h]
                                c["P"] = c["Pn"]; c["TT"] = c["TTn"]
                                if j < 4:
                                    c["Q"] = c["Qn"]
                        for h in hs:
                            c = C[h]; hc = slice(h, h + 1)
                            c["vb"] = mf_.next(); c["kbg"] = mf_.next()
                            act(c["vb"][0], vM3[:, h, :], AF.Copy, [rvM, rsm], [c["vb"][1]], scale=EB[:, hc])
                            act(c["kbg"][0], kM3[:, h, :], AF.Copy, [rkM, rsm], [c["kbg"][1]], scale=EGB[:, hc])
                            for ci in (0, 1):
                                kd, rkd = c["B"]["kd%d" % ci]
                                ts("dve", kd, kM3[:, h, :], KD[ci][:, hc], None, ALU.mult, None, [rkM, rsm], [rkd])
                        for h in hs:
                            c = C[h]
                            pU, rU = slot(); mm(pU, c["TT"][0], c["vb"][0], True, True, [c["TT"][1], c["vb"][1]], [rU])
                            u, ru = c["F"]["u"]
                            cp_dve(u, ru, pU, rU)
                        for h in hs:
                            c = C[h]
                            pW, rW = slot(); mm(pW, c["kbg"][0], c["TT"][0], True, True, [c["TT"][1], c["kbg"][1]], [rW])
                            wT, rwT = c["B"]["wT"]
                            cp_act(wT, rwT, pW, rW)
                        for ci in ((0, 1) if d == 0 else (1, 0)):
                            pr = slice(ci * 64, ci * 64 + 64)
                            for h in hs:
                                c = C[h]; dh = d * 16 + h
                                wT, rwT = c["B"]["wT"]; u, ru = c["F"]["u"]; vnew, rvn = c["B"]["vnew"]
                                pv, rv_ = slot()
                                mm(pv, wT, Sbf[:, dh, :], True, True, [rwT, r_S[dh]], [rv_])
                                tt("dve", vnew[pr, :], u[pr, :], pv[pr, :], ALU.subtract, [ru, rv_], [rvn])
                            for h in hs:
                                c = C[h]; dh = d * 16 + h; hc = slice(h, h + 1)
                                tmp, rtmp = c["F"]["tmp"]
                                po1, ro1 = slot()
                                mm(po1, qT3[:, h, :], Sbf[:, dh, :], True, True, [rqT, r_S[dh]], [ro1])
                                act(tmp[pr, :], po1[pr, :], AF.Copy, [ro1, rsm], [rtmp], scale=EGC[pr, hc])
                            for h in hs:
                                c = C[h]
                                AT, rAT = c["B"]["AT"]; vnew, rvn = c["B"]["vnew"]; tmp, rtmp = c["F"]["tmp"]
                                po2, ro2 = slot()
                                mm(po2, AT, vnew, True, True, [rAT, rvn], [ro2])
                                tt("dve", o3[pr, h, :], tmp[pr, :], po2[pr, :], ALU.add, [rtmp, ro2], [ro])
                            for h in hs:
                                c = C[h]; dh = d * 16 + h; hc = slice(h, h + 1)
                                kd, rkd = c["B"]["kd%d" % ci]; vnew, rvn = c["B"]["vnew"]
                                pS, rS_ = slot()
                                mm(pS, kd, vnew, True, True, [rkd, rvn], [rS_])
                                stt("dve", S32[:, dh, :], S32[:, dh, :], EGL[ci][:, hc], pS, ALU.mult, ALU.add, [r_S[dh], rsm, rS_], [r_S[dh]])
                                act(Sbf[:, dh, :], S32[:, dh, :], AF.Copy, [r_S[dh]], [r_S[dh]])
                    P.dma("sp", OFB[d, tc, :], ot, reads=[ro])
            release()

        def phase_gdn_out(l):
            A.mark()
            tiles = list(range(NTILE)) if l == 0 else list(range(CTXT, NTILE))
            gnb = A.alloc(128, F32); r_gn = Res()
            P.dma("sp", gnb, gdn_norm[l].partition_broadcast(128), writes=[r_gn])
            of_ = Ring(A, 2, 2048, F32); ob_ = Ring(A, 2, 2048, F32); z_ = Ring(A, 2, 2048, F32)
            sq_ = Ring(A, 1, 2048, F32); ss_ = Ring(A, 2, 32, F32); yb_ = Ring(A, 2, 2048, BF16); yt_ = Ring(A, 2, 2048, BF16)
            for t in tiles:
                tc = slice(t * 128, (t + 1) * 128)
                of, rof = of_.next(); ob, rob = ob_.next(); z, rz = z_.next()
                P.dma("sp", of, OFB[0, tc, :], writes=[rof])
                P.dma("sp", ob, OFB[1, tc, :], writes=[rob])
                P.dma("sp", z, ZS[tc, :], writes=[rz])
                tt("pool", of, of, ob, ALU.add, [rof, rob], [rof])
                sq, rsq = sq_.next()
                act(sq, of, AF.Square, [rof], [rsq])
                ss, rss = ss_.next()
                P.op("dve", lambda e, ss=ss, sq=sq: e.reduce_sum(ss[:, 0:16], sq.rearrange("p (h v) -> p h v", h=16), AX.X), reads=[rsq], writes=[rss])
                ts("dve", ss[:, 0:16], ss[:, 0:16], 1.0 / 128, EPS, ALU.mult, ALU.add, [rss], [rss])
                act(ss[:, 0:16], ss[:, 0:16], AF.Sqrt, [rss], [rss])
                P.op("dve", lambda e, ss=ss: e.reciprocal(ss[:, 0:16], ss[:, 0:16]), reads=[rss], writes=[rss])
                for h in range(H):
                    hs = slice(h * 128, (h + 1) * 128)
                    stt("dve", of[:, hs], of[:, hs], ss[:, h:h + 1], gnb, ALU.mult, ALU.mult, [rof, rss, r_gn], [rof])
                yb, ryb = yb_.next()
                tt("dve", yb, of, z, ALU.mult, [rof, rz], [ryb])
                yt, ryt = yt_.next()
                yt3 = yt.rearrange("p (h t) -> p h t", h=16)
                for g in range(4):
                    bank = 6 + (g % 2)
                    for j in range(4):
                        k = g * 4 + j
                        tr(psb[bank][:, j * 128:(j + 1) * 128], yb[:, k * 128:(k + 1) * 128], [ryb], [rps[bank]])
                    act(yt3[:, g * 4:(g + 1) * 4, :], psb[bank][:, 0:512].rearrange("p (j t) -> p j t", j=4), AF.Copy, [rps[bank]], [ryt])
                P.dma("sp", YAT[:, :, tc].rearrange("h p t -> p h t"), yt3, reads=[ryt])
            release()

        def resid_ln(xt, rx, r_, rr, gtb, gb_, bb_, r_b, dst, sr):
            tt("pool", r_, r_, gtb, ALU.mult, [rr, r_b], [rr])
            stt("dve", xt, xt, ALPHA, r_, ALU.mult, ALU.add, [rx, rr], [rx])
            s, rs = sr.next()
            st6 = s[:, 0:24].rearrange("p (c s) -> p c s", s=6)
            xv = xt.rearrange("p (c f) -> p c f", f=512)
            for c in range(4):
                P.op("dve", lambda e, c=c: e.bn_stats(st6[:, c, :], xv[:, c, :]), reads=[rx], writes=[rs])
            P.op("dve", lambda e: e.bn_aggr(s[:, 24:26], st6), reads=[rs], writes=[rs])
            ts("dve", s[:, 26:27], s[:, 25:26], EPS, None, ALU.add, None, [rs], [rs])
            act(s[:, 26:27], s[:, 26:27], AF.Sqrt, [rs], [rs])
            P.op("dve", lambda e: e.reciprocal(s[:, 26:27], s[:, 26:27]), reads=[rs], writes=[rs])
            ts("dve", xt, xt, s[:, 24:25], s[:, 26:27], ALU.subtract, ALU.mult, [rx, rs], [rx])
            tt("pool", xt, xt, gb_, ALU.mult, [rx, r_b], [rx])
            tt("dve", xt, xt, bb_, ALU.add, [rx, r_b], [rx])
            P.dma("sp", dst, xt, reads=[rx])

        def load_bcast(l, gt_idx, g_d, b_d):
            r_b = Res()
            gts = []
            for r in (0, 1):
                a = A.alloc(2048, F32)
                P.dma("sp", a, MODV[l, r, gt_idx * 2048:(gt_idx + 1) * 2048].partition_broadcast(128), writes=[r_b])
                gts.append(a)
            gb_ = A.alloc(2048, F32); bb_ = A.alloc(2048, F32)
            P.dma("sp", gb_, g_d[l].partition_broadcast(128), writes=[r_b])
            P.dma("sp", bb_, b_d[l].partition_broadcast(128), writes=[r_b])
            return gts, gb_, bb_, r_b

        def phase_merge(l):
            A.mark()
            groups = TOKG if l == 0 else TOKG[1:]
            gts, gb_, bb_, r_b = load_bcast(l, 2, ln1_g, ln1_b)
            ya_ = Ring(A, 1, 16 * 512, BF16); yb_ = Ring(A, 1, 16 * 512, BF16); yT_ = Ring(A, 1, 16 * 512, BF16)
            w_ = Ring(A, 4, 16 * 128, BF16); g_ = Ring(A, 4, 512, BF16); t_ = Ring(A, 4, 512, F32)
            wo_ = Ring(A, 2, 16 * 256, BF16)
            m_ = Ring(A, 4, 2048, F32); x_ = Ring(A, 1, 2048, F32); sr = Ring(A, 2, 32, F32)
            bi = 0
            for (t0, n) in groups:
                ya, rya = ya_.next(); yb, ryb = yb_.next(); yT, ryT = yT_.next()
                ya3 = ya.rearrange("p (k t) -> p k t", k=16)[:, :, 0:n]; yb3 = yb.rearrange("p (k t) -> p k t", k=16)[:, :, 0:n]
                yT3 = yT.rearrange("p (k t) -> p k t", k=16)
                P.dma("sp", ya3, YAT[:, :, t0:t0 + n].rearrange("h p t -> p h t"), writes=[rya])
                P.dma("sp", yb3, YBT[:, :, t0:t0 + n].rearrange("h p t -> p h t"), writes=[ryb])
                for m in range(16):
                    wa, rwa = w_.next(); wb, rwb = w_.next()
                    wa3 = wa.rearrange("p (k n) -> p k n", k=16); wb3 = wb.rearrange("p (k n) -> p k n", k=16)
                    wcast(wa3, w_br_a[l][:, m * 128:(m + 1) * 128], rwa)
                    wcast(wb3, w_br_b[l][:, m * 128:(m + 1) * 128], rwb)
                    ga, rga = g_.next(); gb2, rgb = g_.next()
                    P.dma("sp", ga[:, 0:n], GT[m][:, t0:t0 + n], writes=[rga])
                    P.dma("sp", gb2[:, 0:n], GT[16 + m][:, t0:t0 + n], writes=[rgb])
                    ba = bi % 6; bi += 1
                    bb = bi % 6; bi += 1
                    for k in range(16):
                        mm(ps[ba][:, 0:n], wa3[:, k, :], ya3[:, k, :], k == 0, k == 15, [rwa, rya], [rps[ba]])
                    for k in range(16):
                        mm(ps[bb][:, 0:n], wb3[:, k, :], yb3[:, k, :], k == 0, k == 15, [rwb, ryb], [rps[bb]])
                    t1, rt1 = t_.next(); t2, rt2 = t_.next()
                    tt("dve", t1[:, 0:n], ps[ba][:, 0:n], ga[:, 0:n], ALU.mult, [rps[ba], rga], [rt1])
                    tt("dve", t2[:, 0:n], ps[bb][:, 0:n], gb2[:, 0:n], ALU.mult, [rps[bb], rgb], [rt2])
                    tt("pool", yT3[:, m, 0:n], t1[:, 0:n], t2[:, 0:n], ALU.add, [rt1, rt2], [ryT])
                ntl = n // 128
                ms = [m_.next() for _ in range(ntl)]
                for cb in range(8):
                    wo, rwo = wo_.next()
                    wo3 = wo.rearrange("p (k n) -> p k n", k=16)
                    wcast(wo3, w_out[l][:, cb * 256:(cb + 1) * 256], rwo)
                    for ti in range(ntl):
                        bank = bi % 6; bi += 1
                        for k in range(16):
                            mm(ps[bank][:, 0:256], yT3[:, k, ti * 128:(ti + 1) * 128], wo3[:, k, :], k == 0, k == 15, [rwo, ryT], [rps[bank]])
                        act(ms[ti][0][:, cb * 256:(cb + 1) * 256], ps[bank][:, 0:256], AF.Copy, [rps[bank]], [ms[ti][1]])
                for ti in range(ntl):
                    t = t0 // 128 + ti
                    xt, rx = x_.next()
                    P.dma("sp", xt, XS[t * 128:(t + 1) * 128, :], writes=[rx])
                    resid_ln(xt, rx, ms[ti][0], ms[ti][1], gts[1 if t < CTXT else 0], gb_, bb_, r_b, XS[t * 128:(t + 1) * 128, :], sr)
            release()

        def phase_ffn(l, last):
            moe = (l % 2 == 1)
            tiles = list(range(NTILE)) if not last else list(range(CTXT, NTILE))
            ntl = len(tiles); ntok = ntl * 128; tb = tiles[0]
            E = NEXP if moe else 1
            FFC = (FF_EXP if moe else FF_DENSE) // 128
            A.mark()
            hT = A.alloc(16 * ntok, BF16).rearrange("p (k t) -> p k t", k=16); r_hT = Res()
            A.mark()
            extra = None
            if moe:
                rwt = A.alloc(128, F32); r_rw = Res()
                P.dma("sp", rwt, router, writes=[r_rw])
                rw3 = rwt.rearrange("p (k e) -> p k e", e=8)
                hf_ = Ring(A, 1, 2048, F32); lg_ = Ring(A, 2, 40, F32)

                def extra(t, xt, rx):
                    ti = t - tb
                    hf, rhf = hf_.next()
                    hf3 = hf.rearrange("p (k t) -> p k t", k=16)
                    for g in range(4):
                        bank = g % 4
                        for j in range(4):
                            k = g * 4 + j
                            tr(ps[bank][:, j * 128:(j + 1) * 128], xt[:, k * 128:(k + 1) * 128], [rx], [rps[bank]], f32=True)
                        act(hf3[:, g * 4:(g + 1) * 4, :], ps[bank].rearrange("p (j t) -> p j t", j=4), AF.Copy, [rps[bank]], [rhf])
                    for k in range(16):
                        mm(ps[4][:, 0:8], hf3[:, k, :], rw3[:, k, :], k == 0, k == 15, [rhf, r_rw], [rps[4]])
                    lg, rlg = lg_.next()
                    P.op("dve", lambda e: e.tensor_copy(lg[:, 0:8], ps[4][:, 0:8]), reads=[rps[4]], writes=[rlg])
                    P.op("dve", lambda e: e.max(lg[:, 8:16], lg[:, 0:8]), reads=[rlg], writes=[rlg])
                    ts("dve", lg[:, 16:24], lg[:, 0:8], lg[:, 9:10], None, ALU.is_ge, None, [rlg], [rlg])
                    ts("dve", lg[:, 32:33], lg[:, 8:9], -1.0, None, ALU.mult, None, [rlg], [rlg])
                    act(lg[:, 24:32], lg[:, 0:8], AF.Exp, [rlg], [rlg], bias=lg[:, 32:33], scale=1.0)
                    tt("dve", lg[:, 24:32], lg[:, 24:32], lg[:, 16:24], ALU.mult, [rlg], [rlg])
                    P.op("dve", lambda e: e.reduce_sum(lg[:, 33:34], lg[:, 24:32], AX.X), reads=[rlg], writes=[rlg])
                    P.op("dve", lambda e: e.reciprocal(lg[:, 33:34], lg[:, 33:34]), reads=[rlg], writes=[rlg])
                    ts("dve", comb[:, ti, :], lg[:, 24:32], lg[:, 33:34], None, ALU.mult, None, [rlg], [r_comb])
            ln_mod_T(l, 1, tiles, hT, r_hT, lambda t: (t - tb) * 128, extra=extra)
            release()
            A.mark()
            w1_ = Ring(A, 2, 16 * 512, BF16); w3_ = Ring(A, 2, 16 * 512, BF16)
            s1_ = Ring(A, 3, 512, F32); st_ = Ring(A, 3, ntok, BF16)
            tgs = [(c0, min(512, ntok - c0)) for c0 in range(0, ntok, 512)]
            bi = 0
            for e in range(E):
                W1 = moe_w1[0, e] if moe else ffn_w1[0]
                W3 = moe_w3[0, e] if moe else ffn_w3[0]
                for fb in range((FFC + 3) // 4):
                    nfc = min(4, FFC - 4 * fb)
                    w1, rw1 = w1_.next(); w3, rw3_ = w3_.next()
                    w13 = w1.rearrange("p (k n) -> p k n", k=16)[:, :, 0:nfc * 128]; w33 = w3.rearrange("p (k n) -> p k n", k=16)[:, :, 0:nfc * 128]
                    wcast(w13, W1[:, fb * 512:fb * 512 + nfc * 128], rw1)
                    wcast(w33, W3[:, fb * 512:fb * 512 + nfc * 128], rw3_)
                    for fc in range(nfc):
                        stg, rst = st_.next()
                        for (c0, n) in tgs:
                            b1 = bi % 8; bi += 1
                            b3 = bi % 8; bi += 1
                            for k in range(16):
                                mm(ps[b1][:, 0:n], w13[:, k, fc * 128:(fc + 1) * 128], hT[:, k, c0:c0 + n], k == 0, k == 15, [rw1, r_hT], [rps[b1]])
                            for k in range(16):
                                mm(ps[b3][:, 0:n], w33[:, k, fc * 128:(fc + 1) * 128], hT[:, k, c0:c0 + n], k == 0, k == 15, [rw3_, r_hT], [rps[b3]])
                            s1, rs1 = s1_.next()
                            act(s1[:, 0:n], ps[b1][:, 0:n], AF.Silu, [rps[b1]], [rs1])
                            tt("dve", stg[:, c0:c0 + n], s1[:, 0:n], ps[b3][:, 0:n], ALU.mult, [rs1, rps[b3]], [rst])
                        P.dma("sp", ATS[e, fb * 4 + fc, :, 0:ntok], stg, reads=[rst])
            release()
            release()
            A.mark()
            HF = FFC // 2
            w2_ = Ring(A, 2, HF * 512, BF16); at_ = Ring(A, 3, HF * 128, BF16)
            acc = A.alloc(ntl * 512, F32).rearrange("p (t n) -> p t n", n=512); r_acc = [Res() for _ in range(ntl)]
            for cb in range(4):
                for e in range(E):
                    W2 = moe_w2[0, e] if moe else ffn_w2[0]
                    for half in range(2):
                        w2, rw2 = w2_.next()
                        w23 = w2.rearrange("p (f n) -> p f n", n=512)
                        wcast(w23, W2[half * HF * 128:(half + 1) * HF * 128, cb * 512:(cb + 1) * 512], rw2)
                        for ti in range(ntl):
                            at, rat = at_.next()
                            at3 = at.rearrange("p (f t) -> p f t", t=128)
                            P.dma("sp", at3, ATS[e, half * HF:(half + 1) * HF, :, ti * 128:(ti + 1) * 128].rearrange("f p t -> p f t"), writes=[rat])
                            bank = bi % 8; bi += 1
                            for f in range(HF):
                                mm(ps[bank], at3[:, f, :], w23[:, f, :], f == 0, f == HF - 1, [rat, rw2], [rps[bank]])
                            first = (e == 0 and half == 0)
                            if moe:
                                if first:
                                    ts("dve", acc[:, ti, :], ps[bank], comb[:, ti, e:e + 1], None, ALU.mult, None, [rps[bank], r_comb], [r_acc[ti]])
                                else:
                                    stt("dve", acc[:, ti, :], ps[bank], comb[:, ti, e:e + 1], acc[:, ti, :], ALU.mult, ALU.add, [rps[bank], r_comb, r_acc[ti]], [r_acc[ti]])
                            else:
                                if first:
                                    act(acc[:, ti, :], ps[bank], AF.Copy, [rps[bank]], [r_acc[ti]])
                                else:
                                    tt("dve", acc[:, ti, :], acc[:, ti, :], ps[bank], ALU.add, [rps[bank], r_acc[ti]], [r_acc[ti]])
                for ti in range(ntl):
                    t = tiles[ti]
                    P.dma("sp", FO[t * 128:(t + 1) * 128, cb * 512:(cb + 1) * 512], acc[:, ti, :], reads=[r_acc[ti]])
            release()
            A.mark()
            gts, gb_, bb_, r_b = load_bcast(l, 5, ln2_g, ln2_b)
            x_ = Ring(A, 2, 2048, F32); f_ = Ring(A, 2, 2048, F32); sr = Ring(A, 2, 32, F32)
            for t in tiles:
                xt, rx = x_.next(); ft, rf = f_.next()
                P.dma("sp", xt, XS[t * 128:(t + 1) * 128, :], writes=[rx])
                P.dma("sp", ft, FO[t * 128:(t + 1) * 128, :], writes=[rf])
                dst = out_d[(t - CTXT) * 128:(t - CTXT + 1) * 128, :] if last else XS[t * 128:(t + 1) * 128, :]
                resid_ln(xt, rx, ft, rf, gts[1 if t < CTXT else 0], gb_, bb_, r_b, dst, sr)
            release()

        GDN_STEPS = 1 if 'gdn_small' in dbg else NTILE
        GDN_HEADS = 1 if 'gdn_small' in dbg else H
        GDN_LVL = 99
        for x in dbg:
            if x.startswith('gdnlvl'):
                GDN_LVL = int(x[6:])
        phase_init()
        for l in range(nlayers):
            last = (l == 1)
            phase_proj(l)
            if "stop_proj" in dbg:
                break
            phase_mla(l)
            if "stop_mla" in dbg:
                break
            phase_gdn(l)
            phase_gdn_out(l)
            if "stop_gdn" in dbg:
                break
            phase_merge(l)
            if "stop_merge" in dbg:
                break
            phase_ffn(l, last)
        nsem = P.emit()
        print("built: ops", {e: len(P.ops[e]) for e in ENGS}, "nsem", nsem, flush=True)
    return nc


def make_inputs(inputs, b, consts):
    f = np.float32
    m = {}
    m["xs"] = np.ascontiguousarray(np.concatenate([inputs["ctx"][b], inputs["x"][b]], axis=0), dtype=f)
    ccv = np.stack([inputs["c"][b], inputs["c_ctx"]], axis=-1)
    m["cc"] = np.ascontiguousarray(ccv.reshape(16, 128, 2).transpose(1, 0, 2).reshape(128, 32), dtype=f)
    for k in ("w_mod", "b_mod", "w_in", "w_uq", "w_ukv", "w_br_a", "w_br_b", "w_out", "ln1_g", "ln1_b", "ln2_g", "ln2_b",
              "ffn_w1", "ffn_w3", "ffn_w2", "moe_w1", "moe_w3", "moe_w2", "gdn_norm"):
        m[k] = np.ascontiguousarray(inputs[k], dtype=f)
    m["convw"] = np.ascontiguousarray(inputs["conv_w"].reshape(2, 5, 48, 128).transpose(0, 3, 2, 1).reshape(2, 128, 240), dtype=f)
    m["a_log"] = np.ascontiguousarray(inputs["a_log"].reshape(2, 32), dtype=f)
    m["dt_bias"] = np.ascontiguousarray(inputs["dt_bias"].reshape(2, 32), dtype=f)
    qn = inputs["q_norm"].reshape(2, 4, 128).transpose(0, 2, 1)
    kn = inputs["kv_norm"].reshape(2, 4, 128).transpose(0, 2, 1)
    m["qkn"] = np.ascontiguousarray(np.concatenate([qn, kn], axis=2), dtype=f)
    m["router"] = np.ascontiguousarray(inputs["moe_router"][0].reshape(16, 128, 8).transpose(1, 0, 2).reshape(128, 128), dtype=f)
    m.update(consts)
    return m


_NC = None


def kernel(**inputs):
    global _NC
    inputs = {k: np.asarray(v) for k, v in inputs.items()}
    consts = host_consts()
    if _NC is None:
        _NC = build(bass.Bass("TRN2", target_bir_lowering=False))
    in_maps = [make_inputs(inputs, b, consts) for b in range(8)]
    res = run_bass_kernel_spmd(_NC, in_maps, core_ids=list(range(8)))
    return np.stack([np.asarray(r["out"], dtype=np.float32) for r in res.results], axis=0)
```
